# Optimizing a Trainium2 kernel written in Bass

```python
import jax
import jax.numpy as jnp
from jax import lax
import numpy as np

D_MODEL = 1024
BATCH = 2
SEQ = 16384
DEPTH = 2

POOL_WIDTH = 512
POOL_GROUPS = 4
POOL_WINDOWS = (2, 4, 8, 16)
SG_WIDTH = 512
SG_HEADS = 4
SG_CHUNK = 128
NSA_Q_HEADS = 8
NSA_KV_HEADS = 2
GROUP = NSA_Q_HEADS // NSA_KV_HEADS
HEAD_DIM = 64
Q_WIDTH = NSA_Q_HEADS * HEAD_DIM
KV_WIDTH = NSA_KV_HEADS * HEAD_DIM
ROT_DIM = HEAD_DIM // 4
ROPE_THETA = 500000.0
CMP_LEN = 32
CMP_STRIDE = 16
CMP_HIDDEN = 256
SLC_LEN = 64
SLC_TOP = 16
WINDOW = 512
Q_BLOCK = 128
NEG_INF = -1e30
FORCE_SCORE = 1e4
D_FF = 2816
N_EXPERTS = 8
TOP_K = 2
EXPERT_BLOCK = 512
PLE_DIM = 256
N_DENSE = (DEPTH + 1) // 2
N_MOE = DEPTH // 2
ALPHA = (2 * DEPTH) ** 0.25
BETA = (8 * DEPTH) ** -0.25

WIDTHS = (POOL_WIDTH, SG_WIDTH, SG_WIDTH, Q_WIDTH,
          KV_WIDTH, KV_WIDTH, KV_WIDTH, KV_WIDTH, KV_WIDTH, KV_WIDTH,
          NSA_Q_HEADS * 3, 3 * D_MODEL)
IN_WIDTH = sum(WIDTHS)
SPLIT_POINTS = tuple(int(v) for v in np.cumsum(WIDTHS)[:-1])

kernel_name = 'hybrid_pool_gmlp_nsa_moe_deepnorm'


def layer_norm(x, g, b, eps=1e-5):
    xf = x.astype(jnp.float32)
    mu = jnp.mean(xf, axis=-1, keepdims=True)
    var = jnp.mean(jnp.square(xf - mu), axis=-1, keepdims=True)
    return ((xf - mu) * lax.rsqrt(var + eps)).astype(x.dtype) * g + b


def partial_rope(x, positions):
    half = ROT_DIM // 2
    inv = ROPE_THETA ** (-jnp.arange(half, dtype=jnp.float32) / half)
    ang = positions.astype(jnp.float32)[:, :, None, None] * inv
    cos = jnp.cos(ang).astype(x.dtype)
    sin = jnp.sin(ang).astype(x.dtype)
    x1 = x[..., :half]
    x2 = x[..., half:ROT_DIM]
    return jnp.concatenate([x1 * cos - x2 * sin, x2 * cos + x1 * sin, x[..., ROT_DIM:]], axis=-1)


def swiglu(x, wg, wu, wd):
    return (jax.nn.silu(x @ wg) * (x @ wu)) @ wd


def pool_mixer(a, pool_w, pool_scale):
    B_, S_, _ = a.shape
    af = a.astype(jnp.float32)
    wmax = max(POOL_WINDOWS)
    cs = jnp.concatenate([jnp.zeros((B_, wmax, POOL_WIDTH), jnp.float32), jnp.cumsum(af, axis=1)], axis=1)
    gw = POOL_WIDTH // POOL_GROUPS
    outs = []
    for g, w in enumerate(POOL_WINDOWS):
        c = cs[:, :, g * gw:(g + 1) * gw]
        win_sum = c[:, wmax:] - c[:, wmax - w:wmax - w + S_]
        count = jnp.minimum(jnp.arange(1, S_ + 1), w).astype(jnp.float32)[None, :, None]
        outs.append(win_sum / count)
    d = (jnp.concatenate(outs, axis=-1) - af).astype(a.dtype).reshape(B_, S_, POOL_GROUPS, gw)
    y = jnp.einsum('bsgc,gcd->bsgd', d, pool_w).reshape(B_, S_, POOL_WIDTH)
    return y * pool_scale


def spatial_gating_mixer(u, v, ln_g, ln_b, sg_w, sg_b):
    B_, S_, _ = u.shape
    u = jax.nn.gelu(u)
    v = layer_norm(jax.nn.gelu(v), ln_g, ln_b)
    nc = S_ // SG_CHUNK
    hc = SG_WIDTH // SG_HEADS
    v = v.reshape(B_, nc, SG_CHUNK, SG_HEADS, hc)
    causal = jnp.tril(jnp.ones((SG_CHUNK, SG_CHUNK), dtype=bool))
    ws = jnp.where(causal[None], sg_w, 0.0)
    mixed = jnp.einsum('gts,bnsgc->bntgc', ws, v) + sg_b.T[None, None, :, :, None]
    return u * mixed.reshape(B_, S_, SG_WIDTH)


def compress(kv, pos_enc, w1, w2):
    B_, H_, S_, d = kv.shape
    ratio = CMP_LEN // CMP_STRIDE
    chunks = kv.reshape(B_, H_, S_ // CMP_STRIDE, CMP_STRIDE, d)
    n_cmp = S_ // CMP_STRIDE - ratio + 1
    blocks = jnp.concatenate([chunks[:, :, r:r + n_cmp] for r in range(ratio)], axis=3)
    blocks = (blocks + pos_enc).reshape(B_, H_, n_cmp, CMP_LEN * d)
    return jax.nn.gelu(blocks @ w1) @ w2


def nsa_attention(q, k_cmp, v_cmp, k_slc, v_slc, k_win, v_win, gates):
    B_, H_, G_, S_, d = q.shape
    n_cmp = k_cmp.shape[2]
    n_slc = S_ // SLC_LEN
    n_top = min(SLC_TOP, n_slc)
    ratio = CMP_LEN // CMP_STRIDE
    n_chunks = S_ // CMP_STRIDE
    k_blk = k_slc.reshape(B_, H_, n_slc, SLC_LEN, d)
    v_blk = v_slc.reshape(B_, H_, n_slc, SLC_LEN, d)
    pad_w = ((0, 0), (0, 0), (WINDOW, 0), (0, 0))
    k_pad = jnp.pad(k_win, pad_w)
    v_pad = jnp.pad(v_win, pad_w)
    cmp_end = jnp.arange(n_cmp) * CMP_STRIDE + CMP_LEN - 1
    blk_id = jnp.arange(n_slc)
    gather = jax.vmap(jax.vmap(lambda tbl, ix: tbl[ix]))

    def block(n):
        qs = n * Q_BLOCK
        t = qs + jnp.arange(Q_BLOCK)
        qb = lax.dynamic_slice_in_dim(q, qs, Q_BLOCK, axis=3)
        gb = lax.dynamic_slice_in_dim(gates, qs, Q_BLOCK, axis=3)
        s = jnp.einsum('bhgqd,bhkd->bhgqk', qb, k_cmp).astype(jnp.float32)
        valid = cmp_end[None, :] <= t[:, None]
        p_cmp = jax.nn.softmax(jnp.where(valid, s, NEG_INF), axis=-1) * valid
        o_cmp = jnp.einsum('bhgqk,bhkd->bhgqd', p_cmp.astype(q.dtype), v_cmp)
        imp = jnp.pad(p_cmp.sum(axis=2), ((0, 0), (0, 0), (0, 0), (ratio - 1, ratio - 1)))
        imp = sum(imp[..., r:r + n_chunks] for r in range(ratio))
        imp = imp.reshape(B_, H_, Q_BLOCK, n_slc, SLC_LEN // CMP_STRIDE).sum(-1)
        cur = t[:, None] // SLC_LEN
        forced = (blk_id == 0) | (blk_id == cur) | (blk_id == cur - 1)
        score = jnp.where(forced, FORCE_SCORE, imp)
        score = jnp.where(blk_id <= cur, score, -1.0)
        _, idx = lax.top_k(score, n_top)
        k_sel = gather(k_blk, idx)
        v_sel = gather(v_blk, idx)
        s = jnp.einsum('bhgqd,bhqnld->bhgqnl', qb, k_sel).astype(jnp.float32)
        pos = idx[..., None] * SLC_LEN + jnp.arange(SLC_LEN)
        ok = (pos <= t[:, None, None])[:, :, None]
        s = jnp.where(ok, s, NEG_INF).reshape(B_, H_, G_, Q_BLOCK, n_top * SLC_LEN)
        p = jax.nn.softmax(s, axis=-1).reshape(B_, H_, G_, Q_BLOCK, n_top, SLC_LEN)
        o_slc = jnp.einsum('bhgqnl,bhqnld->bhgqd', p.astype(q.dtype), v_sel)
        kw = lax.dynamic_slice_in_dim(k_pad, qs, WINDOW + Q_BLOCK, axis=2)
        vw = lax.dynamic_slice_in_dim(v_pad, qs, WINDOW + Q_BLOCK, axis=2)
        s_pos = qs - WINDOW + jnp.arange(WINDOW + Q_BLOCK)
        diff = t[:, None] - s_pos[None, :]
        ok_w = (diff >= 0) & (diff < WINDOW) & (s_pos[None, :] >= 0)
        s = jnp.einsum('bhgqd,bhkd->bhgqk', qb, kw).astype(jnp.float32)
        p = jax.nn.softmax(jnp.where(ok_w, s, NEG_INF), axis=-1)
        o_win = jnp.einsum('bhgqk,bhkd->bhgqd', p.astype(q.dtype), vw)
        return gb[..., 0:1] * o_cmp + gb[..., 1:2] * o_slc + gb[..., 2:3] * o_win

    out = lax.map(block, jnp.arange(S_ // Q_BLOCK))
    return jnp.moveaxis(out, 0, 3).reshape(B_, H_, G_, S_, d)


def token_mixer(x, positions, w_in, pool_w, pool_scale, sg_ln_g, sg_ln_b, sg_w, sg_b,
                cmp_k_pos, cmp_k_w1, cmp_k_w2, cmp_v_pos, cmp_v_w1, cmp_v_w2,
                w_pool_out, w_sg_out, w_nsa_out, w_out):
    B_, S_, _ = x.shape
    proj = x @ w_in
    a, u, v, q, kc, vc, ks, vs, kw, vw, nsa_g, br_g = jnp.split(proj, SPLIT_POINTS, axis=-1)
    y_pool = pool_mixer(a, pool_w, pool_scale) @ w_pool_out
    y_sg = spatial_gating_mixer(u, v, sg_ln_g, sg_ln_b, sg_w, sg_b) @ w_sg_out

    q = partial_rope(q.reshape(B_, S_, NSA_Q_HEADS, HEAD_DIM), positions) * HEAD_DIM ** -0.5
    q = q.reshape(B_, S_, NSA_KV_HEADS, GROUP, HEAD_DIM).transpose(0, 2, 3, 1, 4)

    def kv_heads(t, rope):
        t = t.reshape(B_, S_, NSA_KV_HEADS, HEAD_DIM)
        if rope:
            t = partial_rope(t, positions)
        return t.transpose(0, 2, 1, 3)

    k_cmp = compress(kv_heads(kc, True), cmp_k_pos, cmp_k_w1, cmp_k_w2)
    v_cmp = compress(kv_heads(vc, False), cmp_v_pos, cmp_v_w1, cmp_v_w2)
    gates = jax.nn.sigmoid(nsa_g.reshape(B_, S_, NSA_KV_HEADS, GROUP, 3).transpose(0, 2, 3, 1, 4))
    o = nsa_attention(q, k_cmp, v_cmp, kv_heads(ks, True), kv_heads(vs, False),
                      kv_heads(kw, True), kv_heads(vw, False), gates)
    y_nsa = o.transpose(0, 3, 1, 2, 4).reshape(B_, S_, Q_WIDTH) @ w_nsa_out

    g_pool, g_sg, g_nsa = jnp.split(jax.nn.sigmoid(br_g), 3, axis=-1)
    return (g_pool * y_pool + g_sg * y_sg + g_nsa * y_nsa) @ w_out


def moe_swiglu(x, w_router, b_router, w_gate, w_up, w_down):
    B_, S_, D_ = x.shape
    T = B_ * S_
    TK = T * TOP_K
    xt = x.reshape(T, D_)
    logits = (xt @ w_router).astype(jnp.float32) + b_router.astype(jnp.float32)
    top_logit, top_e = lax.top_k(logits, TOP_K)
    weights = jax.nn.softmax(top_logit, axis=-1)
    flat_e = top_e.reshape(-1)
    flat_tok = jnp.arange(TK, dtype=jnp.int32) // TOP_K
    order = jnp.argsort(flat_e)
    e_sorted = flat_e[order]
    counts = jnp.bincount(flat_e, length=N_EXPERTS)
    padded = (counts + EXPERT_BLOCK - 1) // EXPERT_BLOCK * EXPERT_BLOCK
    start = jnp.cumsum(counts) - counts
    pstart = jnp.cumsum(padded) - padded
    dest = pstart[e_sorted] + jnp.arange(TK) - start[e_sorted]
    n_rows = (-(-TK // EXPERT_BLOCK) + N_EXPERTS) * EXPERT_BLOCK
    n_blocks = n_rows // EXPERT_BLOCK
    row_tok = jnp.full((n_rows,), T, jnp.int32).at[dest].set(flat_tok[order])
    row_w = jnp.zeros((n_rows,), jnp.float32).at[dest].set(weights.reshape(-1)[order])
    block_e = jnp.searchsorted(jnp.cumsum(padded), jnp.arange(n_blocks) * EXPERT_BLOCK, side='right')
    block_e = jnp.minimum(block_e, N_EXPERTS - 1)
    x_rows = jnp.concatenate([xt, jnp.zeros((1, D_), x.dtype)], axis=0)[row_tok]
    x_rows = x_rows.reshape(n_blocks, EXPERT_BLOCK, D_)

    def expert_block(args):
        xb, e = args
        return swiglu(xb, w_gate[e], w_up[e], w_down[e])

    y = lax.map(expert_block, (x_rows, block_e)).reshape(n_rows, D_)
    y = y * row_w[:, None].astype(y.dtype)
    out = jnp.zeros((T + 1, D_), y.dtype).at[row_tok].add(y)[:T]
    return out.reshape(B_, S_, D_)


def setup_inputs(seed: int = 0) -> dict:
    key = jax.random.key(seed)
    keys = iter(jax.random.split(key, 40))

    def nrm(shape, scale):
        return jax.random.normal(next(keys), shape, jnp.float32) * scale

    gw = POOL_WIDTH // POOL_GROUPS
    hc = SG_WIDTH // SG_HEADS
    return {
        'x': nrm((BATCH, SEQ, D_MODEL), 1.0),
        'p': nrm((DEPTH, BATCH, SEQ, PLE_DIM), 1.0),
        'positions': jax.random.randint(next(keys), (BATCH, 1), 0, 4096, jnp.int32) + jnp.arange(SEQ, dtype=jnp.int32)[None, :],
        'w_in': nrm((DEPTH, D_MODEL, IN_WIDTH), D_MODEL ** -0.5),
        'pool_w': nrm((DEPTH, POOL_GROUPS, gw, gw), gw ** -0.5),
        'pool_scale': 1.0 + nrm((DEPTH, POOL_WIDTH), 0.1),
        'sg_ln_g': 1.0 + nrm((DEPTH, SG_WIDTH), 0.05),
        'sg_ln_b': nrm((DEPTH, SG_WIDTH), 0.02),
        'sg_w': nrm((DEPTH, SG_HEADS, SG_CHUNK, SG_CHUNK), 0.5 * SG_CHUNK ** -0.5),
        'sg_b': 1.0 + nrm((DEPTH, SG_HEADS, SG_CHUNK), 0.1),
        'cmp_k_pos': nrm((DEPTH, CMP_LEN, HEAD_DIM), 0.02),
        'cmp_k_w1': nrm((DEPTH, CMP_LEN * HEAD_DIM, CMP_HIDDEN), (CMP_LEN * HEAD_DIM) ** -0.5),
        'cmp_k_w2': nrm((DEPTH, CMP_HIDDEN, HEAD_DIM), CMP_HIDDEN ** -0.5),
        'cmp_v_pos': nrm((DEPTH, CMP_LEN, HEAD_DIM), 0.02),
        'cmp_v_w1': nrm((DEPTH, CMP_LEN * HEAD_DIM, CMP_HIDDEN), (CMP_LEN * HEAD_DIM) ** -0.5),
        'cmp_v_w2': nrm((DEPTH, CMP_HIDDEN, HEAD_DIM), CMP_HIDDEN ** -0.5),
        'w_pool_out': nrm((DEPTH, POOL_WIDTH, D_MODEL), POOL_WIDTH ** -0.5),
        'w_sg_out': nrm((DEPTH, SG_WIDTH, D_MODEL), SG_WIDTH ** -0.5),
        'w_nsa_out': nrm((DEPTH, Q_WIDTH, D_MODEL), Q_WIDTH ** -0.5),
        'w_out': nrm((DEPTH, D_MODEL, D_MODEL), BETA * D_MODEL ** -0.5),
        'ln1_g': 1.0 + nrm((DEPTH, D_MODEL), 0.05),
        'ln1_b': nrm((DEPTH, D_MODEL), 0.02),
        'ffn_w_gate': nrm((N_DENSE, D_MODEL, D_FF), D_MODEL ** -0.5),
        'ffn_w_up': nrm((N_DENSE, D_MODEL, D_FF), D_MODEL ** -0.5),
        'ffn_w_down': nrm((N_DENSE, D_FF, D_MODEL), BETA * D_FF ** -0.5),
        'moe_router': nrm((N_MOE, D_MODEL, N_EXPERTS), D_MODEL ** -0.5),
        'moe_router_b': nrm((N_MOE, N_EXPERTS), 0.01),
        'moe_w_gate': nrm((N_MOE, N_EXPERTS, D_MODEL, D_FF), D_MODEL ** -0.5),
        'moe_w_up': nrm((N_MOE, N_EXPERTS, D_MODEL, D_FF), D_MODEL ** -0.5),
        'moe_w_down': nrm((N_MOE, N_EXPERTS, D_FF, D_MODEL), BETA * D_FF ** -0.5),
        'ple_gate_w': nrm((DEPTH, D_MODEL, D_MODEL), D_MODEL ** -0.5),
        'ple_gate_b': nrm((DEPTH, D_MODEL), 0.02),
        'ple_proj': nrm((DEPTH, PLE_DIM, D_MODEL), BETA * PLE_DIM ** -0.5),
        'ln2_g': 1.0 + nrm((DEPTH, D_MODEL), 0.05),
        'ln2_b': nrm((DEPTH, D_MODEL), 0.02),
    }


def reference(x, p, positions, w_in, pool_w, pool_scale, sg_ln_g, sg_ln_b, sg_w, sg_b,
              cmp_k_pos, cmp_k_w1, cmp_k_w2, cmp_v_pos, cmp_v_w1, cmp_v_w2,
              w_pool_out, w_sg_out, w_nsa_out, w_out, ln1_g, ln1_b,
              ffn_w_gate, ffn_w_up, ffn_w_down,
              moe_router, moe_router_b, moe_w_gate, moe_w_up, moe_w_down,
              ple_gate_w, ple_gate_b, ple_proj, ln2_g, ln2_b):
    for i in range(DEPTH):
        h = token_mixer(x, positions, w_in[i], pool_w[i], pool_scale[i], sg_ln_g[i], sg_ln_b[i],
                        sg_w[i], sg_b[i], cmp_k_pos[i], cmp_k_w1[i], cmp_k_w2[i],
                        cmp_v_pos[i], cmp_v_w1[i], cmp_v_w2[i],
                        w_pool_out[i], w_sg_out[i], w_nsa_out[i], w_out[i])
        x = layer_norm(ALPHA * x + h, ln1_g[i], ln1_b[i])
        j = i // 2
        if i % 2 == 0:
            f = swiglu(x, ffn_w_gate[j], ffn_w_up[j], ffn_w_down[j])
        else:
            f = moe_swiglu(x, moe_router[j], moe_router_b[j], moe_w_gate[j], moe_w_up[j], moe_w_down[j])
        ple = jax.nn.sigmoid(x @ ple_gate_w[i] + ple_gate_b[i]) * (p[i] @ ple_proj[i])
        x = layer_norm(ALPHA * x + f + ple, ln2_g[i], ln2_b[i])
    return x
```

```python
import contextlib
import numpy as np
import ml_dtypes
import concourse.bass as bass
import concourse.mybir as mybir
from concourse.bass_utils import run_bass_kernel_spmd

F32 = mybir.dt.float32
BF16 = mybir.dt.bfloat16
I32 = mybir.dt.int32
AF = mybir.ActivationFunctionType
ALU = mybir.AluOpType
AX = mybir.AxisListType
NPBF = ml_dtypes.bfloat16

D = 1024
POOL_W = 512
SG_W = 512
QW = 512
KVW = 128
HD = 64
DFF = 2816
NEXP = 8
PLE = 256
DEPTH = 2
ALPHA = (2 * DEPTH) ** 0.25
HALO = 16
BLK = 128
EXT = BLK + HALO
NEGM = -30000.0
WIDTHS = (512, 512, 512, 512, 128, 128, 128, 128, 128, 128, 24, 3072)
OFFS = np.concatenate([[0], np.cumsum(WIDTHS)]).astype(int)
(O_A, O_U, O_V, O_Q, O_KC, O_VC, O_KS, O_VS, O_KW, O_VW, O_NG, O_BG) = [int(v) for v in OFFS[:-1]]

COMPUTE = ("tensor", "vector", "scalar", "gpsimd")
SAME_ENGINE_SYNC = True


class Buf:
    __slots__ = ("name", "w", "r", "rd", "psum")

    def __init__(self, name="", psum=False):
        self.name = name
        self.psum = psum
        self.w = None
        self.r = {}
        self.rd = []


class Op:
    __slots__ = ("id", "eng", "fn", "deps", "dma", "tok", "needed")

    def __init__(self, id, eng, fn, deps, dma):
        self.id = id; self.eng = eng; self.fn = fn; self.deps = deps; self.dma = dma
        self.tok = None; self.needed = False


class Sched:
    def __init__(self, nc):
        self.nc = nc
        self.ops = []
        self.per_eng = {e: [] for e in ("tensor", "vector", "scalar", "gpsimd", "sync")}
        self.n_dma_sems = {"sync": 10, "gpsimd": 8, "scalar": 4}

    def op(self, eng, fn, reads=(), writes=(), dma=False):
        deps = set()
        oid = len(self.ops)
        px = [b for b in reads if b.psum and b not in writes]
        if px:
            writes = list(writes) + px
            reads = [b for b in reads if not b.psum]
        for b in reads:
            if b.w is not None:
                deps.add(b.w)
        for b in writes:
            if b.w is not None:
                deps.add(b.w)
            deps.update(b.r.values())
            deps.update(b.rd)
        for b in reads:
            if dma:
                b.rd.append(oid)
            else:
                b.r[eng] = oid
        for b in writes:
            b.w = oid
            b.r = {}
            b.rd = []
        deps.discard(oid)
        o = Op(oid, eng, fn, sorted(deps), dma)
        self.ops.append(o)
        self.per_eng[eng].append(o)
        return o

    def dma(self, q, out_ap, in_ap, reads=(), writes=(), **kw):
        return self.op(q, lambda e: e.dma_start(out=out_ap, in_=in_ap, **kw), reads, writes, dma=True)

    def finish(self, es, final_wait_ops=()):
        nc = self.nc
        ops = self.ops
        for o in ops:
            for d in o.deps:
                p = ops[d]
                if p.dma:
                    continue
                if p.eng != o.eng or o.dma or (SAME_ENGINE_SYNC and o.eng != "tensor"):
                    p.needed = True
        sems = {e: es.enter_context(nc.semaphore("s_" + e)) for e in COMPUTE}
        dsems = {q: [es.enter_context(nc.semaphore(f"d_{q}{i}")) for i in range(n)]
                 for q, n in self.n_dma_sems.items()}
        cnt = {e: 0 for e in COMPUTE}
        dcnt = {q: [0] * n for q, n in self.n_dma_sems.items()}
        drr = {q: 0 for q in self.n_dma_sems}
        prev_on_sem = {}
        for o in ops:
            if o.dma:
                j = drr[o.eng]; drr[o.eng] = (j + 1) % len(dsems[o.eng])
                dcnt[o.eng][j] += 16
                key = (o.eng, j)
                o.tok = (dsems[o.eng][j], dcnt[o.eng][j], key, prev_on_sem.get(key))
                prev_on_sem[key] = o.id
            elif o.needed:
                cnt[o.eng] += 1
                o.tok = (sems[o.eng], cnt[o.eng], o.eng, None)
        self.stats = dict(cnt=cnt, nops=len(ops), per_eng={e: len(v) for e, v in self.per_eng.items()})
        block = es.enter_context(nc.Block())

        def emit(engname, eng):
            seen = {}
            for o in self.per_eng[engname]:
                need = {}
                for d in o.deps:
                    p = ops[d]
                    if (not p.dma) and p.eng == o.eng and not o.dma and not (SAME_ENGINE_SYNC and o.eng != "tensor"):
                        continue
                    sem, val, key, _ = p.tok
                    if need.get(key, (None, 0))[1] < val:
                        need[key] = (sem, val)
                if o.dma and o.tok[3] is not None:
                    sem, val, key, _ = ops[o.tok[3]].tok
                    if need.get(key, (None, 0))[1] < val:
                        need[key] = (sem, val)
                for key, (sem, val) in need.items():
                    if seen.get(key, 0) < val:
                        eng.wait_ge(sem, val)
                        seen[key] = val
                ins = o.fn(eng)
                if o.dma:
                    ins.then_inc(o.tok[0], 16)
                elif o.needed:
                    ins.then_inc(o.tok[0], 1)
            if engname == "sync":
                for o in final_wait_ops:
                    sem, val, key, _ = o.tok
                    if seen.get(key, 0) < val:
                        eng.wait_ge(sem, val)
                        seen[key] = val

        @block.tensor
        def _(e):
            emit("tensor", e)

        @block.vector
        def _(e):
            emit("vector", e)

        @block.scalar
        def _(e):
            emit("scalar", e)

        @block.gpsimd
        def _(e):
            emit("gpsimd", e)

        @block.sync
        def _(e):
            emit("sync", e)


class Ctx:
    def __init__(self, name="k"):
        self.nc = bass.Bass("TRN2", target_bir_lowering=False)
        self.S = Sched(self.nc)
        self.es = contextlib.ExitStack()
        self.outs = []
        self._n = 0
        self.psum = []
        self._pi = 0
        self._dq = 0

    def din(self, name, shape, dt=F32):
        return self.nc.dram_tensor(name, list(shape), dt, kind="ExternalInput").ap()

    def dout(self, name, shape, dt=F32):
        return self.nc.dram_tensor(name, list(shape), dt, kind="ExternalOutput").ap()

    def sb(self, shape, dt=F32, name=None, es=None):
        self._n += 1
        t = (es or self.es).enter_context(self.nc.sbuf_tensor(name or f"t{self._n}", list(shape), dt))
        return t

    def init_psum(self, n=8):
        for i in range(n):
            t = self.es.enter_context(self.nc.psum_tensor(f"ps{i}", [128, 512], F32))
            self.psum.append((t, Buf(f"ps{i}", psum=True)))

    def ps(self):
        p = self.psum[self._pi % len(self.psum)]
        self._pi += 1
        return p

    def q(self):
        self._dq += 1
        return ("sync", "gpsimd")[self._dq % 2]

    def mm(self, out, lhsT, rhs, start, stop, reads, writes):
        return self.S.op("tensor", lambda e: e.matmul(out, lhsT=lhsT, rhs=rhs, start=start, stop=stop),
                         reads, writes)

    def tr(self, out, in_, ident, reads, writes):
        return self.S.op("tensor", lambda e: e.transpose(out, in_, ident), reads, writes)

    def act(self, out, in_, func, reads, writes, **kw):
        return self.S.op("scalar", lambda e: e.activation(out=out, in_=in_, func=func, **kw), reads, writes)

    def v(self, meth, reads, writes, eng="vector", **kw):
        return self.S.op(eng, lambda e: getattr(e, meth)(**kw), reads, writes)

    def load(self, out, in_, writes, reads=(), q=None, **kw):
        return self.S.dma(q or self.q(), out, in_, reads=reads, writes=writes, **kw)

    def store(self, out, in_, reads, q=None, writes=(), **kw):
        o = self.S.dma(q or self.q(), out, in_, reads=reads, writes=writes, **kw)
        self.outs.append(o)
        return o

    def finish(self):
        self.S.finish(self.es, self.outs)
        self.es.close()
        return self.nc

    def load_cast(self, dst, dst_buf, src, stage_pool, eng_cycle=("vector", "gpsimd")):
        st, sbuf = stage_pool[self._n % len(stage_pool)]
        self._n += 1
        shp = list(dst.shape)
        view = st
        n = 1
        for s in shp[1:]:
            n *= s
        sv = st[0:shp[0], 0:n]
        if len(shp) == 3:
            sv = sv.rearrange("p (a b) -> p a b", b=shp[2])
        self.load(sv, src, writes=[sbuf])
        eng = eng_cycle[self._n % len(eng_cycle)]
        self.v("tensor_copy", [sbuf], [dst_buf], eng=eng, out=dst, in_=sv)


def make_consts():
    ident = np.eye(128, dtype=np.float32)
    perm = np.zeros((128, 128), np.float32)
    for hh in range(2):
        for d in range(64):
            if d < 8:
                perm[hh * 64 + d + 8, hh * 64 + d] = 1
            elif d < 16:
                perm[hh * 64 + d - 8, hh * 64 + d] = 1
            else:
                perm[hh * 64 + d, hh * 64 + d] = 1
    half = 8
    inv = (500000.0 ** (-np.arange(half, dtype=np.float32) / half)).astype(np.float32)
    invf = np.zeros((128, 1), np.float32)
    sgn = np.zeros((128, 1), np.float32)
    for hh in range(2):
        for d in range(16):
            invf[hh * 64 + d, 0] = inv[d % 8]
            sgn[hh * 64 + d, 0] = -1.0 if d < 8 else 1.0
    return dict(ident=ident, perm=perm, invf=invf, sgn=sgn)


def load_x_tile(c, xin, blk0, nb, xt, bxt, xh, bxh):
    for b in range(nb):
        r0 = (blk0 + b) * EXT
        c.load(xt[:, b, :], xin[r0 + HALO:r0 + EXT, :], writes=[bxt])
        if xh is not None:
            c.load(xh[16 * b:16 * b + 16, :], xin[r0:r0 + HALO, :], writes=[bxh])


def transpose_x(c, xt, bxt, nb, xT, bxT, ident, bid, xh=None, bxh=None, xTh=None, bxTh=None):
    for k in range(8):
        pt, pb = c.ps()
        for b in range(nb):
            c.tr(pt[:, b * 128:(b + 1) * 128], xt[:, b, k * 128:(k + 1) * 128], ident[:, :], [bxt, bid], [pb])
        if k % 2 == 0:
            c.v("tensor_copy", [pb], [bxT], out=xT[:, k, 0:nb * 128], in_=pt[:, 0:nb * 128])
        else:
            c.act(xT[:, k, 0:nb * 128], pt[:, 0:nb * 128], AF.Copy, [pb], [bxT])
        if xh is not None:
            pt, pb = c.ps()
            c.tr(pt[:, 0:128], xh[:, k * 128:(k + 1) * 128], ident[:, :], [bxh, bid], [pb])
            c.v("tensor_copy", [pb], [bxTh], out=xTh[:, k, 0:16 * nb], in_=pt[:, 0:16 * nb])


WA_COLS = 1816
A_Q, A_KC, A_VC, A_KS, A_KW, A_VS, A_VW, A_NG = 0, 1024, 1152, 1280, 1408, 1536, 1664, 1792


def build_s1a(NB, stop=99):
    c = Ctx()
    nc = c.nc
    T = NB * BLK
    TB = min(2, NB)
    NT = NB // TB
    NS = NB * 8
    xin = c.din("xin", [NB * EXT, D])
    pos = c.din("pos", [1, NB * EXT], I32)
    wa = c.din("wa", [D, WA_COLS])
    kposT = c.din("kposT", [64, 32]); kw1 = c.din("kw1", [2048, 256]); kw2 = c.din("kw2", [256, 64])
    vposT = c.din("vposT", [64, 32]); vw1 = c.din("vw1", [2048, 256]); vw2 = c.din("vw2", [256, 64])
    identd = c.din("ident", [128, 128]); permd = c.din("perm", [128, 128])
    invfd = c.din("invf", [128, 1]); sgnd = c.din("sgn", [128, 1])
    QT = c.dout("QT", [8, 64, T], BF16)
    gates = c.dout("gates", [T, 24])
    KsT = c.dout("KsT", [128, T], BF16); KwT = c.dout("KwT", [128, T], BF16)
    Vs = c.dout("Vs", [T, 128], BF16); Vw = c.dout("Vw", [T, 128], BF16)
    kcmpT = c.dout("kcmpT", [2, 64, NS], BF16); vcmp = c.dout("vcmp", [NS, 128], BF16)
    c.init_psum()

    ident = c.sb([128, 128]); bid = Buf()
    c.load(ident[:, :], identd[:, :], [bid])
    stage = [(c.sb([128, 2048]), Buf()) for _ in range(1)]
    permb = c.sb([128, 128], BF16); bperm = Buf()
    c.load_cast(permb[:, :], bperm, permd[:, :], stage)
    invf = c.sb([128, 1]); sgn = c.sb([128, 1]); bsm = Buf()
    c.load(invf[:, :], invfd[:, :], [bsm]); c.load(sgn[:, :], sgnd[:, :], [bsm])
    wab = c.sb([128, 8, WA_COLS], BF16); bwa = Buf()
    for k in range(8):
        c.load_cast(wab[:, k, :], bwa, wa[k * 128:(k + 1) * 128, :], stage)
    w1b = {}; w2b = {}; pTb = {}; bcw = Buf()
    for nm, w1d, w2d, pd in (("k", kw1, kw2, kposT), ("v", vw1, vw2, vposT)):
        w1b[nm] = c.sb([128, 32, 256], BF16)
        w1v = w1d.rearrange("(l d) h -> d l h", d=64)
        for half in range(2):
            for l0 in range(0, 32, 8):
                c.load_cast(w1b[nm][half * 64:half * 64 + 64, l0:l0 + 8, :], bcw, w1v[:, l0:l0 + 8, :], stage)
        w2b[nm] = c.sb([128, 2, 64], BF16)
        c.load_cast(w2b[nm][:, :, :], bcw, w2d.rearrange("(c p) d -> p c d", p=128), stage)
        pTb[nm] = c.sb([128, 32], BF16)
        c.v("memset", [], [bcw], ap=pTb[nm][:, :], constant=0.0)
        c.load_cast(pTb[nm][0:64, :], bcw, pd[:, :], stage)
    if stop <= 0:
        return c.finish()
    NP = NB * EXT
    Ct, bC, St, bS = rope_tables(c, pos, NP, invf, sgn, bsm)
    C3 = Ct[:, :].rearrange("p (n e) -> p n e", e=EXT)
    S3 = St[:, :].rearrange("p (n e) -> p n e", e=EXT)
    if stop <= 1:
        return c.finish()
    kcx = [c.sb([128, NB, EXT], BF16) for _ in range(2)]; vcx = [c.sb([128, NB, EXT], BF16) for _ in range(2)]
    bkcx = Buf(); bvcx = Buf()
    for t_ in kcx:
        c.v("memset", [], [bkcx], eng="gpsimd", ap=t_[:, :, :], constant=0.0)
    for t_ in vcx:
        c.v("memset", [], [bvcx], eng="gpsimd", ap=t_[:, :, :], constant=0.0)

    xt = [c.sb([128, TB, D]) for _ in range(2)]; bxt = [Buf(), Buf()]
    _xh = c.sb([128, D]); _bxh = Buf()
    xh = [_xh, _xh]; bxh = [_bxh, _bxh]
    c.v("memset", [], [_bxh], eng="gpsimd", ap=_xh[:, :], constant=0.0)
    xT = [c.sb([128, 8, TB * 128], BF16) for _ in range(2)]; bxT = [Buf(), Buf()]
    xTh = [c.sb([128, 8, 128], BF16) for _ in range(2)]; bxTh = [Buf(), Buf()]
    _q = c.sb([128, 8, TB * 128], BF16); _bq = Buf()
    qst = [_q, _q]; bqst = [_bq, _bq]
    kst = [c.sb([128, 2, TB * 128], BF16) for _ in range(2)]; bkst = [Buf(), Buf()]
    vsw = [c.sb([128, TB, 256], BF16) for _ in range(2)]; bvsw = [Buf(), Buf()]
    gts = [c.sb([128, TB, 24]) for _ in range(2)]; bgts = [Buf(), Buf()]
    rope = RopeEvac(c, permb, bperm, TB * 128, bC, bS, nbuf=2)

    for ti in range(NT):
        p = ti % 2
        blk0 = ti * TB
        load_x_tile(c, xin, blk0, TB, xt[p], bxt[p], xh[p], bxh[p])
        transpose_x(c, xt[p], bxt[p], TB, xT[p], bxT[p], ident, bid, xh[p], bxh[p], xTh[p], bxTh[p])
        if stop <= 1.1:
            continue
        NC = TB * 128
        Cown = C3[:, blk0:blk0 + TB, HALO:EXT]; Sown = S3[:, blk0:blk0 + TB, HALO:EXT]
        Chal = C3[:, blk0:blk0 + TB, 0:HALO]; Shal = S3[:, blk0:blk0 + TB, 0:HALO]

        def proj(col, xTt, bx, ncols):
            pt, pb = c.ps()
            for k in range(8):
                c.mm(pt[:, 0:ncols], wab[:, k, col:col + 128], xTt[:, k, 0:ncols], k == 0, k == 7, [bwa, bx], [pb])
            return pt, pb

        v3 = lambda ap: ap.rearrange("p (n e) -> p n e", n=TB)
        for h in range(8):
            pt, pb = proj(A_Q + 128 * h, xT[p], bxT[p], NC)
            rope(pt, pb, NC, 0.125, Cown, Sown, [(slice(0, 128), v3(qst[p][:, h, :]), bqst[p])])
        if stop <= 1.2:
            continue
        c.store(QT[:, :, blk0 * 128:blk0 * 128 + NC].rearrange("h d t -> d h t"), qst[p][0:64, :, :], [bqst[p]])
        if stop <= 1.3:
            continue
        for j, col in enumerate((A_KS, A_KW)):
            pt, pb = proj(col, xT[p], bxT[p], NC)
            rope(pt, pb, NC, 1.0, Cown, Sown, [(slice(0, 128), v3(kst[p][:, j, :]), bkst[p])])
        c.store(KsT[:, blk0 * 128:blk0 * 128 + NC], kst[p][:, 0, :], [bkst[p]])
        c.store(KwT[:, blk0 * 128:blk0 * 128 + NC], kst[p][:, 1, :], [bkst[p]])
        pt, pb = proj(A_KC, xT[p], bxT[p], NC)
        rope(pt, pb, NC, 1.0, Cown, Sown, [(slice(0, 64), kcx[0][0:64, blk0:blk0 + TB, HALO:EXT], bkcx),
                                           (slice(64, 128), kcx[1][64:128, blk0:blk0 + TB, HALO:EXT], bkcx)])
        pt, pb = proj(A_KC, xTh[p], bxTh[p], 16 * TB)
        rope(pt, pb, 16 * TB, 1.0, Chal, Shal, [(slice(0, 64), kcx[0][0:64, blk0:blk0 + TB, 0:HALO], bkcx),
                                                (slice(64, 128), kcx[1][64:128, blk0:blk0 + TB, 0:HALO], bkcx)])
        pt, pb = proj(A_VC, xT[p], bxT[p], NC)
        for hh in range(2):
            ps_ = slice(hh * 64, hh * 64 + 64)
            c.act(vcx[hh][ps_, blk0:blk0 + TB, HALO:EXT], v3(pt[ps_, 0:NC]), AF.Copy, [pb], [bvcx])
        pt, pb = proj(A_VC, xTh[p], bxTh[p], 16 * TB)
        for hh in range(2):
            ps_ = slice(hh * 64, hh * 64 + 64)
            c.act(vcx[hh][ps_, blk0:blk0 + TB, 0:HALO], v3(pt[ps_, 0:16 * TB]), AF.Copy, [pb], [bvcx])
        if stop <= 1.4:
            continue
        for b in range(TB):
            pt, pb = c.ps()
            for k in range(8):
                c.mm(pt[:, 0:280], xT[p][:, k, b * 128:(b + 1) * 128], wab[:, k, A_VS:A_VS + 280], k == 0, k == 7,
                     [bwa, bxT[p]], [pb])
            c.v("tensor_copy", [pb], [bvsw[p]], out=vsw[p][:, b, :], in_=pt[:, 0:256])
            c.act(gts[p][:, b, :], pt[:, 256:280], AF.Sigmoid, [pb], [bgts[p]])
        rows = slice(blk0 * 128, blk0 * 128 + NC)
        c.store(Vs[rows, :].rearrange("(n p) f -> p n f", p=128), vsw[p][:, :, 0:128], [bvsw[p]])
        c.store(Vw[rows, :].rearrange("(n p) f -> p n f", p=128), vsw[p][:, :, 128:256], [bvsw[p]])
        c.store(gates[rows, :].rearrange("(n p) f -> p n f", p=128), gts[p][:, :, :], [bgts[p]])

    if stop <= 2:
        return c.finish()
    cb = c.sb([128, 2, 2]); bcb = Buf()
    gh = c.sb([128, 2, NS], BF16); bgh = Buf()
    kco = c.sb([64, 2, NS], BF16); bkco = Buf()
    vco = c.sb([128, (NS + 127) // 128, 128], BF16); bvco = Buf()
    for kvi, (nm, ext, bext) in enumerate((("k", kcx, bkcx), ("v", vcx, bvcx))):
        for cc in range(2):
            pt, pb = c.ps()
            for l in range(32):
                c.mm(pt[:, 0:1], w1b[nm][:, l, cc * 128:(cc + 1) * 128], pTb[nm][:, l:l + 1], l == 0, l == 31,
                     [bcw], [pb])
            c.v("tensor_copy", [pb], [bcb], out=cb[:, kvi, cc:cc + 1], in_=pt[:, 0:1])
        for h in range(2):
            for cc in range(2):
                pt, pb = c.ps()
                for l in range(32):
                    rhs = ext[h][:, :, :].rearrange("p n (s r) -> p n s r", r=16)[:, :, l // 16:l // 16 + 8, l % 16]
                    c.mm(pt[:, 0:NS].rearrange("p (n s) -> p n s", s=8), w1b[nm][:, l, cc * 128:(cc + 1) * 128], rhs,
                         l == 0, l == 31, [bcw, bext], [pb])
                c.act(gh[:, cc, :], pt[:, 0:NS], AF.Gelu_apprx_tanh, [pb, bcb], [bgh], bias=cb[:, kvi, cc:cc + 1])
            if nm == "k":
                pt, pb = c.ps()
                for cc in range(2):
                    c.mm(pt[0:64, 0:NS], w2b[nm][:, cc, :], gh[:, cc, :], cc == 0, cc == 1, [bcw, bgh], [pb])
                c.v("tensor_copy", [pb], [bkco], out=kco[:, h, :], in_=pt[0:64, 0:NS])
            else:
                for s0 in range(0, NS, 128):
                    sn = min(128, NS - s0)
                    pt, pb = c.ps()
                    for cc in range(2):
                        c.mm(pt[0:sn, 0:64], gh[:, cc, s0:s0 + sn], w2b[nm][:, cc, :], cc == 0, cc == 1, [bcw, bgh], [pb])
                    c.v("tensor_copy", [pb], [bvco], out=vco[0:sn, s0 // 128, h * 64:(h + 1) * 64], in_=pt[0:sn, 0:64])
    c.store(kcmpT.rearrange("h d s -> d h s"), kco[:, :, :], [bkco])
    if NS >= 128:
        c.store(vcmp.rearrange("(n p) f -> p n f", p=128), vco[:, :, :], [bvco])
    else:
        c.store(vcmp[:, :], vco[0:NS, 0, :], [bvco])
    return c.finish()


def rope_tables(c, pos, NP, invf, sgn, bsm):
    Ct = c.sb([128, NP]); St = c.sb([128, NP]); bC = Buf(); bS = Buf()
    CH = 576
    posi = c.sb([128, CH], I32); bpos = Buf()
    ang = c.sb([128, CH]); bang = Buf()
    tmpA = c.sb([128, CH]); tmpB = c.sb([128, CH]); tmpI = c.sb([128, CH], I32); btmp = Buf()
    TWO_PI = float(2 * np.pi)
    for c0 in range(0, NP, CH):
        w = min(CH, NP - c0)
        c.load(posi[:, 0:w], pos[0:1, c0:c0 + w].partition_broadcast(128), [bpos])
        c.v("tensor_copy", [bpos], [bang], out=ang[:, 0:w], in_=posi[:, 0:w])
        c.v("tensor_scalar", [bang, bsm], [bang], out=ang[:, 0:w], in0=ang[:, 0:w], scalar1=invf[:, 0:1], scalar2=None,
            op0=ALU.mult)
        for dst, bdst, shift in ((St, bS, 0.0), (Ct, bC, float(np.pi / 2))):
            A = tmpA[:, 0:w]; B = tmpB[:, 0:w]; I_ = tmpI[:, 0:w]
            c.v("tensor_scalar", [bang], [btmp], out=A, in0=ang[:, 0:w], scalar1=shift, scalar2=None, op0=ALU.add)
            c.v("tensor_scalar", [btmp], [btmp], out=I_, in0=A, scalar1=float(1 / TWO_PI), scalar2=None, op0=ALU.mult)
            c.v("tensor_copy", [btmp], [btmp], out=B, in_=I_)
            c.v("scalar_tensor_tensor", [btmp], [btmp], out=A, in0=B, scalar=-TWO_PI, in1=A, op0=ALU.mult, op1=ALU.add)
            c.v("tensor_scalar", [btmp], [btmp], out=B, in0=A, scalar1=float(np.pi), scalar2=-TWO_PI, op0=ALU.is_gt,
                op1=ALU.mult)
            c.v("tensor_tensor", [btmp], [btmp], out=A, in0=A, in1=B, op=ALU.add)
            c.v("tensor_scalar", [btmp], [btmp], out=B, in0=A, scalar1=float(-np.pi), scalar2=TWO_PI, op0=ALU.is_lt,
                op1=ALU.mult)
            c.v("tensor_tensor", [btmp], [btmp], out=A, in0=A, in1=B, op=ALU.add)
            c.v("tensor_scalar", [btmp], [btmp], out=A, in0=A, scalar1=float(np.pi), scalar2=float(-np.pi), op0=ALU.min,
                op1=ALU.max)
            c.act(dst[:, c0:c0 + w], A, AF.Sin, [btmp], [bdst])
    c.v("tensor_scalar", [bS, bsm], [bS], out=St[:, :], in0=St[:, :], scalar1=sgn[:, 0:1], scalar2=None, op0=ALU.mult)
    return Ct, bC, St, bS


ROPE_DBG = 99


class RopeEvac:
    def __init__(self, c, permb, bperm, maxcols, bC, bS, nbuf=3):
        self.c = c; self.permb = permb; self.bperm = bperm; self.bC = bC; self.bS = bS
        self.n = nbuf; self.i = 0
        self.kb = [c.sb([128, maxcols], BF16) for _ in range(nbuf)]; self.bkb = [Buf() for _ in range(nbuf)]
        self.t1 = [c.sb([128, maxcols]) for _ in range(nbuf)]; self.bt1 = [Buf() for _ in range(nbuf)]
        self.t2 = [c.sb([128, maxcols]) for _ in range(nbuf)]; self.bt2 = [Buf() for _ in range(nbuf)]

    def __call__(self, pt, pb, ncols, scale, Cap, Sap, dsts):
        c = self.c
        i = self.i % self.n; self.i += 1
        nbk = Cap.shape[1]
        kb, t1, t2 = self.kb[i], self.t1[i], self.t2[i]
        c.act(kb[:, 0:ncols], pt[:, 0:ncols], AF.Copy, [pb], [self.bkb[i]], scale=float(scale))
        if ROPE_DBG <= 0:
            return
        pp, ppb = c.ps()
        c.mm(pp[:, 0:ncols], self.permb[:, :], kb[:, 0:ncols], True, True, [self.bperm, self.bkb[i]], [ppb])
        v3 = lambda ap: ap.rearrange("p (n e) -> p n e", n=nbk)
        if ROPE_DBG <= 1:
            return
        c.v("scalar_tensor_tensor", [pb, self.bC], [self.bt1[i]], out=v3(t1[:, 0:ncols]), in0=v3(pt[:, 0:ncols]),
            scalar=float(scale), in1=Cap, op0=ALU.mult, op1=ALU.mult)
        if ROPE_DBG <= 2:
            return
        c.v("tensor_tensor", [ppb, self.bS], [self.bt2[i]], out=v3(t2[:, 0:ncols]), in0=v3(pp[:, 0:ncols]), in1=Sap,
            op=ALU.mult)
        if ROPE_DBG <= 3:
            return
        for (psl, dst3, bdst) in dsts:
            c.v("tensor_tensor", [self.bt1[i], self.bt2[i]], [bdst], eng="gpsimd", out=dst3,
                in0=v3(t1[psl, 0:ncols]), in1=v3(t2[psl, 0:ncols]), op=ALU.add)


def core_block_ids(S, core):
    nblk = S // BLK
    return core // 4, list(range(core % 4, nblk, 4))


def make_xin(xb, blocks):
    S, F = xb.shape
    out = np.zeros((len(blocks) * EXT, F), xb.dtype)
    for i, n in enumerate(blocks):
        lo = n * BLK - HALO
        if lo >= 0:
            out[i * EXT:(i + 1) * EXT] = xb[lo:lo + EXT]
        else:
            out[i * EXT + HALO:(i + 1) * EXT] = xb[0:BLK]
    return out


def wa_cols(w_in_l):
    sl = lambda o, w: w_in_l[:, o:o + w]
    z = np.zeros((w_in_l.shape[0], 64), w_in_l.dtype)
    qs = []
    for h in range(8):
        qs += [sl(O_Q + 64 * h, 64), z]
    return np.ascontiguousarray(np.concatenate(
        qs + [sl(O_KC, 128), sl(O_VC, 128), sl(O_KS, 128), sl(O_KW, 128), sl(O_VS, 128), sl(O_VW, 128),
              sl(O_NG, 24)], axis=1))


def s1a_inputs(x, positions, L, W, S, consts):
    maps = []
    wa = wa_cols(W["w_in"][L])
    for core in range(8):
        b, blocks = core_block_ids(S, core)
        xin = make_xin(x[b], blocks)
        pos = make_xin(positions[b][:, None].astype(np.int32), blocks).reshape(1, -1)
        maps.append(dict(
            xin=xin, pos=np.ascontiguousarray(pos), wa=wa,
            kposT=np.ascontiguousarray(W["cmp_k_pos"][L].T), kw1=W["cmp_k_w1"][L], kw2=W["cmp_k_w2"][L],
            vposT=np.ascontiguousarray(W["cmp_v_pos"][L].T), vw1=W["cmp_v_w1"][L], vw2=W["cmp_v_w2"][L],
            ident=consts["ident"], perm=consts["perm"], invf=consts["invf"], sgn=consts["sgn"]))
    return maps


DBG_K = -1


def build_s3(NB, S, stop=99):
    c = Ctx()
    T = NB * BLK
    NSLOT = S // 16
    NBLKT = S // 64
    NKT = S // 128
    NCHT = max(1, NSLOT // 128)
    QT = c.din("QT", [8, 64, T], BF16)
    gates = c.din("gates", [T, 24])
    KAd = c.din("KA", [2, 128, S], BF16)
    VsAd = c.din("VsA", [2, S, 65], BF16)
    KwBd = c.din("KwB", [NB, 2, 128, 640], BF16)
    VwBd = c.din("VwB", [NB, 2, 640, 65], BF16)
    kcTd = c.din("kcT", [2, 128, NSLOT], BF16)
    vcAd = c.din("vcA", [NSLOT, 128], BF16)
    tqd = c.din("tq", [128, NB]); curd = c.din("cur", [128, NB]); curm1d = c.din("curm1", [128, NB])
    sendd = c.din("slot_end", [128, NSLOT]); blkidxd = c.din("blkidx", [128, NBLKT])
    sdmd = c.din("sdm", [4, 128, 128], BF16); wmaskd = c.din("wmask", [2, 128, 128], BF16)
    identbd = c.din("identb", [128, 128], BF16)
    oT = c.dout("oT", [512, T], BF16)

    nc = c.nc
    def bank(nm):
        t = c.es.enter_context(nc.psum_tensor(nm, [128, 512], F32))
        return t, Buf(nm, psum=True)
    SC = [bank("sc0"), bank("sc1")]
    OC = bank("oc"); OS = bank("os"); OW = bank("ow")
    ST = [bank("st0"), bank("st1"), bank("st2")]

    bres = Buf()
    KA = [c.sb([128, S], BF16) for _ in range(2)]
    VS = [c.sb([128, NKT, 65], BF16) for _ in range(2)]
    for hk in range(2):
        for c0 in range(0, S, 2048):
            c.load(KA[hk][:, c0:c0 + 2048], KAd[hk, :, c0:c0 + 2048], [bres])
        vv = VsAd[hk].rearrange("(n p) f -> p n f", p=128)
        for n0 in range(0, NKT, 32):
            n1 = min(NKT, n0 + 32)
            c.load(VS[hk][:, n0:n1, :], vv[:, n0:n1, :], [bres])
    kcT = [c.sb([128, NSLOT], BF16) for _ in range(2)]
    for hk in range(2):
        c.load(kcT[hk][:, :], kcTd[hk], [bres])
    vcS = c.sb([128, NCHT, 128], BF16)
    c.load(vcS[:, :, :], vcAd.rearrange("(n p) f -> p n f", p=128), [bres])
    tq = c.sb([128, NB]); cur = c.sb([128, NB]); curm1 = c.sb([128, NB])
    c.load(tq[:, :], tqd[:, :], [bres]); c.load(cur[:, :], curd[:, :], [bres]); c.load(curm1[:, :], curm1d[:, :], [bres])
    send = c.sb([128, NSLOT]); blkidx = c.sb([128, NBLKT])
    c.load(send[:, :], sendd[:, :], [bres]); c.load(blkidx[:, :], blkidxd[:, :], [bres])
    identb = c.sb([128, 128], BF16)
    c.load(identb[:, :], identbd[:, :], [bres])
    sdm = c.sb([128, 4, 4, 128], BF16)
    wmask = c.sb([128, 2, 4, 128], BF16)
    for r in range(4):
        for g in range(4):
            c.load(sdm[:, r, g, :], sdmd[r], [bres])
    for r in range(2):
        for g in range(4):
            c.load(wmask[:, r, g, :], wmaskd[r], [bres])

    NG_MAX = (NBLKT + 63) // 64
    QP = [c.sb([128, 4, 128], BF16) for _ in range(2)]; bQP = [Buf(), Buf()]
    QM = [[c.sb([128, 4, 128], BF16) for _ in range(NG_MAX)] for _ in range(2)]
    bQM = [[Buf() for _ in range(NG_MAX)] for _ in range(2)]
    for p in range(2):
        c.v("memset", [], [bQP[p]], eng="gpsimd", ap=QP[p][:, :, :], constant=0.0)
    em = [[c.sb([128, NSLOT], BF16) for _ in range(4)] for _ in range(2)]; bem = [[Buf() for _ in range(4)] for _ in range(2)]
    e32 = [c.sb([128, NSLOT]) for _ in range(2)]; be32 = [Buf(), Buf()]
    rs = [c.sb([128, 4]) for _ in range(2)]; rinv = [c.sb([128, 4]) for _ in range(2)]; brs = [Buf(), Buf()]
    mx = c.sb([128, 4]); nmx = c.sb([128, 4]); bmx = Buf()
    Pacc = c.sb([128, NSLOT]); bPacc = Buf()
    valid = [c.sb([128, NSLOT]) for _ in range(2)]; bvalid = [Buf(), Buf()]
    m1 = [c.sb([128, NBLKT]) for _ in range(2)]; m2 = [c.sb([128, NBLKT]) for _ in range(2)]
    le = [c.sb([128, NBLKT]) for _ in range(2)]; bblk = [Buf(), Buf()]
    imp = c.sb([128, NBLKT]); tmpi = c.sb([128, NBLKT]); work = c.sb([128, NBLKT]); bimp = Buf()
    v8a = c.sb([128, 8]); v8b = c.sb([128, 8])
    MN = [c.sb([128, 64 + NBLKT + 64], BF16) for _ in range(2)]; bMN = [Buf(), Buf()]
    for p in range(2):
        c.v("memset", [], [bMN[p]], eng="gpsimd", ap=MN[p][:, :], constant=NEGM)
    emT = [c.sb([128, 128], BF16) for _ in range(3)]; bemT = [Buf() for _ in range(3)]
    PT = [c.sb([128, 512], BF16) for _ in range(3)]; bPT = [Buf() for _ in range(3)]
    KwB = [c.sb([128, 640], BF16) for _ in range(2)]; VwB = [c.sb([128, 5, 65], BF16) for _ in range(2)]
    bKw = [Buf(), Buf()]
    gt = [c.sb([128, 24]) for _ in range(2)]; bgt = [Buf(), Buf()]
    oacc = [c.sb([128, 8, 64]) for _ in range(2)]; boacc = [Buf(), Buf()]
    ob = c.sb([128, 512], BF16); bob = Buf()
    oTt = [c.sb([128, 4, 128], BF16) for _ in range(2)]; boTt = [Buf(), Buf()]
    coef = c.sb([128, 3, 4]); den = c.sb([128, 2, 4]); bcoef = Buf()
    cnt = {"st": 0, "emT": 0}

    def dims(j):
        NSj = min(NSLOT, 128 * ((j + 1 + 3) // 4))
        NBj = NSj // 4
        NGj = (NBj + 63) // 64
        return NSj, NBj, NGj

    def phase1a(k):
        j, hk = divmod(k, 2)
        p = k % 2
        jp = j % 2
        NSj, NBj, NGj = dims(j)
        qsrc = QT[hk * 4:(hk + 1) * 4, :, j * 128:(j + 1) * 128].rearrange("g d t -> d g t")
        c.load(QP[p][0:64, :, :], qsrc, [bQP[p]])
        for G in range(NGj):
            c.load(QM[p][G][0:64, :, :], qsrc, [bQM[p][G]])
        if hk == 0:
            c.load(gt[jp][:, :], gates[j * 128:(j + 1) * 128, :], [bgt[jp]])
            c.v("tensor_scalar", [bres], [bvalid[jp]], out=valid[jp][:, 0:NSj], in0=send[:, 0:NSj], scalar1=tq[:, j:j + 1],
                scalar2=NEGM, op0=ALU.is_gt, op1=ALU.mult)
            c.v("tensor_scalar", [bres], [bblk[jp]], out=m1[jp][:, 0:NBj], in0=blkidx[:, 0:NBj], scalar1=cur[:, j:j + 1],
                scalar2=1e4, op0=ALU.is_equal, op1=ALU.mult)
            c.v("tensor_scalar", [bres], [bblk[jp]], out=m2[jp][:, 0:NBj], in0=blkidx[:, 0:NBj], scalar1=curm1[:, j:j + 1],
                scalar2=1e4, op0=ALU.is_equal, op1=ALU.mult)
            c.v("tensor_tensor", [bblk[jp]], [bblk[jp]], out=m1[jp][:, 0:NBj], in0=m1[jp][:, 0:NBj], in1=m2[jp][:, 0:NBj],
                op=ALU.max)
            c.v("memset", [], [bblk[jp]], ap=m1[jp][:, 0:1], constant=1e4)
            c.v("tensor_scalar", [bres], [bblk[jp]], out=le[jp][:, 0:NBj], in0=blkidx[:, 0:NBj], scalar1=cur[:, j:j + 1],
                scalar2=None, op0=ALU.is_le)
        nch = (NSj + 511) // 512
        for g in range(4):
            for ci in range(nch):
                c0 = ci * 512; w = min(512, NSj - c0)
                c.mm(SC[ci][0][:, 0:w], QP[p][:, g, :], kcT[hk][:, c0:c0 + w], True, True, [bQP[p], bres], [SC[ci][1]])
                c.v("tensor_reduce", [SC[ci][1]], [bmx], out=mx[:, ci:ci + 1], in_=SC[ci][0][:, 0:w], axis=AX.X, op=ALU.max)
            if nch == 2:
                c.v("tensor_tensor", [bmx], [bmx], out=mx[:, 0:1], in0=mx[:, 0:1], in1=mx[:, 1:2], op=ALU.max)
            c.v("tensor_scalar", [bmx], [bmx], out=nmx[:, g:g + 1], in0=mx[:, 0:1], scalar1=-1.0, scalar2=None, op0=ALU.mult)
            for ci in range(nch):
                c0 = ci * 512; w = min(512, NSj - c0)
                c.v("tensor_tensor", [SC[ci][1], bvalid[jp]], [be32[g % 2]], out=e32[g % 2][:, c0:c0 + w],
                    in0=SC[ci][0][:, 0:w], in1=valid[jp][:, c0:c0 + w], op=ALU.add)
            c.act(em[p][g][:, 0:NSj], e32[g % 2][:, 0:NSj], AF.Exp, [be32[g % 2], bmx], [bem[p][g], brs[p]],
                  bias=nmx[:, g:g + 1], accum_out=rs[p][:, g:g + 1])
        c.v("tensor_scalar", [brs[p]], [brs[p]], out=rs[p][:, :], in0=rs[p][:, :], scalar1=1e-30, scalar2=None, op0=ALU.max)
        c.v("reciprocal", [brs[p]], [brs[p]], out=rinv[p][:, :], in_=rs[p][:, :])
        c.v("tensor_scalar", [bem[p][0], brs[p]], [bPacc], out=Pacc[:, 0:NSj], in0=em[p][0][:, 0:NSj],
            scalar1=rinv[p][:, 0:1], scalar2=None, op0=ALU.mult)
        for g in range(1, 4):
            c.v("scalar_tensor_tensor", [bem[p][g], brs[p], bPacc], [bPacc], out=Pacc[:, 0:NSj], in0=em[p][g][:, 0:NSj],
                scalar=rinv[p][:, g:g + 1], in1=Pacc[:, 0:NSj], op0=ALU.mult, op1=ALU.add)
        P4 = Pacc[:, 0:NSj].rearrange("p (b f) -> p b f", f=4)
        I = imp[:, 0:NBj]; Tm = tmpi[:, 0:NBj]
        c.v("tensor_tensor", [bPacc], [bimp], out=Tm, in0=P4[:, :, 1], in1=P4[:, :, 2], op=ALU.add)
        c.v("tensor_tensor", [bPacc, bimp], [bimp], out=Tm, in0=Tm, in1=P4[:, :, 3], op=ALU.add)
        c.v("scalar_tensor_tensor", [bPacc, bimp], [bimp], out=I, in0=Tm, scalar=2.0, in1=P4[:, :, 0], op0=ALU.mult,
            op1=ALU.add)
        if NBj > 1:
            c.v("tensor_tensor", [bPacc, bimp], [bimp], out=imp[:, 0:NBj - 1], in0=imp[:, 0:NBj - 1], in1=P4[:, 1:NBj, 0],
                op=ALU.add)
        c.v("tensor_tensor", [bimp, bblk[jp]], [bimp], out=I, in0=I, in1=m1[jp][:, 0:NBj], op=ALU.max)
        c.v("scalar_tensor_tensor", [bimp, bblk[jp]], [bimp], out=I, in0=I, scalar=1.0, in1=le[jp][:, 0:NBj], op0=ALU.add,
            op1=ALU.mult)
        c.v("tensor_scalar", [bimp], [bimp], out=I, in0=I, scalar1=-1.0, scalar2=None, op0=ALU.add)
        c.v("max", [bimp], [bimp], out=v8a[:, :], in_=I)
        c.v("match_replace", [bimp], [bimp], out=work[:, 0:NBj], in_to_replace=v8a[:, :], in_values=I, imm_value=-2.0)
        c.v("max", [bimp], [bimp], out=v8b[:, :], in_=work[:, 0:NBj])
        c.v("tensor_scalar", [bimp], [bimp], out=Tm, in0=I, scalar1=v8b[:, 7:8], scalar2=None, op0=ALU.is_ge)
        c.v("tensor_tensor", [bimp, bblk[jp]], [bimp], out=Tm, in0=Tm, in1=le[jp][:, 0:NBj], op=ALU.mult)
        c.v("tensor_scalar", [bimp], [bMN[p]], out=MN[p][:, 64:64 + NBj], in0=Tm, scalar1=-1.0, scalar2=-NEGM, op0=ALU.add,
            op1=ALU.mult)

    def tr_bank(i):
        t, b = SC[i % 2]
        return t[:, 0:64].bitcast(BF16), b

    def phase1b(k):
        j, hk = divmod(k, 2)
        p = k % 2
        NSj, NBj, NGj = dims(j)
        for G in range(NGj):
            tv, tb = tr_bank(G)
            c.tr(tv, MN[p][:, 64 * G:64 * G + 128], identb[:, :], [bMN[p], bres], [tb])
            for g in range(4):
                if g % 2 == 0:
                    c.v("tensor_copy", [tb], [bQM[p][G]], out=QM[p][G][64:128, g, :], in_=tv[64:128, :])
                else:
                    c.act(QM[p][G][64:128, g, :], tv[64:128, :], AF.Copy, [tb], [bQM[p][G]])
        first = True
        for g in range(4):
            for ch in range(NSj // 128):
                i = cnt["emT"]; cnt["emT"] += 1
                tv, tb = tr_bank(i)
                c.tr(tv, em[p][g][:, ch * 128:(ch + 1) * 128], identb[:, :], [bem[p][g], bres], [tb])
                et = emT[i % 3]; bet = bemT[i % 3]
                if i % 2 == 0:
                    c.v("tensor_copy", [tb], [bet], out=et[:, :], in_=tv)
                else:
                    c.act(et[:, :], tv, AF.Copy, [tb], [bet])
                last = (g == 3 and ch == NSj // 128 - 1)
                c.S.op("tensor", (lambda e, et=et, g=g, ch=ch, first=first, last=last: e.matmul(
                    OC[0][:, g * 64:(g + 1) * 64], lhsT=et[:, :], rhs=vcS[:, ch, hk * 64:(hk + 1) * 64], start=first,
                    stop=last, skip_group_check=True)), [bet, bres], [OC[1]])
                first = False

    def attend(kT_of, v_of, ntiles, qrhs_of, mask_of, Obank, reads_k):
        first = True
        for i in range(ntiles):
            si = cnt["st"]; cnt["st"] += 1
            st, stb = ST[si % 3]
            msk = mask_of(i)
            rhs, brhs = qrhs_of(i)
            c.mm(st[:, 0:512], kT_of(i), rhs, True, msk is None, reads_k + [brhs], [stb])
            if msk is not None:
                c.mm(st[:, 0:512], identb[:, :], msk, False, True, [bres], [stb])
            pt = PT[si % 3]; bpt = bPT[si % 3]
            c.act(pt[:, :], st[:, 0:512], AF.Exp, [stb], [bpt])
            for g in range(4):
                last = (i == ntiles - 1 and g == 3)
                c.S.op("tensor", (lambda e, pt=pt, g=g, i=i, first=first, last=last: e.matmul(
                    Obank[0][:, g * 65:(g + 1) * 65], lhsT=pt[:, g * 128:(g + 1) * 128], rhs=v_of(i), start=first,
                    stop=last, skip_group_check=True)), [bpt] + reads_k, [Obank[1]])
                first = False

    def phase2(k):
        j, hk = divmod(k, 2)
        p = k % 2
        jp = j % 2
        NSj, NBj, NGj = dims(j)
        NTj = 4 * j + 4
        flat = lambda t: t[:, :, :].rearrange("p g q -> p (g q)")
        attend(lambda i: KA[hk][:, i * 128:(i + 1) * 128], lambda i: VS[hk][:, i, :], NTj,
               lambda i: (flat(QM[p][i // 32]), bQM[p][i // 32]),
               lambda i: (flat4(sdm, i - 4 * j) if i >= 4 * j else None), OS, [bres])
        c.load(KwB[p][:, :], KwBd[j, hk], [bKw[p]])
        c.load(VwB[p][:, :, :], VwBd[j, hk].rearrange("(n p) f -> p n f", p=128), [bKw[p]])
        attend(lambda i: KwB[p][:, i * 128:(i + 1) * 128], lambda i: VwB[p][:, i, :], 5,
               lambda i: (flat(QP[p]), bQP[p]),
               lambda i: (flat4(wmask, 0) if i == 0 else (flat4(wmask, 1) if i == 4 else None)), OW, [bKw[p]])
        if DBG_K == k:
            dbgt = c.sb([128, 1024]); bdbg = Buf()
            dbg = c.dout("dbg", [128, 1024])
            c.v("tensor_copy", [OC[1]], [bdbg], out=dbgt[:, 0:256], in_=OC[0][:, 0:256])
            c.v("tensor_copy", [OS[1]], [bdbg], out=dbgt[:, 256:516], in_=OS[0][:, 0:260])
            c.v("tensor_copy", [OW[1]], [bdbg], out=dbgt[:, 516:776], in_=OW[0][:, 0:260])
            c.v("tensor_copy", [brs[p]], [bdbg], out=dbgt[:, 776:780], in_=rinv[p][:, :])
            c.v("tensor_copy", [bMN[p]], [bdbg], out=dbgt[:, 780:780 + NBj], in_=MN[p][:, 64:64 + NBj])
            c.v("tensor_copy", [bQM[p][0]], [bdbg], out=dbgt[:, 900:1024], in_=QM[p][0][:, 0, 0:124])
            c.store(dbg[:, :], dbgt[:, :], [bdbg])
        g3 = gt[jp][:, hk * 12:(hk + 1) * 12].rearrange("p (g b) -> p g b", b=3)
        c.v("tensor_tensor", [brs[p], bgt[jp]], [bcoef], out=coef[:, 0, :], in0=rinv[p][:, :], in1=g3[:, :, 0], op=ALU.mult)
        for bi, Ob in ((1, OS), (2, OW)):
            dv = Ob[0][:, 0:260].rearrange("p (g f) -> p g f", f=65)[:, :, 64]
            c.v("tensor_copy", [Ob[1]], [bcoef], out=den[:, bi - 1, :], in_=dv)
            c.v("reciprocal", [bcoef], [bcoef], out=den[:, bi - 1, :], in_=den[:, bi - 1, :])
            c.v("tensor_tensor", [bcoef, bgt[jp]], [bcoef], out=coef[:, bi, :], in0=den[:, bi - 1, :], in1=g3[:, :, bi],
                op=ALU.mult)
        for g in range(4):
            dst = oacc[jp][:, hk * 4 + g, :]
            c.v("tensor_scalar", [OC[1], bcoef], [boacc[jp]], out=dst, in0=OC[0][:, g * 64:(g + 1) * 64],
                scalar1=coef[:, 0, g:g + 1], scalar2=None, op0=ALU.mult)
            c.v("scalar_tensor_tensor", [OS[1], bcoef, boacc[jp]], [boacc[jp]], out=dst, in0=OS[0][:, g * 65:g * 65 + 64],
                scalar=coef[:, 1, g:g + 1], in1=dst, op0=ALU.mult, op1=ALU.add)
            c.v("scalar_tensor_tensor", [OW[1], bcoef, boacc[jp]], [boacc[jp]], out=dst, in0=OW[0][:, g * 65:g * 65 + 64],
                scalar=coef[:, 2, g:g + 1], in1=dst, op0=ALU.mult, op1=ALU.add)
        if hk == 1:
            c.act(ob[:, :], oacc[jp][:, :, :].rearrange("p h d -> p (h d)"), AF.Copy, [boacc[jp]], [bob])
            for ch in range(4):
                tv, tb = tr_bank(ch)
                c.tr(tv, ob[:, ch * 128:(ch + 1) * 128], identb[:, :], [bob, bres], [tb])
                c.v("tensor_copy", [tb], [boTt[jp]], out=oTt[jp][:, ch, :], in_=tv)
            c.store(oT[:, j * 128:(j + 1) * 128].rearrange("(c p) t -> p c t", p=128), oTt[jp][:, :, :], [boTt[jp]])

    def flat4(t, r):
        return t[:, r, :, :].rearrange("p g q -> p (g q)")

    NK = NB * 2
    if stop <= 1:
        return c.finish()
    phase1a(0)
    if stop <= 2:
        return c.finish()
    phase1b(0)
    if stop <= 3:
        return c.finish()
    if stop <= 4:
        phase2(0)
        return c.finish()
    for k in range(NK):
        if stop >= 10 and k >= stop - 10:
            break
        if k + 1 < NK:
            phase1a(k + 1)
        phase2(k)
        if k + 1 < NK:
            phase1b(k + 1)
    return c.finish()


def s3_inputs(s1a_res, S):
    nblk = S // BLK
    NB = nblk // 4
    NSLOT = S // 16
    full = {}
    for b in range(2):
        Ks = np.zeros((128, S), NPBF); Kw = np.zeros((128, S), NPBF)
        Vs_ = np.zeros((S, 128), NPBF); Vw_ = np.zeros((S, 128), NPBF)
        kc = np.zeros((2, 64, NSLOT), NPBF); vc = np.zeros((NSLOT, 128), NPBF)
        for cp in range(4):
            core = b * 4 + cp
            _, blocks = core_block_ids(S, core)
            r = s1a_res[core]
            for i, n in enumerate(blocks):
                ts = slice(n * 128, (n + 1) * 128); ls = slice(i * 128, (i + 1) * 128)
                Ks[:, ts] = r["KsT"][:, ls]; Kw[:, ts] = r["KwT"][:, ls]
                Vs_[ts] = r["Vs"][ls]; Vw_[ts] = r["Vw"][ls]
                kc[:, :, n * 8:(n + 1) * 8] = r["kcmpT"][:, :, i * 8:(i + 1) * 8]
                vc[n * 8:(n + 1) * 8] = r["vcmp"][i * 8:(i + 1) * 8]
        full[b] = (Ks, Kw, Vs_, Vw_, kc, vc)
    E = np.zeros((64, S), NPBF)
    keyblk = (np.arange(S) // 64) % 64
    E[keyblk, np.arange(S)] = 1
    slot_end = (16 * np.arange(NSLOT) + 15).astype(np.float32); slot_end[0] = 1e9
    slot_end = np.ascontiguousarray(np.broadcast_to(slot_end, (128, NSLOT)))
    blkidx = np.ascontiguousarray(np.broadcast_to(np.arange(S // 64, dtype=np.float32), (128, S // 64)))
    kk = np.arange(128)[:, None]; qq = np.arange(128)[None, :]
    tri = np.where(kk > qq, NEGM, 0.0).astype(NPBF)
    wm0 = np.where(kk <= qq, NEGM, 0.0).astype(NPBF)
    wmask = np.stack([wm0, tri])
    identb = np.eye(128, dtype=np.float32).astype(NPBF)
    maps = []
    for core in range(8):
        b, blocks = core_block_ids(S, core)
        cp = core % 4
        Ks, Kw, Vs_, Vw_, kc, vc = full[b]
        KA = np.zeros((2, 128, S), NPBF); VsA = np.zeros((2, S, 65), NPBF)
        for hk in range(2):
            KA[hk, 0:64] = Ks[hk * 64:(hk + 1) * 64]; KA[hk, 64:128] = E
            VsA[hk, :, 0:64] = Vs_[:, hk * 64:(hk + 1) * 64]; VsA[hk, :, 64] = 1
        KwB = np.zeros((NB, 2, 128, 640), NPBF); VwB = np.zeros((NB, 2, 640, 65), NPBF)
        for i, n in enumerate(blocks):
            lo = n * 128 - 512
            s0 = max(lo, 0)
            for hk in range(2):
                KwB[i, hk, 0:64, s0 - lo:] = Kw[hk * 64:(hk + 1) * 64, s0:n * 128 + 128]
                VwB[i, hk, s0 - lo:, 0:64] = Vw_[s0:n * 128 + 128, hk * 64:(hk + 1) * 64]
                VwB[i, hk, s0 - lo:, 64] = 1
        kcT = np.zeros((2, 128, NSLOT), NPBF); kcT[:, 0:64] = kc
        t = (np.array(blocks)[None, :] * 128 + np.arange(128)[:, None]).astype(np.float32)
        sdm = np.zeros((4, 128, 128), NPBF); sdm[cp] = tri
        maps.append(dict(QT=s1a_res[core]["QT"], gates=s1a_res[core]["gates"], KA=KA, VsA=VsA, KwB=KwB, VwB=VwB,
                         kcT=kcT, vcA=vc, tq=t, cur=np.floor(t / 64).astype(np.float32),
                         curm1=(np.floor(t / 64) - 1).astype(np.float32), slot_end=slot_end, blkidx=blkidx,
                         sdm=sdm, wmask=wmask, identb=identb))
    return maps


def bcast_load(c, dram_vec, n, buf):
    t = c.sb([128, n])
    c.load(t[:, :], dram_vec[0:1, :].partition_broadcast(128), [buf])
    return t


class LNorm:
    def __init__(self, c, width):
        self.c = c; self.w = width; self.nch = width // 512
        self.stats = c.sb([128, self.nch, 6]); self.mv = c.sb([128, 2]); self.sd = c.sb([128, 1]); self.b = Buf()

    def __call__(self, z, bz, gbc, bbc, bgb, out, bout, eps=1e-5):
        c = self.c
        for i in range(self.nch):
            c.v("bn_stats", [bz], [self.b], out=self.stats[:, i, :], in_=z[:, i * 512:(i + 1) * 512])
        c.v("bn_aggr", [self.b], [self.b], out=self.mv[:, :], in_=self.stats[:, :, :].rearrange("p a b -> p (a b)"))
        c.v("tensor_scalar", [self.b], [self.b], out=self.sd[:, :], in0=self.mv[:, 1:2], scalar1=float(eps), scalar2=None,
            op0=ALU.add)
        c.act(self.sd[:, :], self.sd[:, :], AF.Sqrt, [self.b], [self.b])
        c.v("reciprocal", [self.b], [self.b], out=self.sd[:, :], in_=self.sd[:, :])
        c.v("tensor_scalar", [bz, self.b], [bz], out=z, in0=z, scalar1=self.mv[:, 0:1], scalar2=self.sd[:, 0:1],
            op0=ALU.subtract, op1=ALU.mult)
        c.v("tensor_tensor", [bz, bgb], [bz], eng="gpsimd", out=z, in0=z, in1=gbc, op=ALU.mult)
        c.v("tensor_tensor", [bz, bgb], [bout], out=out, in0=z, in1=bbc, op=ALU.add)


def build_s4a1(NB):
    c = Ctx()
    T = NB * BLK
    TB = min(4, NB)
    NT = NB // TB
    NC = TB * 128
    xin = c.din("xin", [NB * EXT, D])
    wb1d = c.din("wb1", [D, 1536])
    poolwd = c.din("pool_w", [4, 128, 128]); pscd = c.din("pool_scale", [128, 4])
    lngd = c.din("sg_ln_g", [1, 512]); lnbd = c.din("sg_ln_b", [1, 512])
    sgwTd = c.din("sg_wT", [4, 128, 128]); sgbd = c.din("sg_b", [1, 512]); trild = c.din("trilT", [128, 128])
    wpod = c.din("w_pool_out", [512, D]); wsod = c.din("w_sg_out", [512, D])
    invcd = c.din("invc", [128, 64]); identd = c.din("ident", [128, 128])
    ypo = c.dout("ypo", [D, T], BF16); yso = c.dout("yso", [D, T], BF16)
    c.init_psum()
    bw = Buf()
    ident = c.sb([128, 128]); bid = Buf()
    c.load(ident[:, :], identd[:, :], [bid])
    stage = [(c.sb([128, 2048]), Buf()) for _ in range(2)]
    wb1 = c.sb([128, 8, 1536], BF16)
    for k in range(8):
        c.load_cast(wb1[:, k, :], bw, wb1d[k * 128:(k + 1) * 128, :], stage)
    pwb = c.sb([128, 4, 128], BF16)
    c.load_cast(pwb[:, :, :], bw, poolwd.rearrange("g c d -> c g d"), stage)
    psc = c.sb([128, 4]); c.load(psc[:, :], pscd[:, :], [bw])
    wpo = c.sb([128, 4, D], BF16); wso = c.sb([128, 4, D], BF16)
    for g in range(4):
        c.load_cast(wpo[:, g, :], bw, wpod[g * 128:(g + 1) * 128, :], stage)
        c.load_cast(wso[:, g, :], bw, wsod[g * 128:(g + 1) * 128, :], stage)
    lng = bcast_load(c, lngd, 512, bw); lnb = bcast_load(c, lnbd, 512, bw); sgb = bcast_load(c, sgbd, 512, bw)
    tril = c.sb([128, 128]); c.load(tril[:, :], trild[:, :], [bw])
    wsf = c.sb([128, 4, 128]); c.load(wsf[:, :, :], sgwTd.rearrange("g s t -> s g t"), [bw])
    wsT = c.sb([128, 4, 128], BF16)
    for g in range(4):
        c.v("tensor_tensor", [bw], [bw], out=wsT[:, g, :], in0=wsf[:, g, :], in1=tril[:, :], op=ALU.mult)
    invc = c.sb([128, 4, 16]); c.load(invc[:, :, :], invcd.rearrange("p (g t) -> p g t", t=16), [bw])

    xt = [c.sb([128, TB, D]) for _ in range(2)]; bxt = [Buf(), Buf()]
    xh = [c.sb([128, D]) for _ in range(2)]; bxh = [Buf(), Buf()]
    for i in range(2):
        c.v("memset", [], [bxh[i]], eng="gpsimd", ap=xh[i][:, :], constant=0.0)
    xT = [c.sb([128, 8, NC], BF16) for _ in range(2)]; bxT = [Buf(), Buf()]
    xTh = [c.sb([128, 8, 128], BF16) for _ in range(2)]; bxTh = [Buf(), Buf()]
    aext = [c.sb([128, TB, EXT]) for _ in range(4)]; baext = [Buf() for _ in range(4)]
    B1 = c.sb([128, TB, EXT]); B2 = c.sb([128, TB, EXT]); bB1 = Buf(); bB2 = Buf()
    dT = c.sb([128, 4, NC], BF16); bdT = Buf()
    ypT = c.sb([128, 4, NC], BF16); bypT = Buf()
    uT = c.sb([128, 4, NC]); buT = Buf()
    gv = [c.sb([128, 512]) for _ in range(2)]; bgv = [Buf(), Buf()]
    vnb = [c.sb([128, 512], BF16) for _ in range(2)]; bvnb = [Buf(), Buf()]
    mtmp = c.sb([128, 512]); bmtmp = Buf()
    sguT = c.sb([128, 4, NC], BF16); bsgu = Buf()
    outb = [c.sb([128, 8, NC], BF16) for _ in range(2)]; boutb = [Buf(), Buf()]
    fix = c.sb([128, 16]); bfix = Buf()
    ln = LNorm(c, 512)
    v3 = lambda ap: ap.rearrange("p (n e) -> p n e", n=TB)

    for ti in range(NT):
        p = ti % 2
        blk0 = ti * TB
        load_x_tile(c, xin, blk0, TB, xt[p], bxt[p], xh[p], bxh[p])
        transpose_x(c, xt[p], bxt[p], TB, xT[p], bxT[p], ident, bid, xh[p], bxh[p], xTh[p], bxTh[p])
        for g in range(4):
            pt, pb = c.ps()
            for k in range(8):
                c.mm(pt[:, 0:NC], wb1[:, k, g * 128:(g + 1) * 128], xT[p][:, k, :], k == 0, k == 7, [bw, bxT[p]], [pb])
            c.act(aext[g][:, :, HALO:EXT], v3(pt[:, 0:NC]), AF.Copy, [pb], [baext[g]])
            pt, pb = c.ps()
            for k in range(8):
                c.mm(pt[:, 0:16 * TB], wb1[:, k, g * 128:(g + 1) * 128], xTh[p][:, k, 0:16 * TB], k == 0, k == 7,
                     [bw, bxTh[p]], [pb])
            c.v("tensor_copy", [pb], [baext[g]], out=aext[g][:, :, 0:HALO], in_=v3(pt[:, 0:16 * TB]))
            A = aext[g]
            src, bsrc = A, baext[g]
            sh = 1
            for step in range(g + 1):
                dst, bdst = (B1, bB1) if step % 2 == 0 else (B2, bB2)
                lo = 2 * sh - 1
                c.v("tensor_tensor", [bsrc], [bdst], eng="gpsimd", out=dst[:, :, lo:EXT], in0=src[:, :, lo:EXT],
                    in1=src[:, :, lo - sh:EXT - sh], op=ALU.add)
                src, bsrc = dst, bdst
                sh *= 2
            w = 2 ** (g + 1)
            c.v("scalar_tensor_tensor", [bsrc, baext[g]], [bdT], out=v3(dT[:, g, :]), in0=src[:, :, HALO:EXT],
                scalar=1.0 / w, in1=A[:, :, HALO:EXT], op0=ALU.mult, op1=ALU.subtract)
            if ti == 0:
                c.v("tensor_tensor", [bsrc, bw], [bfix], out=fix[:, :], in0=src[:, 0, HALO:HALO + 16], in1=invc[:, g, :],
                    op=ALU.mult)
                c.v("tensor_tensor", [bfix, baext[g]], [bdT], out=dT[:, g, 0:16], in0=fix[:, :], in1=A[:, 0, HALO:HALO + 16],
                    op=ALU.subtract)
            pt, pb = c.ps()
            c.mm(pt[:, 0:NC], pwb[:, g, :], dT[:, g, :], True, True, [bw, bdT], [pb])
            c.act(ypT[:, g, :], pt[:, 0:NC], AF.Copy, [pb, bw], [bypT], scale=psc[:, g:g + 1])
        for dc in range(8):
            pt, pb = c.ps()
            for g in range(4):
                c.mm(pt[:, 0:NC], wpo[:, g, dc * 128:(dc + 1) * 128], ypT[:, g, :], g == 0, g == 3, [bw, bypT], [pb])
            if dc % 2 == 0:
                c.v("tensor_copy", [pb], [boutb[0]], out=outb[0][:, dc, :], in_=pt[:, 0:NC])
            else:
                c.act(outb[0][:, dc, :], pt[:, 0:NC], AF.Copy, [pb], [boutb[0]])
        c.store(ypo[:, blk0 * 128:blk0 * 128 + NC].rearrange("(c p) t -> p c t", p=128), outb[0][:, :, :], [boutb[0]])
        for g in range(4):
            pt, pb = c.ps()
            for k in range(8):
                c.mm(pt[:, 0:NC], wb1[:, k, 512 + g * 128:512 + (g + 1) * 128], xT[p][:, k, :], k == 0, k == 7,
                     [bw, bxT[p]], [pb])
            c.act(uT[:, g, :], pt[:, 0:NC], AF.Gelu_apprx_tanh, [pb], [buT])
        for b in range(TB):
            q = b % 2
            pt, pb = c.ps()
            for k in range(8):
                c.mm(pt[:, 0:512], xT[p][:, k, b * 128:(b + 1) * 128], wb1[:, k, 1024:1536], k == 0, k == 7,
                     [bw, bxT[p]], [pb])
            c.act(gv[q][:, :], pt[:, 0:512], AF.Gelu_apprx_tanh, [pb], [bgv[q]])
            ln(gv[q][:, :], bgv[q], lng[:, :], lnb[:, :], bw, vnb[q][:, :], bvnb[q])
            pt, pb = c.ps()
            for g in range(4):
                c.mm(pt[:, g * 128:(g + 1) * 128], vnb[q][:, g * 128:(g + 1) * 128], wsT[:, g, :], True, True,
                     [bvnb[q], bw], [pb])
            c.v("tensor_tensor", [pb, bw], [bmtmp], out=mtmp[:, :], in0=pt[:, 0:512], in1=sgb[:, :], op=ALU.add)
            c.v("tensor_tensor", [bmtmp, buT], [bsgu], out=sguT[:, :, b * 128:(b + 1) * 128],
                in0=mtmp[:, :].rearrange("p (g t) -> p g t", g=4), in1=uT[:, :, b * 128:(b + 1) * 128], op=ALU.mult)
        for dc in range(8):
            pt, pb = c.ps()
            for g in range(4):
                c.mm(pt[:, 0:NC], wso[:, g, dc * 128:(dc + 1) * 128], sguT[:, g, :], g == 0, g == 3, [bw, bsgu], [pb])
            if dc % 2 == 0:
                c.v("tensor_copy", [pb], [boutb[1]], out=outb[1][:, dc, :], in_=pt[:, 0:NC])
            else:
                c.act(outb[1][:, dc, :], pt[:, 0:NC], AF.Copy, [pb], [boutb[1]])
        c.store(yso[:, blk0 * 128:blk0 * 128 + NC].rearrange("(c p) t -> p c t", p=128), outb[1][:, :, :], [boutb[1]])
    return c.finish()


def build_s4a2(NB, moe):
    c = Ctx()
    T = NB * BLK
    TB = min(2, NB)
    NT = NB // TB
    NC = TB * 128
    xin = c.din("xin", [NB * EXT, D])
    oTd = c.din("oT", [512, T], BF16); ypod = c.din("ypo", [D, T], BF16); ysod = c.din("yso", [D, T], BF16)
    wbgd = c.din("wbg", [D, 3072]); wnod = c.din("w_nsa_out", [512, D]); woutd = c.din("w_out", [D, D])
    g1d = c.din("ln1_g", [1, D]); b1d = c.din("ln1_b", [1, D]); identd = c.din("ident", [128, 128])
    x1o = c.dout("x1", [T, D]); x1To = c.dout("x1T", [D, T], BF16)
    if moe:
        wrd = c.din("w_router", [D, 8]); brd = c.din("b_router", [1, 8])
        rwo = c.dout("rw", [T, 8])
    c.init_psum()
    bw = Buf()
    ident = c.sb([128, 128]); bid = Buf()
    c.load(ident[:, :], identd[:, :], [bid])
    stage = [(c.sb([128, 2048]), Buf()) for _ in range(2)]
    wbg = c.sb([128, 8, 3072], BF16)
    for k in range(8):
        for h0 in range(0, 3072, 1536):
            c.load_cast(wbg[:, k, h0:h0 + 1536], bw, wbgd[k * 128:(k + 1) * 128, h0:h0 + 1536], stage)
    wno = c.sb([128, 4, D], BF16)
    for g in range(4):
        c.load_cast(wno[:, g, :], bw, wnod[g * 128:(g + 1) * 128, :], stage)
    wout = c.sb([128, 8, D], BF16)
    for k in range(8):
        c.load_cast(wout[:, k, :], bw, woutd[k * 128:(k + 1) * 128, :], stage)
    g1 = bcast_load(c, g1d, D, bw); b1 = bcast_load(c, b1d, D, bw)
    if moe:
        wr = c.sb([128, 8, 8]); c.load(wr[:, :, :], wrd.rearrange("(k p) e -> p k e", p=128), [bw])
        brb = bcast_load(c, brd, 8, bw)

    xt = [c.sb([128, TB, D]) for _ in range(2)]; bxt = [Buf(), Buf()]
    xT = [c.sb([128, 8, NC], BF16) for _ in range(2)]; bxT = [Buf(), Buf()]
    oTt = [c.sb([128, 4, NC], BF16) for _ in range(2)]; ypt = [c.sb([128, 8, NC], BF16) for _ in range(2)]
    yst = [c.sb([128, 8, NC], BF16) for _ in range(2)]; bin_ = [Buf(), Buf()]
    gsb = [c.sb([128, NC]) for _ in range(3)]; bgsb = [Buf() for _ in range(3)]
    acc = c.sb([128, NC]); bacc = Buf()
    mT = c.sb([128, 8, NC], BF16); bmT = Buf()
    z = [c.sb([128, D]) for _ in range(2)]; bz = [Buf(), Buf()]
    x1 = [c.sb([128, D]) for _ in range(2)]; bx1 = [Buf(), Buf()]
    x1Tb = [c.sb([128, 8, 128], BF16) for _ in range(2)]; bx1T = [Buf(), Buf()]
    x1Tf = c.sb([128, 8, 128]); bx1Tf = Buf()
    lg = c.sb([128, 8]); v8 = c.sb([128, 8]); dl = c.sb([128, 2]); rwt = c.sb([128, 8]); rw2 = c.sb([128, 8]); brw = Buf()
    ln = LNorm(c, D)

    for ti in range(NT):
        p = ti % 2
        blk0 = ti * TB
        cols = slice(blk0 * 128, blk0 * 128 + NC)
        load_x_tile(c, xin, blk0, TB, xt[p], bxt[p], None, None)
        transpose_x(c, xt[p], bxt[p], TB, xT[p], bxT[p], ident, bid)
        c.load(oTt[p][:, :, :], oTd[:, cols].rearrange("(c p) t -> p c t", p=128), [bin_[p]])
        c.load(ypt[p][:, :, :], ypod[:, cols].rearrange("(c p) t -> p c t", p=128), [bin_[p]])
        c.load(yst[p][:, :, :], ysod[:, cols].rearrange("(c p) t -> p c t", p=128), [bin_[p]])
        for dc in range(8):
            for br in range(3):
                pt, pb = c.ps()
                for k in range(8):
                    c.mm(pt[:, 0:NC], wbg[:, k, br * 1024 + dc * 128:br * 1024 + (dc + 1) * 128], xT[p][:, k, :], k == 0,
                         k == 7, [bw, bxT[p]], [pb])
                c.act(gsb[br][:, :], pt[:, 0:NC], AF.Sigmoid, [pb], [bgsb[br]])
            pn, pnb = c.ps()
            for g in range(4):
                c.mm(pn[:, 0:NC], wno[:, g, dc * 128:(dc + 1) * 128], oTt[p][:, g, :], g == 0, g == 3, [bw, bin_[p]], [pnb])
            c.v("tensor_tensor", [bgsb[0], bin_[p]], [bacc], out=acc[:, :], in0=gsb[0][:, :], in1=ypt[p][:, dc, :], op=ALU.mult)
            c.v("tensor_tensor", [bgsb[1], bin_[p]], [bgsb[1]], eng="gpsimd", out=gsb[1][:, :], in0=gsb[1][:, :],
                in1=yst[p][:, dc, :], op=ALU.mult)
            c.v("tensor_tensor", [bgsb[2], pnb], [bgsb[2]], out=gsb[2][:, :], in0=gsb[2][:, :], in1=pn[:, 0:NC], op=ALU.mult)
            c.v("tensor_tensor", [bacc, bgsb[1]], [bacc], eng="gpsimd", out=acc[:, :], in0=acc[:, :], in1=gsb[1][:, :],
                op=ALU.add)
            c.v("tensor_tensor", [bacc, bgsb[2]], [bmT], out=mT[:, dc, :], in0=acc[:, :], in1=gsb[2][:, :], op=ALU.add)
        for b in range(TB):
            q = b % 2
            for half in range(2):
                pt, pb = c.ps()
                for k in range(8):
                    c.mm(pt[:, 0:512], mT[:, k, b * 128:(b + 1) * 128], wout[:, k, half * 512:(half + 1) * 512], k == 0,
                         k == 7, [bw, bmT], [pb])
                c.v("scalar_tensor_tensor", [bxt[p], pb], [bz[q]], out=z[q][:, half * 512:(half + 1) * 512],
                    in0=xt[p][:, b, half * 512:(half + 1) * 512], scalar=float(ALPHA), in1=pt[:, 0:512], op0=ALU.mult,
                    op1=ALU.add)
            ln(z[q][:, :], bz[q], g1[:, :], b1[:, :], bw, x1[q][:, :], bx1[q])
            rows = slice((blk0 + b) * 128, (blk0 + b + 1) * 128)
            c.store(x1o[rows, :], x1[q][:, :], [bx1[q]])
            for k in range(8):
                pt, pb = c.ps()
                c.tr(pt[:, 0:128], x1[q][:, k * 128:(k + 1) * 128], ident[:, :], [bx1[q], bid], [pb])
                c.act(x1Tb[q][:, k, :], pt[:, 0:128], AF.Copy, [pb], [bx1T[q]])
                if moe:
                    c.v("tensor_copy", [pb], [bx1Tf], out=x1Tf[:, k, :], in_=pt[:, 0:128])
            c.store(x1To[:, rows].rearrange("(c p) t -> p c t", p=128), x1Tb[q][:, :, :], [bx1T[q]])
            if moe:
                pt, pb = c.ps()
                for k in range(8):
                    c.mm(pt[:, 0:8], x1Tf[:, k, :], wr[:, k, :], k == 0, k == 7, [bx1Tf, bw], [pb])
                c.v("tensor_tensor", [pb, bw], [brw], out=lg[:, :], in0=pt[:, 0:8], in1=brb[:, :], op=ALU.add)
                c.v("max", [brw], [brw], out=v8[:, :], in_=lg[:, :])
                c.v("tensor_tensor", [brw], [brw], out=dl[:, 0:1], in0=v8[:, 0:1], in1=v8[:, 1:2], op=ALU.subtract)
                c.v("tensor_tensor", [brw], [brw], out=dl[:, 1:2], in0=v8[:, 1:2], in1=v8[:, 0:1], op=ALU.subtract)
                c.act(dl[:, :], dl[:, :], AF.Sigmoid, [brw], [brw])
                c.v("tensor_scalar", [brw], [brw], out=rwt[:, :], in0=lg[:, :], scalar1=v8[:, 0:1], scalar2=dl[:, 0:1],
                    op0=ALU.is_equal, op1=ALU.mult)
                c.v("tensor_scalar", [brw], [brw], out=rw2[:, :], in0=lg[:, :], scalar1=v8[:, 1:2], scalar2=dl[:, 1:2],
                    op0=ALU.is_equal, op1=ALU.mult)
                c.v("tensor_tensor", [brw], [brw], out=rwt[:, :], in0=rwt[:, :], in1=rw2[:, :], op=ALU.add)
                c.store(rwo[rows, :], rwt[:, :], [brw])
    return c.finish()


def s4a_inputs(x, oT_res, L, W, S, consts):
    m1, m2 = [], []
    w_in = W["w_in"][L]
    wb1 = np.ascontiguousarray(w_in[:, 0:1536])
    wbg = np.ascontiguousarray(w_in[:, O_BG:O_BG + 3072])
    kk = np.arange(128)[:, None]; tt = np.arange(128)[None, :]
    trilT = (kk <= tt).astype(np.float32)
    sg_wT = np.ascontiguousarray(W["sg_w"][L].transpose(0, 2, 1))
    psc = np.ascontiguousarray(W["pool_scale"][L].reshape(4, 128).T)
    for core in range(8):
        b, blocks = core_block_ids(S, core)
        xin = make_xin(x[b], blocks)
        invc = np.zeros((128, 4, 16), np.float32)
        for g in range(4):
            w = 2 ** (g + 1)
            if blocks[0] == 0:
                invc[:, g, :] = 1.0 / np.minimum(np.arange(1, 17), w)
            else:
                invc[:, g, :] = 1.0 / w
        m1.append(dict(xin=xin, wb1=wb1, pool_w=W["pool_w"][L], pool_scale=psc, sg_ln_g=W["sg_ln_g"][L][None, :],
                       sg_ln_b=W["sg_ln_b"][L][None, :], sg_wT=sg_wT, sg_b=W["sg_b"][L].reshape(1, 512), trilT=trilT,
                       w_pool_out=W["w_pool_out"][L], w_sg_out=W["w_sg_out"][L], invc=invc.reshape(128, 64),
                       ident=consts["ident"]))
        d2 = dict(xin=xin, oT=oT_res[core]["oT"], wbg=wbg, w_nsa_out=W["w_nsa_out"][L], w_out=W["w_out"][L],
                  ln1_g=W["ln1_g"][L][None, :], ln1_b=W["ln1_b"][L][None, :], ident=consts["ident"])
        if L % 2 == 1:
            d2["w_router"] = W["moe_router"][L // 2]; d2["b_router"] = W["moe_router_b"][L // 2][None, :]
        m2.append(d2)
    return m1, m2


def build_s4b(NB, NE):
    c = Ctx()
    T = NB * BLK
    TB = min(4, NB)
    NT = NB // TB
    NC = TB * 128
    NFC = DFF // 128
    x1Td = c.din("x1T", [D, T], BF16)
    rwd = c.din("rw", [T, NE])
    wgd = c.din("wg", [NE, D, DFF]); wud = c.din("wu", [NE, D, DFF]); wdd = c.din("wd", [NE, DFF, D])
    fo = c.dout("f", [T, D])
    c.init_psum()
    x1T = [c.sb([128, 8, NC], BF16) for _ in range(2)]; bx = [Buf(), Buf()]
    rw = [c.sb([128, TB, NE]) for _ in range(2)]
    NW = 3
    wgst = [c.sb([128, 8, 128]) for _ in range(NW)]; bwgst = [Buf() for _ in range(NW)]
    wust = [c.sb([128, 8, 128]) for _ in range(NW)]; bwust = [Buf() for _ in range(NW)]
    wgc = [c.sb([128, 8, 128], BF16) for _ in range(NW)]; bwgc = [Buf() for _ in range(NW)]
    wuc = [c.sb([128, 8, 128], BF16) for _ in range(NW)]; bwuc = [Buf() for _ in range(NW)]
    wdst = [c.sb([128, D]) for _ in range(NW)]; bwdst = [Buf() for _ in range(NW)]
    wdb = [c.sb([128, NFC, D], BF16) for _ in range(2)]; bwdb = [Buf(), Buf()]
    hT = c.sb([128, NFC, NC], BF16); bhT = Buf()
    sg = [c.sb([128, NC]) for _ in range(2)]; bsg = [Buf(), Buf()]
    _f = c.sb([128, TB, D]); _bf = Buf()
    facc = [_f, _f]; bfacc = [_bf, _bf]
    it = 0
    for ti in range(NT):
        p = ti % 2
        cols = slice(ti * NC, (ti + 1) * NC)
        c.load(x1T[p][:, :, :], x1Td[:, cols].rearrange("(c p) t -> p c t", p=128), [bx[p]])
        c.load(rw[p][:, :, :], rwd[cols, :].rearrange("(n p) e -> p n e", p=128), [bx[p]],
               allow_slow_non_contiguous=True)
        for e in range(NE):
            wp = it % 2; it += 1
            for cc in range(NFC):
                i = cc % NW
                fs = slice(cc * 128, (cc + 1) * 128)
                c.load(wgst[i][:, :, :], wgd[e][:, fs].rearrange("(k p) f -> p k f", p=128), [bwgst[i]], q="sync")
                c.load(wust[i][:, :, :], wud[e][:, fs].rearrange("(k p) f -> p k f", p=128), [bwust[i]], q="sync")
                c.load(wdst[i][:, :], wdd[e][fs, :], [bwdst[i]], q="gpsimd")
                c.v("tensor_copy", [bwgst[i]], [bwgc[i]], out=wgc[i][:, :, :], in_=wgst[i][:, :, :])
                c.act(wuc[i][:, :, :], wust[i][:, :, :], AF.Copy, [bwust[i]], [bwuc[i]])
                c.v("tensor_copy", [bwdst[i]], [bwdb[wp]], eng="gpsimd", out=wdb[wp][:, cc, :], in_=wdst[i][:, :])
                pg, pgb = c.ps()
                for k in range(8):
                    c.mm(pg[:, 0:NC], wgc[i][:, k, :], x1T[p][:, k, :], k == 0, k == 7, [bwgc[i], bx[p]], [pgb])
                pu, pub = c.ps()
                for k in range(8):
                    c.mm(pu[:, 0:NC], wuc[i][:, k, :], x1T[p][:, k, :], k == 0, k == 7, [bwuc[i], bx[p]], [pub])
                s = cc % 2
                c.act(sg[s][:, :], pg[:, 0:NC], AF.Silu, [pgb], [bsg[s]])
                c.v("tensor_tensor", [bsg[s], pub], [bhT], out=hT[:, cc, :], in0=sg[s][:, :], in1=pu[:, 0:NC], op=ALU.mult)
            for b in range(TB):
                for half in range(2):
                    pt, pb = c.ps()
                    for cc in range(NFC):
                        c.mm(pt[:, 0:512], hT[:, cc, b * 128:(b + 1) * 128], wdb[wp][:, cc, half * 512:(half + 1) * 512],
                             cc == 0, cc == NFC - 1, [bhT, bwdb[wp]], [pb])
                    dst = facc[p][:, b, half * 512:(half + 1) * 512]
                    if e == 0:
                        c.v("tensor_scalar", [pb, bx[p]], [bfacc[p]], out=dst, in0=pt[:, 0:512], scalar1=rw[p][:, b, e:e + 1],
                            scalar2=None, op0=ALU.mult)
                    else:
                        c.v("scalar_tensor_tensor", [pb, bx[p], bfacc[p]], [bfacc[p]], out=dst, in0=pt[:, 0:512],
                            scalar=rw[p][:, b, e:e + 1], in1=dst, op0=ALU.mult, op1=ALU.add)
        c.store(fo[cols, :].rearrange("(n p) d -> p n d", p=128), facc[p][:, :, :], [bfacc[p]])
    return c.finish()


def build_s4c(NB):
    c = Ctx()
    T = NB * BLK
    x1d = c.din("x1", [T, D]); x1Td = c.din("x1T", [D, T], BF16); fd = c.din("f", [T, D]); pd = c.din("p", [T, PLE])
    wpgd = c.din("wpg", [D, D]); bpgd = c.din("bpg", [1, D]); wppd = c.din("wpp", [PLE, D])
    g2d = c.din("ln2_g", [1, D]); b2d = c.din("ln2_b", [1, D]); identd = c.din("ident", [128, 128])
    xo = c.dout("x2", [T, D])
    c.init_psum()
    bw = Buf()
    ident = c.sb([128, 128]); bid = Buf()
    c.load(ident[:, :], identd[:, :], [bid])
    stage = [(c.sb([128, 2048]), Buf()) for _ in range(2)]
    wpg = c.sb([128, 8, D], BF16)
    for k in range(8):
        c.load_cast(wpg[:, k, :], bw, wpgd[k * 128:(k + 1) * 128, :], stage)
    wpp = c.sb([128, 2, D], BF16)
    for k in range(2):
        c.load_cast(wpp[:, k, :], bw, wppd[k * 128:(k + 1) * 128, :], stage)
    bpg = bcast_load(c, bpgd, D, bw); g2 = bcast_load(c, g2d, D, bw); b2 = bcast_load(c, b2d, D, bw)
    x1 = [c.sb([128, D]) for _ in range(2)]; f = [c.sb([128, D]) for _ in range(2)]; pt_ = [c.sb([128, PLE]) for _ in range(2)]
    x1T = [c.sb([128, 8, 128], BF16) for _ in range(2)]; bin_ = [Buf(), Buf()]
    pT = [c.sb([128, 2, 128], BF16) for _ in range(2)]; bpT = [Buf(), Buf()]
    gate = [c.sb([128, D]) for _ in range(2)]; bgate = [Buf(), Buf()]
    z = [c.sb([128, D]) for _ in range(2)]; bz = [Buf(), Buf()]
    out = [c.sb([128, D]) for _ in range(2)]; bout = [Buf(), Buf()]
    ln = LNorm(c, D)
    for j in range(NB):
        q = j % 2
        rows = slice(j * 128, (j + 1) * 128)
        c.load(x1[q][:, :], x1d[rows, :], [bin_[q]]); c.load(f[q][:, :], fd[rows, :], [bin_[q]])
        c.load(pt_[q][:, :], pd[rows, :], [bin_[q]])
        c.load(x1T[q][:, :, :], x1Td[:, rows].rearrange("(c p) t -> p c t", p=128), [bin_[q]])
        for k in range(2):
            tp, tb = c.ps()
            c.tr(tp[:, 0:128], pt_[q][:, k * 128:(k + 1) * 128], ident[:, :], [bin_[q], bid], [tb])
            c.v("tensor_copy", [tb], [bpT[q]], out=pT[q][:, k, :], in_=tp[:, 0:128])
        for half in range(2):
            hs = slice(half * 512, (half + 1) * 512)
            pg, pgb = c.ps()
            for k in range(8):
                c.mm(pg[:, 0:512], x1T[q][:, k, :], wpg[:, k, hs], k == 0, k == 7, [bin_[q], bw], [pgb])
            c.v("tensor_tensor", [pgb, bw], [bgate[q]], out=gate[q][:, hs], in0=pg[:, 0:512], in1=bpg[:, hs], op=ALU.add)
            c.act(gate[q][:, hs], gate[q][:, hs], AF.Sigmoid, [bgate[q]], [bgate[q]])
            pp, ppb = c.ps()
            for k in range(2):
                c.mm(pp[:, 0:512], pT[q][:, k, :], wpp[:, k, hs], k == 0, k == 1, [bpT[q], bw], [ppb])
            c.v("tensor_tensor", [bgate[q], ppb], [bgate[q]], out=gate[q][:, hs], in0=gate[q][:, hs], in1=pp[:, 0:512],
                op=ALU.mult)
        c.v("scalar_tensor_tensor", [bin_[q]], [bz[q]], out=z[q][:, :], in0=x1[q][:, :], scalar=float(ALPHA),
            in1=f[q][:, :], op0=ALU.mult, op1=ALU.add)
        c.v("tensor_tensor", [bz[q], bgate[q]], [bz[q]], out=z[q][:, :], in0=z[q][:, :], in1=gate[q][:, :], op=ALU.add)
        ln(z[q][:, :], bz[q], g2[:, :], b2[:, :], bw, out[q][:, :], bout[q])
        c.store(xo[rows, :], out[q][:, :], [bout[q]])
    return c.finish()


_PROGS = {}


def _prog(key, fn):
    if key not in _PROGS:
        _PROGS[key] = fn()
    return _PROGS[key]


def _run(nc, maps):
    return run_bass_kernel_spmd(nc, maps, core_ids=list(range(8))).results


def forward(inp, S):
    W = {k: np.asarray(v) for k, v in inp.items()}
    x = np.ascontiguousarray(W["x"], dtype=np.float32)
    positions = np.asarray(W["positions"]).astype(np.int32)
    consts = make_consts()
    NB = S // BLK // 4
    for L in range(DEPTH):
        moe = (L % 2 == 1)
        r1 = _run(_prog(("s1a", NB), lambda: build_s1a(NB)), s1a_inputs(x, positions, L, W, S, consts))
        r3 = _run(_prog(("s3", NB, S), lambda: build_s3(NB, S)), s3_inputs(r1, S))
        m1, m2 = s4a_inputs(x, r3, L, W, S, consts)
        del r1
        ra1 = _run(_prog(("s4a1", NB), lambda: build_s4a1(NB)), m1)
        for core in range(8):
            m2[core]["ypo"] = ra1[core]["ypo"]; m2[core]["yso"] = ra1[core]["yso"]
        ra2 = _run(_prog(("s4a2", NB, moe), lambda: build_s4a2(NB, moe)), m2)
        del m1, m2, ra1, r3
        j = L // 2
        if moe:
            wg, wu, wd = W["moe_w_gate"][j], W["moe_w_up"][j], W["moe_w_down"][j]
            NE = NEXP
        else:
            wg, wu, wd = W["ffn_w_gate"][j][None], W["ffn_w_up"][j][None], W["ffn_w_down"][j][None]
            NE = 1
        mb = []
        for core in range(8):
            rw = ra2[core]["rw"] if moe else np.ones((NB * BLK, 1), np.float32)
            mb.append(dict(x1T=ra2[core]["x1T"], rw=rw, wg=wg, wu=wu, wd=wd))
        rb = _run(_prog(("s4b", NB, NE), lambda: build_s4b(NB, NE)), mb)
        del mb
        mc = []
        for core in range(8):
            b, blocks = core_block_ids(S, core)
            tok = np.concatenate([np.arange(n * BLK, (n + 1) * BLK) for n in blocks])
            mc.append(dict(x1=ra2[core]["x1"], x1T=ra2[core]["x1T"], f=rb[core]["f"],
                           p=np.ascontiguousarray(W["p"][L][b][tok]), wpg=W["ple_gate_w"][L],
                           bpg=W["ple_gate_b"][L][None, :], wpp=W["ple_proj"][L], ln2_g=W["ln2_g"][L][None, :],
                           ln2_b=W["ln2_b"][L][None, :], ident=consts["ident"]))
        rc = _run(_prog(("s4c", NB), lambda: build_s4c(NB)), mc)
        xn = np.empty_like(x)
        for core in range(8):
            b, blocks = core_block_ids(S, core)
            for i, n in enumerate(blocks):
                xn[b, n * BLK:(n + 1) * BLK] = rc[core]["x2"][i * BLK:(i + 1) * BLK]
        x = xn
        del ra2, rb, rc, mc
    return x


def kernel(**inputs):
    S = int(np.asarray(inputs["x"]).shape[1])
    return forward(inputs, S).astype(np.float32)
```

```python
import contextlib
import numpy as np
import ml_dtypes
import concourse.bass as bass
import concourse.mybir as mybir
from concourse.bass_utils import run_bass_kernel_spmd

F32 = mybir.dt.float32
BF16 = mybir.dt.bfloat16
I32 = mybir.dt.int32
AF = mybir.ActivationFunctionType
ALU = mybir.AluOpType
AX = mybir.AxisListType
NPBF = ml_dtypes.bfloat16

D = 1024
POOL_W = 512
SG_W = 512
QW = 512
KVW = 128
HD = 64
DFF = 2816
NEXP = 8
PLE = 256
DEPTH = 2
ALPHA = (2 * DEPTH) ** 0.25
HALO = 16
BLK = 128
EXT = BLK + HALO
NEGM = -30000.0
WIDTHS = (512, 512, 512, 512, 128, 128, 128, 128, 128, 128, 24, 3072)
OFFS = np.concatenate([[0], np.cumsum(WIDTHS)]).astype(int)
(O_A, O_U, O_V, O_Q, O_KC, O_VC, O_KS, O_VS, O_KW, O_VW, O_NG, O_BG) = [int(v) for v in OFFS[:-1]]

COMPUTE = ("tensor", "vector", "scalar", "gpsimd")
SAME_ENGINE_SYNC = True


class Buf:
    __slots__ = ("name", "w", "r", "rd", "psum")

    def __init__(self, name="", psum=False):
        self.name = name
        self.psum = psum
        self.w = None
        self.r = {}
        self.rd = []


class Op:
    __slots__ = ("id", "eng", "fn", "deps", "dma", "tok", "needed")

    def __init__(self, id, eng, fn, deps, dma):
        self.id = id; self.eng = eng; self.fn = fn; self.deps = deps; self.dma = dma
        self.tok = None; self.needed = False


class Sched:
    def __init__(self, nc, es_global):
        self.nc = nc
        self.n_dma_sems = {"sync": 10, "gpsimd": 8, "scalar": 4}
        self.sems = {e: es_global.enter_context(nc.semaphore("s_" + e)) for e in COMPUTE}
        self.dsems = {q: [es_global.enter_context(nc.semaphore(f"d_{q}{i}")) for i in range(n)]
                      for q, n in self.n_dma_sems.items()}
        self.cnt = {e: 0 for e in COMPUTE}
        self.dcnt = {q: [0] * n for q, n in self.n_dma_sems.items()}
        self.drr = {q: 0 for q in self.n_dma_sems}
        self.bar_cnt = {e: 0 for e in COMPUTE}
        self.bar_dcnt = {q: [0] * n for q, n in self.n_dma_sems.items()}
        self._reset()

    def _reset(self):
        self.ops = []
        self.per_eng = {e: [] for e in ("tensor", "vector", "scalar", "gpsimd", "sync")}

    def op(self, eng, fn, reads=(), writes=(), dma=False):
        deps = set()
        oid = len(self.ops)
        px = [b for b in reads if b.psum and b not in writes]
        if px:
            writes = list(writes) + px
            reads = [b for b in reads if not b.psum]
        for b in reads:
            if b.w is not None:
                deps.add(b.w)
        for b in writes:
            if b.w is not None:
                deps.add(b.w)
            deps.update(b.r.values())
            deps.update(b.rd)
        for b in reads:
            if dma:
                b.rd.append(oid)
            else:
                b.r[eng] = oid
        for b in writes:
            b.w = oid
            b.r = {}
            b.rd = []
        deps.discard(oid)
        o = Op(oid, eng, fn, sorted(deps), dma)
        self.ops.append(o)
        self.per_eng[eng].append(o)
        return o

    def dma(self, q, out_ap, in_ap, reads=(), writes=(), **kw):
        return self.op(q, lambda e: e.dma_start(out=out_ap, in_=in_ap, **kw), reads, writes, dma=True)

    def flush(self):
        nc = self.nc
        ops = self.ops
        if not ops:
            return
        for o in ops:
            for d in o.deps:
                p = ops[d]
                if p.dma:
                    continue
                if p.eng != o.eng or o.dma or (SAME_ENGINE_SYNC and o.eng != "tensor"):
                    p.needed = True
        for e in COMPUTE:
            lst = [o for o in self.per_eng[e] if not o.dma]
            if lst:
                lst[-1].needed = True
        sems, dsems, cnt, dcnt, drr = self.sems, self.dsems, self.cnt, self.dcnt, self.drr
        prev_on_sem = {}
        for o in ops:
            if o.dma:
                j = drr[o.eng]; drr[o.eng] = (j + 1) % len(dsems[o.eng])
                dcnt[o.eng][j] += 16
                key = (o.eng, j)
                o.tok = (dsems[o.eng][j], dcnt[o.eng][j], key, prev_on_sem.get(key))
                prev_on_sem[key] = o.id
            elif o.needed:
                cnt[o.eng] += 1
                o.tok = (sems[o.eng], cnt[o.eng], o.eng, None)
        bar_cnt, bar_dcnt = self.bar_cnt, self.bar_dcnt

        def emit(engname, eng):
            seen = {}
            for F in COMPUTE:
                if F != engname and bar_cnt[F] > 0:
                    eng.wait_ge(sems[F], bar_cnt[F]); seen[F] = bar_cnt[F]
            for q, vals in bar_dcnt.items():
                for j, v in enumerate(vals):
                    if v > 0:
                        eng.wait_ge(dsems[q][j], v); seen[(q, j)] = v
            for o in self.per_eng[engname]:
                need = {}
                for d in o.deps:
                    p = ops[d]
                    if (not p.dma) and p.eng == o.eng and not o.dma and not (SAME_ENGINE_SYNC and o.eng != "tensor"):
                        continue
                    sem, val, key, _ = p.tok
                    if need.get(key, (None, 0))[1] < val:
                        need[key] = (sem, val)
                if o.dma and o.tok[3] is not None:
                    sem, val, key, _ = ops[o.tok[3]].tok
                    if need.get(key, (None, 0))[1] < val:
                        need[key] = (sem, val)
                for key, (sem, val) in need.items():
                    if seen.get(key, 0) < val:
                        eng.wait_ge(sem, val)
                        seen[key] = val
                ins = o.fn(eng)
                if o.dma:
                    ins.then_inc(o.tok[0], 16)
                elif o.needed:
                    ins.then_inc(o.tok[0], 1)
            if engname == "sync":
                for q, vals in dcnt.items():
                    for j, v in enumerate(vals):
                        if v > 0 and seen.get((q, j), 0) < v:
                            eng.wait_ge(dsems[q][j], v); seen[(q, j)] = v

        with contextlib.ExitStack() as bes:
            block = bes.enter_context(nc.Block())

            @block.tensor
            def _(e):
                emit("tensor", e)

            @block.vector
            def _(e):
                emit("vector", e)

            @block.scalar
            def _(e):
                emit("scalar", e)

            @block.gpsimd
            def _(e):
                emit("gpsimd", e)

            @block.sync
            def _(e):
                emit("sync", e)
        self.bar_cnt = dict(cnt)
        self.bar_dcnt = {q: list(v) for q, v in dcnt.items()}
        self._reset()


class Ctx:
    def __init__(self, name="k", chained=False):
        self.nc = bass.Bass("TRN2", target_bir_lowering=False)
        self.ges = contextlib.ExitStack()
        self.S = Sched(self.nc, self.ges)
        self.es = contextlib.ExitStack()
        self.outs = []
        self._n = 0
        self.psum = []
        self._pi = 0
        self._dq = 0
        self.chained = chained
        self.dram = {}
        self.in_names = []

    def din(self, name, shape, dt=F32):
        if name in self.dram:
            ap = self.dram[name]
            assert list(ap.shape) == list(shape), (name, ap.shape, shape)
            return ap
        ap = self.nc.dram_tensor(name, list(shape), dt, kind="ExternalInput").ap()
        self.dram[name] = ap
        self.in_names.append(name)
        return ap

    def dout(self, name, shape, dt=F32):
        ap = self.nc.dram_tensor(name, list(shape), dt, kind="ExternalOutput").ap()
        self.dram[name] = ap
        return ap

    def end_stage(self):
        self.S.flush()
        self.es.close()
        self.es = contextlib.ExitStack()
        self.psum = []
        self._pi = 0
        self.outs = []

    def done(self):
        if self.chained:
            self.end_stage()
            return self
        return self.finish()

    def sb(self, shape, dt=F32, name=None, es=None):
        self._n += 1
        t = (es or self.es).enter_context(self.nc.sbuf_tensor(name or f"t{self._n}", list(shape), dt))
        return t

    def init_psum(self, n=8):
        for i in range(n):
            self._n += 1
            t = self.es.enter_context(self.nc.psum_tensor(f"ps{i}_{self._n}", [128, 512], F32))
            self.psum.append((t, Buf(f"ps{i}", psum=True)))

    def ps(self):
        p = self.psum[self._pi % len(self.psum)]
        self._pi += 1
        return p

    def q(self):
        self._dq += 1
        return ("sync", "gpsimd")[self._dq % 2]

    def mm(self, out, lhsT, rhs, start, stop, reads, writes):
        return self.S.op("tensor", lambda e: e.matmul(out, lhsT=lhsT, rhs=rhs, start=start, stop=stop),
                         reads, writes)

    def tr(self, out, in_, ident, reads, writes):
        return self.S.op("tensor", lambda e: e.transpose(out, in_, ident), reads, writes)

    def act(self, out, in_, func, reads, writes, **kw):
        return self.S.op("scalar", lambda e: e.activation(out=out, in_=in_, func=func, **kw), reads, writes)

    def v(self, meth, reads, writes, eng="vector", **kw):
        return self.S.op(eng, lambda e: getattr(e, meth)(**kw), reads, writes)

    def load(self, out, in_, writes, reads=(), q=None, **kw):
        return self.S.dma(q or self.q(), out, in_, reads=reads, writes=writes, **kw)

    def store(self, out, in_, reads, q=None, writes=(), **kw):
        o = self.S.dma(q or self.q(), out, in_, reads=reads, writes=writes, **kw)
        self.outs.append(o)
        return o

    def finish(self):
        self.end_stage()
        self.ges.close()
        return self.nc

    def load_cast(self, dst, dst_buf, src, stage_pool, eng_cycle=("vector", "gpsimd")):
        st, sbuf = stage_pool[self._n % len(stage_pool)]
        self._n += 1
        shp = list(dst.shape)
        view = st
        n = 1
        for s in shp[1:]:
            n *= s
        sv = st[0:shp[0], 0:n]
        if len(shp) == 3:
            sv = sv.rearrange("p (a b) -> p a b", b=shp[2])
        self.load(sv, src, writes=[sbuf])
        eng = eng_cycle[self._n % len(eng_cycle)]
        self.v("tensor_copy", [sbuf], [dst_buf], eng=eng, out=dst, in_=sv)


def make_consts():
    ident = np.eye(128, dtype=np.float32)
    perm = np.zeros((128, 128), np.float32)
    for hh in range(2):
        for d in range(64):
            if d < 8:
                perm[hh * 64 + d + 8, hh * 64 + d] = 1
            elif d < 16:
                perm[hh * 64 + d - 8, hh * 64 + d] = 1
            else:
                perm[hh * 64 + d, hh * 64 + d] = 1
    half = 8
    inv = (500000.0 ** (-np.arange(half, dtype=np.float32) / half)).astype(np.float32)
    invf = np.zeros((128, 1), np.float32)
    sgn = np.zeros((128, 1), np.float32)
    for hh in range(2):
        for d in range(16):
            invf[hh * 64 + d, 0] = inv[d % 8]
            sgn[hh * 64 + d, 0] = -1.0 if d < 8 else 1.0
    return dict(ident=ident, perm=perm, invf=invf, sgn=sgn)


def load_x_tile(c, xin, blk0, nb, xt, bxt, xh, bxh):
    for b in range(nb):
        r0 = (blk0 + b) * EXT
        c.load(xt[:, b, :], xin[r0 + HALO:r0 + EXT, :], writes=[bxt])
        if xh is not None:
            c.load(xh[16 * b:16 * b + 16, :], xin[r0:r0 + HALO, :], writes=[bxh])


def transpose_x(c, xt, bxt, nb, xT, bxT, ident, bid, xh=None, bxh=None, xTh=None, bxTh=None):
    for k in range(8):
        pt, pb = c.ps()
        for b in range(nb):
            c.tr(pt[:, b * 128:(b + 1) * 128], xt[:, b, k * 128:(k + 1) * 128], ident[:, :], [bxt, bid], [pb])
        if k % 2 == 0:
            c.v("tensor_copy", [pb], [bxT], out=xT[:, k, 0:nb * 128], in_=pt[:, 0:nb * 128])
        else:
            c.act(xT[:, k, 0:nb * 128], pt[:, 0:nb * 128], AF.Copy, [pb], [bxT])
        if xh is not None:
            pt, pb = c.ps()
            c.tr(pt[:, 0:128], xh[:, k * 128:(k + 1) * 128], ident[:, :], [bxh, bid], [pb])
            c.v("tensor_copy", [pb], [bxTh], out=xTh[:, k, 0:16 * nb], in_=pt[:, 0:16 * nb])


WA_COLS = 1816
A_Q, A_KC, A_VC, A_KS, A_KW, A_VS, A_VW, A_NG = 0, 1024, 1152, 1280, 1408, 1536, 1664, 1792


def build_s1a(NB, stop=99, c=None):
    c = c or Ctx()
    nc = c.nc
    T = NB * BLK
    TB = min(2, NB)
    NT = NB // TB
    NS = NB * 8
    xin = c.din("xin", [NB * EXT, D])
    pos = c.din("pos", [1, NB * EXT], I32)
    wa = c.din("wa", [D, WA_COLS])
    kposT = c.din("kposT", [64, 32]); kw1 = c.din("kw1", [2048, 256]); kw2 = c.din("kw2", [256, 64])
    vposT = c.din("vposT", [64, 32]); vw1 = c.din("vw1", [2048, 256]); vw2 = c.din("vw2", [256, 64])
    identd = c.din("ident", [128, 128]); permd = c.din("perm", [128, 128])
    invfd = c.din("invf", [128, 1]); sgnd = c.din("sgn", [128, 1])
    QT = c.dout("QT", [8, 64, T], BF16)
    gates = c.dout("gates", [T, 24])
    KsT = c.dout("KsT", [128, T], BF16); KwT = c.dout("KwT", [128, T], BF16)
    Vs = c.dout("Vs", [T, 128], BF16); Vw = c.dout("Vw", [T, 128], BF16)
    kcmpT = c.dout("kcmpT", [2, 64, NS], BF16); vcmp = c.dout("vcmp", [NS, 128], BF16)
    c.init_psum()

    ident = c.sb([128, 128]); bid = Buf()
    c.load(ident[:, :], identd[:, :], [bid])
    stage = [(c.sb([128, 2048]), Buf()) for _ in range(1)]
    permb = c.sb([128, 128], BF16); bperm = Buf()
    c.load_cast(permb[:, :], bperm, permd[:, :], stage)
    invf = c.sb([128, 1]); sgn = c.sb([128, 1]); bsm = Buf()
    c.load(invf[:, :], invfd[:, :], [bsm]); c.load(sgn[:, :], sgnd[:, :], [bsm])
    wab = c.sb([128, 8, WA_COLS], BF16); bwa = Buf()
    for k in range(8):
        c.load_cast(wab[:, k, :], bwa, wa[k * 128:(k + 1) * 128, :], stage)
    w1b = {}; w2b = {}; pTb = {}; bcw = Buf()
    for nm, w1d, w2d, pd in (("k", kw1, kw2, kposT), ("v", vw1, vw2, vposT)):
        w1b[nm] = c.sb([128, 32, 256], BF16)
        w1v = w1d.rearrange("(l d) h -> d l h", d=64)
        for half in range(2):
            for l0 in range(0, 32, 8):
                c.load_cast(w1b[nm][half * 64:half * 64 + 64, l0:l0 + 8, :], bcw, w1v[:, l0:l0 + 8, :], stage)
        w2b[nm] = c.sb([128, 2, 64], BF16)
        c.load_cast(w2b[nm][:, :, :], bcw, w2d.rearrange("(c p) d -> p c d", p=128), stage)
        pTb[nm] = c.sb([128, 32], BF16)
        c.v("memset", [], [bcw], ap=pTb[nm][:, :], constant=0.0)
        c.load_cast(pTb[nm][0:64, :], bcw, pd[:, :], stage)
    if stop <= 0:
        return c.done()
    NP = NB * EXT
    Ct, bC, St, bS = rope_tables(c, pos, NP, invf, sgn, bsm)
    C3 = Ct[:, :].rearrange("p (n e) -> p n e", e=EXT)
    S3 = St[:, :].rearrange("p (n e) -> p n e", e=EXT)
    if stop <= 1:
        return c.done()
    kcx = [c.sb([128, NB, EXT], BF16) for _ in range(2)]; vcx = [c.sb([128, NB, EXT], BF16) for _ in range(2)]
    bkcx = Buf(); bvcx = Buf()
    for t_ in kcx:
        c.v("memset", [], [bkcx], eng="gpsimd", ap=t_[:, :, :], constant=0.0)
    for t_ in vcx:
        c.v("memset", [], [bvcx], eng="gpsimd", ap=t_[:, :, :], constant=0.0)

    xt = [c.sb([128, TB, D]) for _ in range(2)]; bxt = [Buf(), Buf()]
    _xh = c.sb([128, D]); _bxh = Buf()
    xh = [_xh, _xh]; bxh = [_bxh, _bxh]
    c.v("memset", [], [_bxh], eng="gpsimd", ap=_xh[:, :], constant=0.0)
    xT = [c.sb([128, 8, TB * 128], BF16) for _ in range(2)]; bxT = [Buf(), Buf()]
    xTh = [c.sb([128, 8, 128], BF16) for _ in range(2)]; bxTh = [Buf(), Buf()]
    _q = c.sb([128, 8, TB * 128], BF16); _bq = Buf()
    qst = [_q, _q]; bqst = [_bq, _bq]
    kst = [c.sb([128, 2, TB * 128], BF16) for _ in range(2)]; bkst = [Buf(), Buf()]
    vsw = [c.sb([128, TB, 256], BF16) for _ in range(2)]; bvsw = [Buf(), Buf()]
    gts = [c.sb([128, TB, 24]) for _ in range(2)]; bgts = [Buf(), Buf()]
    rope = RopeEvac(c, permb, bperm, TB * 128, bC, bS, nbuf=2)

    for ti in range(NT):
        p = ti % 2
        blk0 = ti * TB
        load_x_tile(c, xin, blk0, TB, xt[p], bxt[p], xh[p], bxh[p])
        transpose_x(c, xt[p], bxt[p], TB, xT[p], bxT[p], ident, bid, xh[p], bxh[p], xTh[p], bxTh[p])
        if stop <= 1.1:
            continue
        NC = TB * 128
        Cown = C3[:, blk0:blk0 + TB, HALO:EXT]; Sown = S3[:, blk0:blk0 + TB, HALO:EXT]
        Chal = C3[:, blk0:blk0 + TB, 0:HALO]; Shal = S3[:, blk0:blk0 + TB, 0:HALO]

        def proj(col, xTt, bx, ncols):
            pt, pb = c.ps()
            for k in range(8):
                c.mm(pt[:, 0:ncols], wab[:, k, col:col + 128], xTt[:, k, 0:ncols], k == 0, k == 7, [bwa, bx], [pb])
            return pt, pb

        v3 = lambda ap: ap.rearrange("p (n e) -> p n e", n=TB)
        for h in range(8):
            pt, pb = proj(A_Q + 128 * h, xT[p], bxT[p], NC)
            rope(pt, pb, NC, 0.125, Cown, Sown, [(slice(0, 128), v3(qst[p][:, h, :]), bqst[p])])
        if stop <= 1.2:
            continue
        c.store(QT[:, :, blk0 * 128:blk0 * 128 + NC].rearrange("h d t -> d h t"), qst[p][0:64, :, :], [bqst[p]])
        if stop <= 1.3:
            continue
        for j, col in enumerate((A_KS, A_KW)):
            pt, pb = proj(col, xT[p], bxT[p], NC)
            rope(pt, pb, NC, 1.0, Cown, Sown, [(slice(0, 128), v3(kst[p][:, j, :]), bkst[p])])
        c.store(KsT[:, blk0 * 128:blk0 * 128 + NC], kst[p][:, 0, :], [bkst[p]])
        c.store(KwT[:, blk0 * 128:blk0 * 128 + NC], kst[p][:, 1, :], [bkst[p]])
        pt, pb = proj(A_KC, xT[p], bxT[p], NC)
        rope(pt, pb, NC, 1.0, Cown, Sown, [(slice(0, 64), kcx[0][0:64, blk0:blk0 + TB, HALO:EXT], bkcx),
                                           (slice(64, 128), kcx[1][64:128, blk0:blk0 + TB, HALO:EXT], bkcx)])
        pt, pb = proj(A_KC, xTh[p], bxTh[p], 16 * TB)
        rope(pt, pb, 16 * TB, 1.0, Chal, Shal, [(slice(0, 64), kcx[0][0:64, blk0:blk0 + TB, 0:HALO], bkcx),
                                                (slice(64, 128), kcx[1][64:128, blk0:blk0 + TB, 0:HALO], bkcx)])
        pt, pb = proj(A_VC, xT[p], bxT[p], NC)
        for hh in range(2):
            ps_ = slice(hh * 64, hh * 64 + 64)
            c.act(vcx[hh][ps_, blk0:blk0 + TB, HALO:EXT], v3(pt[ps_, 0:NC]), AF.Copy, [pb], [bvcx])
        pt, pb = proj(A_VC, xTh[p], bxTh[p], 16 * TB)
        for hh in range(2):
            ps_ = slice(hh * 64, hh * 64 + 64)
            c.act(vcx[hh][ps_, blk0:blk0 + TB, 0:HALO], v3(pt[ps_, 0:16 * TB]), AF.Copy, [pb], [bvcx])
        if stop <= 1.4:
            continue
        for b in range(TB):
            pt, pb = c.ps()
            for k in range(8):
                c.mm(pt[:, 0:280], xT[p][:, k, b * 128:(b + 1) * 128], wab[:, k, A_VS:A_VS + 280], k == 0, k == 7,
                     [bwa, bxT[p]], [pb])
            c.v("tensor_copy", [pb], [bvsw[p]], out=vsw[p][:, b, :], in_=pt[:, 0:256])
            c.act(gts[p][:, b, :], pt[:, 256:280], AF.Sigmoid, [pb], [bgts[p]])
        rows = slice(blk0 * 128, blk0 * 128 + NC)
        c.store(Vs[rows, :].rearrange("(n p) f -> p n f", p=128), vsw[p][:, :, 0:128], [bvsw[p]])
        c.store(Vw[rows, :].rearrange("(n p) f -> p n f", p=128), vsw[p][:, :, 128:256], [bvsw[p]])
        c.store(gates[rows, :].rearrange("(n p) f -> p n f", p=128), gts[p][:, :, :], [bgts[p]])

    if stop <= 2:
        return c.done()
    cb = c.sb([128, 2, 2]); bcb = Buf()
    gh = c.sb([128, 2, NS], BF16); bgh = Buf()
    kco = c.sb([64, 2, NS], BF16); bkco = Buf()
    vco = c.sb([128, (NS + 127) // 128, 128], BF16); bvco = Buf()
    for kvi, (nm, ext, bext) in enumerate((("k", kcx, bkcx), ("v", vcx, bvcx))):
        for cc in range(2):
            pt, pb = c.ps()
            for l in range(32):
                c.mm(pt[:, 0:1], w1b[nm][:, l, cc * 128:(cc + 1) * 128], pTb[nm][:, l:l + 1], l == 0, l == 31,
                     [bcw], [pb])
            c.v("tensor_copy", [pb], [bcb], out=cb[:, kvi, cc:cc + 1], in_=pt[:, 0:1])
        for h in range(2):
            for cc in range(2):
                pt, pb = c.ps()
                for l in range(32):
                    rhs = ext[h][:, :, :].rearrange("p n (s r) -> p n s r", r=16)[:, :, l // 16:l // 16 + 8, l % 16]
                    c.mm(pt[:, 0:NS].rearrange("p (n s) -> p n s", s=8), w1b[nm][:, l, cc * 128:(cc + 1) * 128], rhs,
                         l == 0, l == 31, [bcw, bext], [pb])
                c.act(gh[:, cc, :], pt[:, 0:NS], AF.Gelu_apprx_tanh, [pb, bcb], [bgh], bias=cb[:, kvi, cc:cc + 1])
            if nm == "k":
                pt, pb = c.ps()
                for cc in range(2):
                    c.mm(pt[0:64, 0:NS], w2b[nm][:, cc, :], gh[:, cc, :], cc == 0, cc == 1, [bcw, bgh], [pb])
                c.v("tensor_copy", [pb], [bkco], out=kco[:, h, :], in_=pt[0:64, 0:NS])
            else:
                for s0 in range(0, NS, 128):
                    sn = min(128, NS - s0)
                    pt, pb = c.ps()
                    for cc in range(2):
                        c.mm(pt[0:sn, 0:64], gh[:, cc, s0:s0 + sn], w2b[nm][:, cc, :], cc == 0, cc == 1, [bcw, bgh], [pb])
                    c.v("tensor_copy", [pb], [bvco], out=vco[0:sn, s0 // 128, h * 64:(h + 1) * 64], in_=pt[0:sn, 0:64])
    c.store(kcmpT.rearrange("h d s -> d h s"), kco[:, :, :], [bkco])
    if NS >= 128:
        c.store(vcmp.rearrange("(n p) f -> p n f", p=128), vco[:, :, :], [bvco])
    else:
        c.store(vcmp[:, :], vco[0:NS, 0, :], [bvco])
    return c.done()


def rope_tables(c, pos, NP, invf, sgn, bsm):
    Ct = c.sb([128, NP]); St = c.sb([128, NP]); bC = Buf(); bS = Buf()
    CH = 576
    posi = c.sb([128, CH], I32); bpos = Buf()
    ang = c.sb([128, CH]); bang = Buf()
    tmpA = c.sb([128, CH]); tmpB = c.sb([128, CH]); tmpI = c.sb([128, CH], I32); btmp = Buf()
    TWO_PI = float(2 * np.pi)
    for c0 in range(0, NP, CH):
        w = min(CH, NP - c0)
        c.load(posi[:, 0:w], pos[0:1, c0:c0 + w].partition_broadcast(128), [bpos])
        c.v("tensor_copy", [bpos], [bang], out=ang[:, 0:w], in_=posi[:, 0:w])
        c.v("tensor_scalar", [bang, bsm], [bang], out=ang[:, 0:w], in0=ang[:, 0:w], scalar1=invf[:, 0:1], scalar2=None,
            op0=ALU.mult)
        for dst, bdst, shift in ((St, bS, 0.0), (Ct, bC, float(np.pi / 2))):
            A = tmpA[:, 0:w]; B = tmpB[:, 0:w]; I_ = tmpI[:, 0:w]
            c.v("tensor_scalar", [bang], [btmp], out=A, in0=ang[:, 0:w], scalar1=shift, scalar2=None, op0=ALU.add)
            c.v("tensor_scalar", [btmp], [btmp], out=I_, in0=A, scalar1=float(1 / TWO_PI), scalar2=None, op0=ALU.mult)
            c.v("tensor_copy", [btmp], [btmp], out=B, in_=I_)
            c.v("scalar_tensor_tensor", [btmp], [btmp], out=A, in0=B, scalar=-TWO_PI, in1=A, op0=ALU.mult, op1=ALU.add)
            c.v("tensor_scalar", [btmp], [btmp], out=B, in0=A, scalar1=float(np.pi), scalar2=-TWO_PI, op0=ALU.is_gt,
                op1=ALU.mult)
            c.v("tensor_tensor", [btmp], [btmp], out=A, in0=A, in1=B, op=ALU.add)
            c.v("tensor_scalar", [btmp], [btmp], out=B, in0=A, scalar1=float(-np.pi), scalar2=TWO_PI, op0=ALU.is_lt,
                op1=ALU.mult)
            c.v("tensor_tensor", [btmp], [btmp], out=A, in0=A, in1=B, op=ALU.add)
            c.v("tensor_scalar", [btmp], [btmp], out=A, in0=A, scalar1=float(np.pi), scalar2=float(-np.pi), op0=ALU.min,
                op1=ALU.max)
            c.act(dst[:, c0:c0 + w], A, AF.Sin, [btmp], [bdst])
    c.v("tensor_scalar", [bS, bsm], [bS], out=St[:, :], in0=St[:, :], scalar1=sgn[:, 0:1], scalar2=None, op0=ALU.mult)
    return Ct, bC, St, bS


ROPE_DBG = 99


class RopeEvac:
    def __init__(self, c, permb, bperm, maxcols, bC, bS, nbuf=3):
        self.c = c; self.permb = permb; self.bperm = bperm; self.bC = bC; self.bS = bS
        self.n = nbuf; self.i = 0
        self.kb = [c.sb([128, maxcols], BF16) for _ in range(nbuf)]; self.bkb = [Buf() for _ in range(nbuf)]
        self.t1 = [c.sb([128, maxcols]) for _ in range(nbuf)]; self.bt1 = [Buf() for _ in range(nbuf)]
        self.t2 = [c.sb([128, maxcols]) for _ in range(nbuf)]; self.bt2 = [Buf() for _ in range(nbuf)]

    def __call__(self, pt, pb, ncols, scale, Cap, Sap, dsts):
        c = self.c
        i = self.i % self.n; self.i += 1
        nbk = Cap.shape[1]
        kb, t1, t2 = self.kb[i], self.t1[i], self.t2[i]
        c.act(kb[:, 0:ncols], pt[:, 0:ncols], AF.Copy, [pb], [self.bkb[i]], scale=float(scale))
        if ROPE_DBG <= 0:
            return
        pp, ppb = c.ps()
        c.mm(pp[:, 0:ncols], self.permb[:, :], kb[:, 0:ncols], True, True, [self.bperm, self.bkb[i]], [ppb])
        v3 = lambda ap: ap.rearrange("p (n e) -> p n e", n=nbk)
        if ROPE_DBG <= 1:
            return
        c.v("scalar_tensor_tensor", [pb, self.bC], [self.bt1[i]], out=v3(t1[:, 0:ncols]), in0=v3(pt[:, 0:ncols]),
            scalar=float(scale), in1=Cap, op0=ALU.mult, op1=ALU.mult)
        if ROPE_DBG <= 2:
            return
        c.v("tensor_tensor", [ppb, self.bS], [self.bt2[i]], out=v3(t2[:, 0:ncols]), in0=v3(pp[:, 0:ncols]), in1=Sap,
            op=ALU.mult)
        if ROPE_DBG <= 3:
            return
        for (psl, dst3, bdst) in dsts:
            c.v("tensor_tensor", [self.bt1[i], self.bt2[i]], [bdst], eng="gpsimd", out=dst3,
                in0=v3(t1[psl, 0:ncols]), in1=v3(t2[psl, 0:ncols]), op=ALU.add)


def core_block_ids(S, core):
    nblk = S // BLK
    return core // 4, list(range(core % 4, nblk, 4))


def make_xin(xb, blocks):
    S, F = xb.shape
    out = np.zeros((len(blocks) * EXT, F), xb.dtype)
    for i, n in enumerate(blocks):
        lo = n * BLK - HALO
        if lo >= 0:
            out[i * EXT:(i + 1) * EXT] = xb[lo:lo + EXT]
        else:
            out[i * EXT + HALO:(i + 1) * EXT] = xb[0:BLK]
    return out


def wa_cols(w_in_l):
    sl = lambda o, w: w_in_l[:, o:o + w]
    z = np.zeros((w_in_l.shape[0], 64), w_in_l.dtype)
    qs = []
    for h in range(8):
        qs += [sl(O_Q + 64 * h, 64), z]
    return np.ascontiguousarray(np.concatenate(
        qs + [sl(O_KC, 128), sl(O_VC, 128), sl(O_KS, 128), sl(O_KW, 128), sl(O_VS, 128), sl(O_VW, 128),
              sl(O_NG, 24)], axis=1))


def s1a_inputs(x, positions, L, W, S, consts):
    maps = []
    wa = wa_cols(W["w_in"][L])
    for core in range(8):
        b, blocks = core_block_ids(S, core)
        xin = make_xin(x[b], blocks)
        pos = make_xin(positions[b][:, None].astype(np.int32), blocks).reshape(1, -1)
        maps.append(dict(
            xin=xin, pos=np.ascontiguousarray(pos), wa=wa,
            kposT=np.ascontiguousarray(W["cmp_k_pos"][L].T), kw1=W["cmp_k_w1"][L], kw2=W["cmp_k_w2"][L],
            vposT=np.ascontiguousarray(W["cmp_v_pos"][L].T), vw1=W["cmp_v_w1"][L], vw2=W["cmp_v_w2"][L],
            ident=consts["ident"], perm=consts["perm"], invf=consts["invf"], sgn=consts["sgn"]))
    return maps


DBG_K = -1


def build_s3(NB, S, stop=99, c=None):
    c = c or Ctx()
    T = NB * BLK
    NSLOT = S // 16
    NBLKT = S // 64
    NKT = S // 128
    NCHT = max(1, NSLOT // 128)
    QT = c.din("QT", [8, 64, T], BF16)
    gates = c.din("gates", [T, 24])
    KAd = c.din("KA", [2, 128, S], BF16)
    VsAd = c.din("VsA", [2, S, 65], BF16)
    KwBd = c.din("KwB", [NB, 2, 128, 640], BF16)
    VwBd = c.din("VwB", [NB, 2, 640, 65], BF16)
    kcTd = c.din("kcT", [2, 128, NSLOT], BF16)
    vcAd = c.din("vcA", [NSLOT, 128], BF16)
    tqd = c.din("tq", [128, NB]); curd = c.din("cur", [128, NB]); curm1d = c.din("curm1", [128, NB])
    sendd = c.din("slot_end", [128, NSLOT]); blkidxd = c.din("blkidx", [128, NBLKT])
    sdmd = c.din("sdm", [4, 128, 128], BF16); wmaskd = c.din("wmask", [2, 128, 128], BF16)
    identbd = c.din("identb", [128, 128], BF16)
    oT = c.dout("oT", [512, T], BF16)

    nc = c.nc
    def bank(nm):
        t = c.es.enter_context(nc.psum_tensor(nm, [128, 512], F32))
        return t, Buf(nm, psum=True)
    SC = [bank("sc0"), bank("sc1")]
    OC = bank("oc"); OS = bank("os"); OW = bank("ow")
    ST = [bank("st0"), bank("st1"), bank("st2")]

    bres = Buf()
    KA = [c.sb([128, S], BF16) for _ in range(2)]
    VS = [c.sb([128, NKT, 65], BF16) for _ in range(2)]
    for hk in range(2):
        for c0 in range(0, S, 2048):
            c.load(KA[hk][:, c0:c0 + 2048], KAd[hk, :, c0:c0 + 2048], [bres])
        vv = VsAd[hk].rearrange("(n p) f -> p n f", p=128)
        for n0 in range(0, NKT, 32):
            n1 = min(NKT, n0 + 32)
            c.load(VS[hk][:, n0:n1, :], vv[:, n0:n1, :], [bres])
    kcT = [c.sb([128, NSLOT], BF16) for _ in range(2)]
    for hk in range(2):
        c.load(kcT[hk][:, :], kcTd[hk], [bres])
    vcS = c.sb([128, NCHT, 128], BF16)
    c.load(vcS[:, :, :], vcAd.rearrange("(n p) f -> p n f", p=128), [bres])
    tq = c.sb([128, NB]); cur = c.sb([128, NB]); curm1 = c.sb([128, NB])
    c.load(tq[:, :], tqd[:, :], [bres]); c.load(cur[:, :], curd[:, :], [bres]); c.load(curm1[:, :], curm1d[:, :], [bres])
    send = c.sb([128, NSLOT]); blkidx = c.sb([128, NBLKT])
    c.load(send[:, :], sendd[:, :], [bres]); c.load(blkidx[:, :], blkidxd[:, :], [bres])
    identb = c.sb([128, 128], BF16)
    c.load(identb[:, :], identbd[:, :], [bres])
    sdm = c.sb([128, 4, 4, 128], BF16)
    wmask = c.sb([128, 2, 4, 128], BF16)
    for r in range(4):
        for g in range(4):
            c.load(sdm[:, r, g, :], sdmd[r], [bres])
    for r in range(2):
        for g in range(4):
            c.load(wmask[:, r, g, :], wmaskd[r], [bres])

    NG_MAX = (NBLKT + 63) // 64
    QP = [c.sb([128, 4, 128], BF16) for _ in range(2)]; bQP = [Buf(), Buf()]
    QM = [[c.sb([128, 4, 128], BF16) for _ in range(NG_MAX)] for _ in range(2)]
    bQM = [[Buf() for _ in range(NG_MAX)] for _ in range(2)]
    for p in range(2):
        c.v("memset", [], [bQP[p]], eng="gpsimd", ap=QP[p][:, :, :], constant=0.0)
    em = [[c.sb([128, NSLOT], BF16) for _ in range(4)] for _ in range(2)]; bem = [[Buf() for _ in range(4)] for _ in range(2)]
    e32 = [c.sb([128, NSLOT]) for _ in range(2)]; be32 = [Buf(), Buf()]
    rs = [c.sb([128, 4]) for _ in range(2)]; rinv = [c.sb([128, 4]) for _ in range(2)]; brs = [Buf(), Buf()]
    mx = c.sb([128, 4]); nmx = c.sb([128, 4]); bmx = Buf()
    Pacc = c.sb([128, NSLOT]); bPacc = Buf()
    valid = [c.sb([128, NSLOT]) for _ in range(2)]; bvalid = [Buf(), Buf()]
    m1 = [c.sb([128, NBLKT]) for _ in range(2)]; m2 = [c.sb([128, NBLKT]) for _ in range(2)]
    le = [c.sb([128, NBLKT]) for _ in range(2)]; bblk = [Buf(), Buf()]
    imp = c.sb([128, NBLKT]); tmpi = c.sb([128, NBLKT]); work = c.sb([128, NBLKT]); bimp = Buf()
    v8a = c.sb([128, 8]); v8b = c.sb([128, 8])
    MN = [c.sb([128, 64 + NBLKT + 64], BF16) for _ in range(2)]; bMN = [Buf(), Buf()]
    for p in range(2):
        c.v("memset", [], [bMN[p]], eng="gpsimd", ap=MN[p][:, :], constant=NEGM)
    emT = [c.sb([128, 128], BF16) for _ in range(3)]; bemT = [Buf() for _ in range(3)]
    PT = [c.sb([128, 512], BF16) for _ in range(3)]; bPT = [Buf() for _ in range(3)]
    KwB = [c.sb([128, 640], BF16) for _ in range(2)]; VwB = [c.sb([128, 5, 65], BF16) for _ in range(2)]
    bKw = [Buf(), Buf()]
    gt = [c.sb([128, 24]) for _ in range(2)]; bgt = [Buf(), Buf()]
    oacc = [c.sb([128, 8, 64]) for _ in range(2)]; boacc = [Buf(), Buf()]
    ob = c.sb([128, 512], BF16); bob = Buf()
    oTt = [c.sb([128, 4, 128], BF16) for _ in range(2)]; boTt = [Buf(), Buf()]
    coef = c.sb([128, 3, 4]); den = c.sb([128, 2, 4]); bcoef = Buf()
    cnt = {"st": 0, "emT": 0}

    def dims(j):
        NSj = min(NSLOT, 128 * ((j + 1 + 3) // 4))
        NBj = NSj // 4
        NGj = (NBj + 63) // 64
        return NSj, NBj, NGj

    def phase1a(k):
        j, hk = divmod(k, 2)
        p = k % 2
        jp = j % 2
        NSj, NBj, NGj = dims(j)
        qsrc = QT[hk * 4:(hk + 1) * 4, :, j * 128:(j + 1) * 128].rearrange("g d t -> d g t")
        c.load(QP[p][0:64, :, :], qsrc, [bQP[p]])
        for G in range(NGj):
            c.load(QM[p][G][0:64, :, :], qsrc, [bQM[p][G]])
        if hk == 0:
            c.load(gt[jp][:, :], gates[j * 128:(j + 1) * 128, :], [bgt[jp]])
            c.v("tensor_scalar", [bres], [bvalid[jp]], out=valid[jp][:, 0:NSj], in0=send[:, 0:NSj], scalar1=tq[:, j:j + 1],
                scalar2=NEGM, op0=ALU.is_gt, op1=ALU.mult)
            c.v("tensor_scalar", [bres], [bblk[jp]], out=m1[jp][:, 0:NBj], in0=blkidx[:, 0:NBj], scalar1=cur[:, j:j + 1],
                scalar2=1e4, op0=ALU.is_equal, op1=ALU.mult)
            c.v("tensor_scalar", [bres], [bblk[jp]], out=m2[jp][:, 0:NBj], in0=blkidx[:, 0:NBj], scalar1=curm1[:, j:j + 1],
                scalar2=1e4, op0=ALU.is_equal, op1=ALU.mult)
            c.v("tensor_tensor", [bblk[jp]], [bblk[jp]], out=m1[jp][:, 0:NBj], in0=m1[jp][:, 0:NBj], in1=m2[jp][:, 0:NBj],
                op=ALU.max)
            c.v("memset", [], [bblk[jp]], ap=m1[jp][:, 0:1], constant=1e4)
            c.v("tensor_scalar", [bres], [bblk[jp]], out=le[jp][:, 0:NBj], in0=blkidx[:, 0:NBj], scalar1=cur[:, j:j + 1],
                scalar2=None, op0=ALU.is_le)
        nch = (NSj + 511) // 512
        for g in range(4):
            for ci in range(nch):
                c0 = ci * 512; w = min(512, NSj - c0)
                c.mm(SC[ci][0][:, 0:w], QP[p][:, g, :], kcT[hk][:, c0:c0 + w], True, True, [bQP[p], bres], [SC[ci][1]])
                c.v("tensor_reduce", [SC[ci][1]], [bmx], out=mx[:, ci:ci + 1], in_=SC[ci][0][:, 0:w], axis=AX.X, op=ALU.max)
            if nch == 2:
                c.v("tensor_tensor", [bmx], [bmx], out=mx[:, 0:1], in0=mx[:, 0:1], in1=mx[:, 1:2], op=ALU.max)
            c.v("tensor_scalar", [bmx], [bmx], out=nmx[:, g:g + 1], in0=mx[:, 0:1], scalar1=-1.0, scalar2=None, op0=ALU.mult)
            for ci in range(nch):
                c0 = ci * 512; w = min(512, NSj - c0)
                c.v("tensor_tensor", [SC[ci][1], bvalid[jp]], [be32[g % 2]], out=e32[g % 2][:, c0:c0 + w],
                    in0=SC[ci][0][:, 0:w], in1=valid[jp][:, c0:c0 + w], op=ALU.add)
            c.act(em[p][g][:, 0:NSj], e32[g % 2][:, 0:NSj], AF.Exp, [be32[g % 2], bmx], [bem[p][g], brs[p]],
                  bias=nmx[:, g:g + 1], accum_out=rs[p][:, g:g + 1])
        c.v("tensor_scalar", [brs[p]], [brs[p]], out=rs[p][:, :], in0=rs[p][:, :], scalar1=1e-30, scalar2=None, op0=ALU.max)
        c.v("reciprocal", [brs[p]], [brs[p]], out=rinv[p][:, :], in_=rs[p][:, :])
        c.v("tensor_scalar", [bem[p][0], brs[p]], [bPacc], out=Pacc[:, 0:NSj], in0=em[p][0][:, 0:NSj],
            scalar1=rinv[p][:, 0:1], scalar2=None, op0=ALU.mult)
        for g in range(1, 4):
            c.v("scalar_tensor_tensor", [bem[p][g], brs[p], bPacc], [bPacc], out=Pacc[:, 0:NSj], in0=em[p][g][:, 0:NSj],
                scalar=rinv[p][:, g:g + 1], in1=Pacc[:, 0:NSj], op0=ALU.mult, op1=ALU.add)
        P4 = Pacc[:, 0:NSj].rearrange("p (b f) -> p b f", f=4)
        I = imp[:, 0:NBj]; Tm = tmpi[:, 0:NBj]
        c.v("tensor_tensor", [bPacc], [bimp], out=Tm, in0=P4[:, :, 1], in1=P4[:, :, 2], op=ALU.add)
        c.v("tensor_tensor", [bPacc, bimp], [bimp], out=Tm, in0=Tm, in1=P4[:, :, 3], op=ALU.add)
        c.v("scalar_tensor_tensor", [bPacc, bimp], [bimp], out=I, in0=Tm, scalar=2.0, in1=P4[:, :, 0], op0=ALU.mult,
            op1=ALU.add)
        if NBj > 1:
            c.v("tensor_tensor", [bPacc, bimp], [bimp], out=imp[:, 0:NBj - 1], in0=imp[:, 0:NBj - 1], in1=P4[:, 1:NBj, 0],
                op=ALU.add)
        c.v("tensor_tensor", [bimp, bblk[jp]], [bimp], out=I, in0=I, in1=m1[jp][:, 0:NBj], op=ALU.max)
        c.v("scalar_tensor_tensor", [bimp, bblk[jp]], [bimp], out=I, in0=I, scalar=1.0, in1=le[jp][:, 0:NBj], op0=ALU.add,
            op1=ALU.mult)
        c.v("tensor_scalar", [bimp], [bimp], out=I, in0=I, scalar1=-1.0, scalar2=None, op0=ALU.add)
        c.v("max", [bimp], [bimp], out=v8a[:, :], in_=I)
        c.v("match_replace", [bimp], [bimp], out=work[:, 0:NBj], in_to_replace=v8a[:, :], in_values=I, imm_value=-2.0)
        c.v("max", [bimp], [bimp], out=v8b[:, :], in_=work[:, 0:NBj])
        c.v("tensor_scalar", [bimp], [bimp], out=Tm, in0=I, scalar1=v8b[:, 7:8], scalar2=None, op0=ALU.is_ge)
        c.v("tensor_tensor", [bimp, bblk[jp]], [bimp], out=Tm, in0=Tm, in1=le[jp][:, 0:NBj], op=ALU.mult)
        c.v("tensor_scalar", [bimp], [bMN[p]], out=MN[p][:, 64:64 + NBj], in0=Tm, scalar1=-1.0, scalar2=-NEGM, op0=ALU.add,
            op1=ALU.mult)

    def tr_bank(i):
        t, b = SC[i % 2]
        return t[:, 0:64].bitcast(BF16), b

    def phase1b(k):
        j, hk = divmod(k, 2)
        p = k % 2
        NSj, NBj, NGj = dims(j)
        for G in range(NGj):
            tv, tb = tr_bank(G)
            c.tr(tv, MN[p][:, 64 * G:64 * G + 128], identb[:, :], [bMN[p], bres], [tb])
            for g in range(4):
                if g % 2 == 0:
                    c.v("tensor_copy", [tb], [bQM[p][G]], out=QM[p][G][64:128, g, :], in_=tv[64:128, :])
                else:
                    c.act(QM[p][G][64:128, g, :], tv[64:128, :], AF.Copy, [tb], [bQM[p][G]])
        first = True
        for g in range(4):
            for ch in range(NSj // 128):
                i = cnt["emT"]; cnt["emT"] += 1
                tv, tb = tr_bank(i)
                c.tr(tv, em[p][g][:, ch * 128:(ch + 1) * 128], identb[:, :], [bem[p][g], bres], [tb])
                et = emT[i % 3]; bet = bemT[i % 3]
                if i % 2 == 0:
                    c.v("tensor_copy", [tb], [bet], out=et[:, :], in_=tv)
                else:
                    c.act(et[:, :], tv, AF.Copy, [tb], [bet])
                last = (g == 3 and ch == NSj // 128 - 1)
                c.S.op("tensor", (lambda e, et=et, g=g, ch=ch, first=first, last=last: e.matmul(
                    OC[0][:, g * 64:(g + 1) * 64], lhsT=et[:, :], rhs=vcS[:, ch, hk * 64:(hk + 1) * 64], start=first,
                    stop=last, skip_group_check=True)), [bet, bres], [OC[1]])
                first = False

    def attend(kT_of, v_of, ntiles, qrhs_of, mask_of, Obank, reads_k):
        first = True
        for i in range(ntiles):
            si = cnt["st"]; cnt["st"] += 1
            st, stb = ST[si % 3]
            msk = mask_of(i)
            rhs, brhs = qrhs_of(i)
            c.mm(st[:, 0:512], kT_of(i), rhs, True, msk is None, reads_k + [brhs], [stb])
            if msk is not None:
                c.mm(st[:, 0:512], identb[:, :], msk, False, True, [bres], [stb])
            pt = PT[si % 3]; bpt = bPT[si % 3]
            c.act(pt[:, :], st[:, 0:512], AF.Exp, [stb], [bpt])
            for g in range(4):
                last = (i == ntiles - 1 and g == 3)
                c.S.op("tensor", (lambda e, pt=pt, g=g, i=i, first=first, last=last: e.matmul(
                    Obank[0][:, g * 65:(g + 1) * 65], lhsT=pt[:, g * 128:(g + 1) * 128], rhs=v_of(i), start=first,
                    stop=last, skip_group_check=True)), [bpt] + reads_k, [Obank[1]])
                first = False

    def phase2(k):
        j, hk = divmod(k, 2)
        p = k % 2
        jp = j % 2
        NSj, NBj, NGj = dims(j)
        NTj = 4 * j + 4
        flat = lambda t: t[:, :, :].rearrange("p g q -> p (g q)")
        attend(lambda i: KA[hk][:, i * 128:(i + 1) * 128], lambda i: VS[hk][:, i, :], NTj,
               lambda i: (flat(QM[p][i // 32]), bQM[p][i // 32]),
               lambda i: (flat4(sdm, i - 4 * j) if i >= 4 * j else None), OS, [bres])
        c.load(KwB[p][:, :], KwBd[j, hk], [bKw[p]])
        c.load(VwB[p][:, :, :], VwBd[j, hk].rearrange("(n p) f -> p n f", p=128), [bKw[p]])
        attend(lambda i: KwB[p][:, i * 128:(i + 1) * 128], lambda i: VwB[p][:, i, :], 5,
               lambda i: (flat(QP[p]), bQP[p]),
               lambda i: (flat4(wmask, 0) if i == 0 else (flat4(wmask, 1) if i == 4 else None)), OW, [bKw[p]])
        if DBG_K == k:
            dbgt = c.sb([128, 1024]); bdbg = Buf()
            dbg = c.dout("dbg", [128, 1024])
            c.v("tensor_copy", [OC[1]], [bdbg], out=dbgt[:, 0:256], in_=OC[0][:, 0:256])
            c.v("tensor_copy", [OS[1]], [bdbg], out=dbgt[:, 256:516], in_=OS[0][:, 0:260])
            c.v("tensor_copy", [OW[1]], [bdbg], out=dbgt[:, 516:776], in_=OW[0][:, 0:260])
            c.v("tensor_copy", [brs[p]], [bdbg], out=dbgt[:, 776:780], in_=rinv[p][:, :])
            c.v("tensor_copy", [bMN[p]], [bdbg], out=dbgt[:, 780:780 + NBj], in_=MN[p][:, 64:64 + NBj])
            c.v("tensor_copy", [bQM[p][0]], [bdbg], out=dbgt[:, 900:1024], in_=QM[p][0][:, 0, 0:124])
            c.store(dbg[:, :], dbgt[:, :], [bdbg])
        g3 = gt[jp][:, hk * 12:(hk + 1) * 12].rearrange("p (g b) -> p g b", b=3)
        c.v("tensor_tensor", [brs[p], bgt[jp]], [bcoef], out=coef[:, 0, :], in0=rinv[p][:, :], in1=g3[:, :, 0], op=ALU.mult)
        for bi, Ob in ((1, OS), (2, OW)):
            dv = Ob[0][:, 0:260].rearrange("p (g f) -> p g f", f=65)[:, :, 64]
            c.v("tensor_copy", [Ob[1]], [bcoef], out=den[:, bi - 1, :], in_=dv)
            c.v("reciprocal", [bcoef], [bcoef], out=den[:, bi - 1, :], in_=den[:, bi - 1, :])
            c.v("tensor_tensor", [bcoef, bgt[jp]], [bcoef], out=coef[:, bi, :], in0=den[:, bi - 1, :], in1=g3[:, :, bi],
                op=ALU.mult)
        for g in range(4):
            dst = oacc[jp][:, hk * 4 + g, :]
            c.v("tensor_scalar", [OC[1], bcoef], [boacc[jp]], out=dst, in0=OC[0][:, g * 64:(g + 1) * 64],
                scalar1=coef[:, 0, g:g + 1], scalar2=None, op0=ALU.mult)
            c.v("scalar_tensor_tensor", [OS[1], bcoef, boacc[jp]], [boacc[jp]], out=dst, in0=OS[0][:, g * 65:g * 65 + 64],
                scalar=coef[:, 1, g:g + 1], in1=dst, op0=ALU.mult, op1=ALU.add)
            c.v("scalar_tensor_tensor", [OW[1], bcoef, boacc[jp]], [boacc[jp]], out=dst, in0=OW[0][:, g * 65:g * 65 + 64],
                scalar=coef[:, 2, g:g + 1], in1=dst, op0=ALU.mult, op1=ALU.add)
        if hk == 1:
            c.act(ob[:, :], oacc[jp][:, :, :].rearrange("p h d -> p (h d)"), AF.Copy, [boacc[jp]], [bob])
            for ch in range(4):
                tv, tb = tr_bank(ch)
                c.tr(tv, ob[:, ch * 128:(ch + 1) * 128], identb[:, :], [bob, bres], [tb])
                c.v("tensor_copy", [tb], [boTt[jp]], out=oTt[jp][:, ch, :], in_=tv)
            c.store(oT[:, j * 128:(j + 1) * 128].rearrange("(c p) t -> p c t", p=128), oTt[jp][:, :, :], [boTt[jp]])

    def flat4(t, r):
        return t[:, r, :, :].rearrange("p g q -> p (g q)")

    NK = NB * 2
    if stop <= 1:
        return c.done()
    phase1a(0)
    if stop <= 2:
        return c.done()
    phase1b(0)
    if stop <= 3:
        return c.done()
    if stop <= 4:
        phase2(0)
        return c.done()
    for k in range(NK):
        if stop >= 10 and k >= stop - 10:
            break
        if k + 1 < NK:
            phase1a(k + 1)
        phase2(k)
        if k + 1 < NK:
            phase1b(k + 1)
    return c.done()


def s3_inputs(s1a_res, S):
    nblk = S // BLK
    NB = nblk // 4
    NSLOT = S // 16
    full = {}
    for b in range(2):
        Ks = np.zeros((128, S), NPBF); Kw = np.zeros((128, S), NPBF)
        Vs_ = np.zeros((S, 128), NPBF); Vw_ = np.zeros((S, 128), NPBF)
        kc = np.zeros((2, 64, NSLOT), NPBF); vc = np.zeros((NSLOT, 128), NPBF)
        for cp in range(4):
            core = b * 4 + cp
            _, blocks = core_block_ids(S, core)
            r = s1a_res[core]
            for i, n in enumerate(blocks):
                ts = slice(n * 128, (n + 1) * 128); ls = slice(i * 128, (i + 1) * 128)
                Ks[:, ts] = r["KsT"][:, ls]; Kw[:, ts] = r["KwT"][:, ls]
                Vs_[ts] = r["Vs"][ls]; Vw_[ts] = r["Vw"][ls]
                kc[:, :, n * 8:(n + 1) * 8] = r["kcmpT"][:, :, i * 8:(i + 1) * 8]
                vc[n * 8:(n + 1) * 8] = r["vcmp"][i * 8:(i + 1) * 8]
        full[b] = (Ks, Kw, Vs_, Vw_, kc, vc)
    E = np.zeros((64, S), NPBF)
    keyblk = (np.arange(S) // 64) % 64
    E[keyblk, np.arange(S)] = 1
    slot_end = (16 * np.arange(NSLOT) + 15).astype(np.float32); slot_end[0] = 1e9
    slot_end = np.ascontiguousarray(np.broadcast_to(slot_end, (128, NSLOT)))
    blkidx = np.ascontiguousarray(np.broadcast_to(np.arange(S // 64, dtype=np.float32), (128, S // 64)))
    kk = np.arange(128)[:, None]; qq = np.arange(128)[None, :]
    tri = np.where(kk > qq, NEGM, 0.0).astype(NPBF)
    wm0 = np.where(kk <= qq, NEGM, 0.0).astype(NPBF)
    wmask = np.stack([wm0, tri])
    identb = np.eye(128, dtype=np.float32).astype(NPBF)
    maps = []
    for core in range(8):
        b, blocks = core_block_ids(S, core)
        cp = core % 4
        Ks, Kw, Vs_, Vw_, kc, vc = full[b]
        KA = np.zeros((2, 128, S), NPBF); VsA = np.zeros((2, S, 65), NPBF)
        for hk in range(2):
            KA[hk, 0:64] = Ks[hk * 64:(hk + 1) * 64]; KA[hk, 64:128] = E
            VsA[hk, :, 0:64] = Vs_[:, hk * 64:(hk + 1) * 64]; VsA[hk, :, 64] = 1
        KwB = np.zeros((NB, 2, 128, 640), NPBF); VwB = np.zeros((NB, 2, 640, 65), NPBF)
        for i, n in enumerate(blocks):
            lo = n * 128 - 512
            s0 = max(lo, 0)
            for hk in range(2):
                KwB[i, hk, 0:64, s0 - lo:] = Kw[hk * 64:(hk + 1) * 64, s0:n * 128 + 128]
                VwB[i, hk, s0 - lo:, 0:64] = Vw_[s0:n * 128 + 128, hk * 64:(hk + 1) * 64]
                VwB[i, hk, s0 - lo:, 64] = 1
        kcT = np.zeros((2, 128, NSLOT), NPBF); kcT[:, 0:64] = kc
        t = (np.array(blocks)[None, :] * 128 + np.arange(128)[:, None]).astype(np.float32)
        sdm = np.zeros((4, 128, 128), NPBF); sdm[cp] = tri
        maps.append(dict(QT=s1a_res[core]["QT"], gates=s1a_res[core]["gates"], KA=KA, VsA=VsA, KwB=KwB, VwB=VwB,
                         kcT=kcT, vcA=vc, tq=t, cur=np.floor(t / 64).astype(np.float32),
                         curm1=(np.floor(t / 64) - 1).astype(np.float32), slot_end=slot_end, blkidx=blkidx,
                         sdm=sdm, wmask=wmask, identb=identb))
    return maps


def bcast_load(c, dram_vec, n, buf):
    t = c.sb([128, n])
    c.load(t[:, :], dram_vec[0:1, :].partition_broadcast(128), [buf])
    return t


class LNorm:
    def __init__(self, c, width):
        self.c = c; self.w = width; self.nch = width // 512
        self.stats = c.sb([128, self.nch, 6]); self.mv = c.sb([128, 2]); self.sd = c.sb([128, 1]); self.b = Buf()

    def __call__(self, z, bz, gbc, bbc, bgb, out, bout, eps=1e-5):
        c = self.c
        for i in range(self.nch):
            c.v("bn_stats", [bz], [self.b], out=self.stats[:, i, :], in_=z[:, i * 512:(i + 1) * 512])
        c.v("bn_aggr", [self.b], [self.b], out=self.mv[:, :], in_=self.stats[:, :, :].rearrange("p a b -> p (a b)"))
        c.v("tensor_scalar", [self.b], [self.b], out=self.sd[:, :], in0=self.mv[:, 1:2], scalar1=float(eps), scalar2=None,
            op0=ALU.add)
        c.act(self.sd[:, :], self.sd[:, :], AF.Sqrt, [self.b], [self.b])
        c.v("reciprocal", [self.b], [self.b], out=self.sd[:, :], in_=self.sd[:, :])
        c.v("tensor_scalar", [bz, self.b], [bz], out=z, in0=z, scalar1=self.mv[:, 0:1], scalar2=self.sd[:, 0:1],
            op0=ALU.subtract, op1=ALU.mult)
        c.v("tensor_tensor", [bz, bgb], [bz], eng="gpsimd", out=z, in0=z, in1=gbc, op=ALU.mult)
        c.v("tensor_tensor", [bz, bgb], [bout], out=out, in0=z, in1=bbc, op=ALU.add)


def build_s4a1(NB, c=None):
    c = c or Ctx()
    T = NB * BLK
    TB = min(4, NB)
    NT = NB // TB
    NC = TB * 128
    xin = c.din("xin", [NB * EXT, D])
    wb1d = c.din("wb1", [D, 1536])
    poolwd = c.din("pool_w", [4, 128, 128]); pscd = c.din("pool_scale", [128, 4])
    lngd = c.din("sg_ln_g", [1, 512]); lnbd = c.din("sg_ln_b", [1, 512])
    sgwTd = c.din("sg_wT", [4, 128, 128]); sgbd = c.din("sg_b", [1, 512]); trild = c.din("trilT", [128, 128])
    wpod = c.din("w_pool_out", [512, D]); wsod = c.din("w_sg_out", [512, D])
    invcd = c.din("invc", [128, 64]); identd = c.din("ident", [128, 128])
    ypo = c.dout("ypo", [D, T], BF16); yso = c.dout("yso", [D, T], BF16)
    c.init_psum()
    bw = Buf()
    ident = c.sb([128, 128]); bid = Buf()
    c.load(ident[:, :], identd[:, :], [bid])
    stage = [(c.sb([128, 2048]), Buf()) for _ in range(2)]
    wb1 = c.sb([128, 8, 1536], BF16)
    for k in range(8):
        c.load_cast(wb1[:, k, :], bw, wb1d[k * 128:(k + 1) * 128, :], stage)
    pwb = c.sb([128, 4, 128], BF16)
    c.load_cast(pwb[:, :, :], bw, poolwd.rearrange("g c d -> c g d"), stage)
    psc = c.sb([128, 4]); c.load(psc[:, :], pscd[:, :], [bw])
    wpo = c.sb([128, 4, D], BF16); wso = c.sb([128, 4, D], BF16)
    for g in range(4):
        c.load_cast(wpo[:, g, :], bw, wpod[g * 128:(g + 1) * 128, :], stage)
        c.load_cast(wso[:, g, :], bw, wsod[g * 128:(g + 1) * 128, :], stage)
    lng = bcast_load(c, lngd, 512, bw); lnb = bcast_load(c, lnbd, 512, bw); sgb = bcast_load(c, sgbd, 512, bw)
    tril = c.sb([128, 128]); c.load(tril[:, :], trild[:, :], [bw])
    wsf = c.sb([128, 4, 128]); c.load(wsf[:, :, :], sgwTd.rearrange("g s t -> s g t"), [bw])
    wsT = c.sb([128, 4, 128], BF16)
    for g in range(4):
        c.v("tensor_tensor", [bw], [bw], out=wsT[:, g, :], in0=wsf[:, g, :], in1=tril[:, :], op=ALU.mult)
    invc = c.sb([128, 4, 16]); c.load(invc[:, :, :], invcd.rearrange("p (g t) -> p g t", t=16), [bw])

    xt = [c.sb([128, TB, D]) for _ in range(2)]; bxt = [Buf(), Buf()]
    xh = [c.sb([128, D]) for _ in range(2)]; bxh = [Buf(), Buf()]
    for i in range(2):
        c.v("memset", [], [bxh[i]], eng="gpsimd", ap=xh[i][:, :], constant=0.0)
    xT = [c.sb([128, 8, NC], BF16) for _ in range(2)]; bxT = [Buf(), Buf()]
    xTh = [c.sb([128, 8, 128], BF16) for _ in range(2)]; bxTh = [Buf(), Buf()]
    aext = [c.sb([128, TB, EXT]) for _ in range(4)]; baext = [Buf() for _ in range(4)]
    B1 = c.sb([128, TB, EXT]); B2 = c.sb([128, TB, EXT]); bB1 = Buf(); bB2 = Buf()
    dT = c.sb([128, 4, NC], BF16); bdT = Buf()
    ypT = c.sb([128, 4, NC], BF16); bypT = Buf()
    uT = c.sb([128, 4, NC]); buT = Buf()
    gv = [c.sb([128, 512]) for _ in range(2)]; bgv = [Buf(), Buf()]
    vnb = [c.sb([128, 512], BF16) for _ in range(2)]; bvnb = [Buf(), Buf()]
    mtmp = c.sb([128, 512]); bmtmp = Buf()
    sguT = c.sb([128, 4, NC], BF16); bsgu = Buf()
    outb = [c.sb([128, 8, NC], BF16) for _ in range(2)]; boutb = [Buf(), Buf()]
    fix = c.sb([128, 16]); bfix = Buf()
    ln = LNorm(c, 512)
    v3 = lambda ap: ap.rearrange("p (n e) -> p n e", n=TB)

    for ti in range(NT):
        p = ti % 2
        blk0 = ti * TB
        load_x_tile(c, xin, blk0, TB, xt[p], bxt[p], xh[p], bxh[p])
        transpose_x(c, xt[p], bxt[p], TB, xT[p], bxT[p], ident, bid, xh[p], bxh[p], xTh[p], bxTh[p])
        for g in range(4):
            pt, pb = c.ps()
            for k in range(8):
                c.mm(pt[:, 0:NC], wb1[:, k, g * 128:(g + 1) * 128], xT[p][:, k, :], k == 0, k == 7, [bw, bxT[p]], [pb])
            c.act(aext[g][:, :, HALO:EXT], v3(pt[:, 0:NC]), AF.Copy, [pb], [baext[g]])
            pt, pb = c.ps()
            for k in range(8):
                c.mm(pt[:, 0:16 * TB], wb1[:, k, g * 128:(g + 1) * 128], xTh[p][:, k, 0:16 * TB], k == 0, k == 7,
                     [bw, bxTh[p]], [pb])
            c.v("tensor_copy", [pb], [baext[g]], out=aext[g][:, :, 0:HALO], in_=v3(pt[:, 0:16 * TB]))
            A = aext[g]
            src, bsrc = A, baext[g]
            sh = 1
            for step in range(g + 1):
                dst, bdst = (B1, bB1) if step % 2 == 0 else (B2, bB2)
                lo = 2 * sh - 1
                c.v("tensor_tensor", [bsrc], [bdst], eng="gpsimd", out=dst[:, :, lo:EXT], in0=src[:, :, lo:EXT],
                    in1=src[:, :, lo - sh:EXT - sh], op=ALU.add)
                src, bsrc = dst, bdst
                sh *= 2
            w = 2 ** (g + 1)
            c.v("scalar_tensor_tensor", [bsrc, baext[g]], [bdT], out=v3(dT[:, g, :]), in0=src[:, :, HALO:EXT],
                scalar=1.0 / w, in1=A[:, :, HALO:EXT], op0=ALU.mult, op1=ALU.subtract)
            if ti == 0:
                c.v("tensor_tensor", [bsrc, bw], [bfix], out=fix[:, :], in0=src[:, 0, HALO:HALO + 16], in1=invc[:, g, :],
                    op=ALU.mult)
                c.v("tensor_tensor", [bfix, baext[g]], [bdT], out=dT[:, g, 0:16], in0=fix[:, :], in1=A[:, 0, HALO:HALO + 16],
                    op=ALU.subtract)
            pt, pb = c.ps()
            c.mm(pt[:, 0:NC], pwb[:, g, :], dT[:, g, :], True, True, [bw, bdT], [pb])
            c.act(ypT[:, g, :], pt[:, 0:NC], AF.Copy, [pb, bw], [bypT], scale=psc[:, g:g + 1])
        for dc in range(8):
            pt, pb = c.ps()
            for g in range(4):
                c.mm(pt[:, 0:NC], wpo[:, g, dc * 128:(dc + 1) * 128], ypT[:, g, :], g == 0, g == 3, [bw, bypT], [pb])
            if dc % 2 == 0:
                c.v("tensor_copy", [pb], [boutb[0]], out=outb[0][:, dc, :], in_=pt[:, 0:NC])
            else:
                c.act(outb[0][:, dc, :], pt[:, 0:NC], AF.Copy, [pb], [boutb[0]])
        c.store(ypo[:, blk0 * 128:blk0 * 128 + NC].rearrange("(c p) t -> p c t", p=128), outb[0][:, :, :], [boutb[0]])
        for g in range(4):
            pt, pb = c.ps()
            for k in range(8):
                c.mm(pt[:, 0:NC], wb1[:, k, 512 + g * 128:512 + (g + 1) * 128], xT[p][:, k, :], k == 0, k == 7,
                     [bw, bxT[p]], [pb])
            c.act(uT[:, g, :], pt[:, 0:NC], AF.Gelu_apprx_tanh, [pb], [buT])
        for b in range(TB):
            q = b % 2
            pt, pb = c.ps()
            for k in range(8):
                c.mm(pt[:, 0:512], xT[p][:, k, b * 128:(b + 1) * 128], wb1[:, k, 1024:1536], k == 0, k == 7,
                     [bw, bxT[p]], [pb])
            c.act(gv[q][:, :], pt[:, 0:512], AF.Gelu_apprx_tanh, [pb], [bgv[q]])
            ln(gv[q][:, :], bgv[q], lng[:, :], lnb[:, :], bw, vnb[q][:, :], bvnb[q])
            pt, pb = c.ps()
            for g in range(4):
                c.mm(pt[:, g * 128:(g + 1) * 128], vnb[q][:, g * 128:(g + 1) * 128], wsT[:, g, :], True, True,
                     [bvnb[q], bw], [pb])
            c.v("tensor_tensor", [pb, bw], [bmtmp], out=mtmp[:, :], in0=pt[:, 0:512], in1=sgb[:, :], op=ALU.add)
            c.v("tensor_tensor", [bmtmp, buT], [bsgu], out=sguT[:, :, b * 128:(b + 1) * 128],
                in0=mtmp[:, :].rearrange("p (g t) -> p g t", g=4), in1=uT[:, :, b * 128:(b + 1) * 128], op=ALU.mult)
        for dc in range(8):
            pt, pb = c.ps()
            for g in range(4):
                c.mm(pt[:, 0:NC], wso[:, g, dc * 128:(dc + 1) * 128], sguT[:, g, :], g == 0, g == 3, [bw, bsgu], [pb])
            if dc % 2 == 0:
                c.v("tensor_copy", [pb], [boutb[1]], out=outb[1][:, dc, :], in_=pt[:, 0:NC])
            else:
                c.act(outb[1][:, dc, :], pt[:, 0:NC], AF.Copy, [pb], [boutb[1]])
        c.store(yso[:, blk0 * 128:blk0 * 128 + NC].rearrange("(c p) t -> p c t", p=128), outb[1][:, :, :], [boutb[1]])
    return c.done()


def build_s4a2(NB, moe, c=None):
    c = c or Ctx()
    T = NB * BLK
    TB = min(2, NB)
    NT = NB // TB
    NC = TB * 128
    xin = c.din("xin", [NB * EXT, D])
    oTd = c.din("oT", [512, T], BF16); ypod = c.din("ypo", [D, T], BF16); ysod = c.din("yso", [D, T], BF16)
    wbgd = c.din("wbg", [D, 3072]); wnod = c.din("w_nsa_out", [512, D]); woutd = c.din("w_out", [D, D])
    g1d = c.din("ln1_g", [1, D]); b1d = c.din("ln1_b", [1, D]); identd = c.din("ident", [128, 128])
    x1o = c.dout("x1", [T, D]); x1To = c.dout("x1T", [D, T], BF16)
    if moe:
        wrd = c.din("w_router", [D, 8]); brd = c.din("b_router", [1, 8])
        rwo = c.dout("rw", [T, 8])
    c.init_psum()
    bw = Buf()
    ident = c.sb([128, 128]); bid = Buf()
    c.load(ident[:, :], identd[:, :], [bid])
    stage = [(c.sb([128, 2048]), Buf()) for _ in range(2)]
    wbg = c.sb([128, 8, 3072], BF16)
    for k in range(8):
        for h0 in range(0, 3072, 1536):
            c.load_cast(wbg[:, k, h0:h0 + 1536], bw, wbgd[k * 128:(k + 1) * 128, h0:h0 + 1536], stage)
    wno = c.sb([128, 4, D], BF16)
    for g in range(4):
        c.load_cast(wno[:, g, :], bw, wnod[g * 128:(g + 1) * 128, :], stage)
    wout = c.sb([128, 8, D], BF16)
    for k in range(8):
        c.load_cast(wout[:, k, :], bw, woutd[k * 128:(k + 1) * 128, :], stage)
    g1 = bcast_load(c, g1d, D, bw); b1 = bcast_load(c, b1d, D, bw)
    if moe:
        wr = c.sb([128, 8, 8]); c.load(wr[:, :, :], wrd.rearrange("(k p) e -> p k e", p=128), [bw])
        brb = bcast_load(c, brd, 8, bw)

    xt = [c.sb([128, TB, D]) for _ in range(2)]; bxt = [Buf(), Buf()]
    xT = [c.sb([128, 8, NC], BF16) for _ in range(2)]; bxT = [Buf(), Buf()]
    oTt = [c.sb([128, 4, NC], BF16) for _ in range(2)]; ypt = [c.sb([128, 8, NC], BF16) for _ in range(2)]
    yst = [c.sb([128, 8, NC], BF16) for _ in range(2)]; bin_ = [Buf(), Buf()]
    gsb = [c.sb([128, NC]) for _ in range(3)]; bgsb = [Buf() for _ in range(3)]
    acc = c.sb([128, NC]); bacc = Buf()
    mT = c.sb([128, 8, NC], BF16); bmT = Buf()
    z = [c.sb([128, D]) for _ in range(2)]; bz = [Buf(), Buf()]
    x1 = [c.sb([128, D]) for _ in range(2)]; bx1 = [Buf(), Buf()]
    x1Tb = [c.sb([128, 8, 128], BF16) for _ in range(2)]; bx1T = [Buf(), Buf()]
    x1Tf = c.sb([128, 8, 128]); bx1Tf = Buf()
    lg = c.sb([128, 8]); v8 = c.sb([128, 8]); dl = c.sb([128, 2]); rwt = c.sb([128, 8]); rw2 = c.sb([128, 8]); brw = Buf()
    ln = LNorm(c, D)

    for ti in range(NT):
        p = ti % 2
        blk0 = ti * TB
        cols = slice(blk0 * 128, blk0 * 128 + NC)
        load_x_tile(c, xin, blk0, TB, xt[p], bxt[p], None, None)
        transpose_x(c, xt[p], bxt[p], TB, xT[p], bxT[p], ident, bid)
        c.load(oTt[p][:, :, :], oTd[:, cols].rearrange("(c p) t -> p c t", p=128), [bin_[p]])
        c.load(ypt[p][:, :, :], ypod[:, cols].rearrange("(c p) t -> p c t", p=128), [bin_[p]])
        c.load(yst[p][:, :, :], ysod[:, cols].rearrange("(c p) t -> p c t", p=128), [bin_[p]])
        for dc in range(8):
            for br in range(3):
                pt, pb = c.ps()
                for k in range(8):
                    c.mm(pt[:, 0:NC], wbg[:, k, br * 1024 + dc * 128:br * 1024 + (dc + 1) * 128], xT[p][:, k, :], k == 0,
                         k == 7, [bw, bxT[p]], [pb])
                c.act(gsb[br][:, :], pt[:, 0:NC], AF.Sigmoid, [pb], [bgsb[br]])
            pn, pnb = c.ps()
            for g in range(4):
                c.mm(pn[:, 0:NC], wno[:, g, dc * 128:(dc + 1) * 128], oTt[p][:, g, :], g == 0, g == 3, [bw, bin_[p]], [pnb])
            c.v("tensor_tensor", [bgsb[0], bin_[p]], [bacc], out=acc[:, :], in0=gsb[0][:, :], in1=ypt[p][:, dc, :], op=ALU.mult)
            c.v("tensor_tensor", [bgsb[1], bin_[p]], [bgsb[1]], eng="gpsimd", out=gsb[1][:, :], in0=gsb[1][:, :],
                in1=yst[p][:, dc, :], op=ALU.mult)
            c.v("tensor_tensor", [bgsb[2], pnb], [bgsb[2]], out=gsb[2][:, :], in0=gsb[2][:, :], in1=pn[:, 0:NC], op=ALU.mult)
            c.v("tensor_tensor", [bacc, bgsb[1]], [bacc], eng="gpsimd", out=acc[:, :], in0=acc[:, :], in1=gsb[1][:, :],
                op=ALU.add)
            c.v("tensor_tensor", [bacc, bgsb[2]], [bmT], out=mT[:, dc, :], in0=acc[:, :], in1=gsb[2][:, :], op=ALU.add)
        for b in range(TB):
            q = b % 2
            for half in range(2):
                pt, pb = c.ps()
                for k in range(8):
                    c.mm(pt[:, 0:512], mT[:, k, b * 128:(b + 1) * 128], wout[:, k, half * 512:(half + 1) * 512], k == 0,
                         k == 7, [bw, bmT], [pb])
                c.v("scalar_tensor_tensor", [bxt[p], pb], [bz[q]], out=z[q][:, half * 512:(half + 1) * 512],
                    in0=xt[p][:, b, half * 512:(half + 1) * 512], scalar=float(ALPHA), in1=pt[:, 0:512], op0=ALU.mult,
                    op1=ALU.add)
            ln(z[q][:, :], bz[q], g1[:, :], b1[:, :], bw, x1[q][:, :], bx1[q])
            rows = slice((blk0 + b) * 128, (blk0 + b + 1) * 128)
            c.store(x1o[rows, :], x1[q][:, :], [bx1[q]])
            for k in range(8):
                pt, pb = c.ps()
                c.tr(pt[:, 0:128], x1[q][:, k * 128:(k + 1) * 128], ident[:, :], [bx1[q], bid], [pb])
                c.act(x1Tb[q][:, k, :], pt[:, 0:128], AF.Copy, [pb], [bx1T[q]])
                if moe:
                    c.v("tensor_copy", [pb], [bx1Tf], out=x1Tf[:, k, :], in_=pt[:, 0:128])
            c.store(x1To[:, rows].rearrange("(c p) t -> p c t", p=128), x1Tb[q][:, :, :], [bx1T[q]])
            if moe:
                pt, pb = c.ps()
                for k in range(8):
                    c.mm(pt[:, 0:8], x1Tf[:, k, :], wr[:, k, :], k == 0, k == 7, [bx1Tf, bw], [pb])
                c.v("tensor_tensor", [pb, bw], [brw], out=lg[:, :], in0=pt[:, 0:8], in1=brb[:, :], op=ALU.add)
                c.v("max", [brw], [brw], out=v8[:, :], in_=lg[:, :])
                c.v("tensor_tensor", [brw], [brw], out=dl[:, 0:1], in0=v8[:, 0:1], in1=v8[:, 1:2], op=ALU.subtract)
                c.v("tensor_tensor", [brw], [brw], out=dl[:, 1:2], in0=v8[:, 1:2], in1=v8[:, 0:1], op=ALU.subtract)
                c.act(dl[:, :], dl[:, :], AF.Sigmoid, [brw], [brw])
                c.v("tensor_scalar", [brw], [brw], out=rwt[:, :], in0=lg[:, :], scalar1=v8[:, 0:1], scalar2=dl[:, 0:1],
                    op0=ALU.is_equal, op1=ALU.mult)
                c.v("tensor_scalar", [brw], [brw], out=rw2[:, :], in0=lg[:, :], scalar1=v8[:, 1:2], scalar2=dl[:, 1:2],
                    op0=ALU.is_equal, op1=ALU.mult)
                c.v("tensor_tensor", [brw], [brw], out=rwt[:, :], in0=rwt[:, :], in1=rw2[:, :], op=ALU.add)
                c.store(rwo[rows, :], rwt[:, :], [brw])
    return c.done()


def s4a_inputs(x, oT_res, L, W, S, consts):
    m1, m2 = [], []
    w_in = W["w_in"][L]
    wb1 = np.ascontiguousarray(w_in[:, 0:1536])
    wbg = np.ascontiguousarray(w_in[:, O_BG:O_BG + 3072])
    kk = np.arange(128)[:, None]; tt = np.arange(128)[None, :]
    trilT = (kk <= tt).astype(np.float32)
    sg_wT = np.ascontiguousarray(W["sg_w"][L].transpose(0, 2, 1))
    psc = np.ascontiguousarray(W["pool_scale"][L].reshape(4, 128).T)
    for core in range(8):
        b, blocks = core_block_ids(S, core)
        xin = make_xin(x[b], blocks)
        invc = np.zeros((128, 4, 16), np.float32)
        for g in range(4):
            w = 2 ** (g + 1)
            if blocks[0] == 0:
                invc[:, g, :] = 1.0 / np.minimum(np.arange(1, 17), w)
            else:
                invc[:, g, :] = 1.0 / w
        m1.append(dict(xin=xin, wb1=wb1, pool_w=W["pool_w"][L], pool_scale=psc, sg_ln_g=W["sg_ln_g"][L][None, :],
                       sg_ln_b=W["sg_ln_b"][L][None, :], sg_wT=sg_wT, sg_b=W["sg_b"][L].reshape(1, 512), trilT=trilT,
                       w_pool_out=W["w_pool_out"][L], w_sg_out=W["w_sg_out"][L], invc=invc.reshape(128, 64),
                       ident=consts["ident"]))
        d2 = dict(xin=xin, oT=oT_res[core]["oT"], wbg=wbg, w_nsa_out=W["w_nsa_out"][L], w_out=W["w_out"][L],
                  ln1_g=W["ln1_g"][L][None, :], ln1_b=W["ln1_b"][L][None, :], ident=consts["ident"])
        if L % 2 == 1:
            d2["w_router"] = W["moe_router"][L // 2]; d2["b_router"] = W["moe_router_b"][L // 2][None, :]
        m2.append(d2)
    return m1, m2


def build_s4b(NB, NE, c=None):
    c = c or Ctx()
    T = NB * BLK
    TB = min(4, NB)
    NT = NB // TB
    NC = TB * 128
    NFC = DFF // 128
    x1Td = c.din("x1T", [D, T], BF16)
    rwd = c.din("rw", [T, NE])
    wgd = c.din("wg", [NE, D, DFF]); wud = c.din("wu", [NE, D, DFF]); wdd = c.din("wd", [NE, DFF, D])
    fo = c.dout("f", [T, D])
    c.init_psum()
    x1T = [c.sb([128, 8, NC], BF16) for _ in range(2)]; bx = [Buf(), Buf()]
    rw = [c.sb([128, TB, NE]) for _ in range(2)]
    NW = 3
    wgst = [c.sb([128, 8, 128]) for _ in range(NW)]; bwgst = [Buf() for _ in range(NW)]
    wust = [c.sb([128, 8, 128]) for _ in range(NW)]; bwust = [Buf() for _ in range(NW)]
    wgc = [c.sb([128, 8, 128], BF16) for _ in range(NW)]; bwgc = [Buf() for _ in range(NW)]
    wuc = [c.sb([128, 8, 128], BF16) for _ in range(NW)]; bwuc = [Buf() for _ in range(NW)]
    wdst = [c.sb([128, D]) for _ in range(NW)]; bwdst = [Buf() for _ in range(NW)]
    wdb = [c.sb([128, NFC, D], BF16) for _ in range(2)]; bwdb = [Buf(), Buf()]
    hT = c.sb([128, NFC, NC], BF16); bhT = Buf()
    sg = [c.sb([128, NC]) for _ in range(2)]; bsg = [Buf(), Buf()]
    _f = c.sb([128, TB, D]); _bf = Buf()
    facc = [_f, _f]; bfacc = [_bf, _bf]
    it = 0
    for ti in range(NT):
        p = ti % 2
        cols = slice(ti * NC, (ti + 1) * NC)
        c.load(x1T[p][:, :, :], x1Td[:, cols].rearrange("(c p) t -> p c t", p=128), [bx[p]])
        c.load(rw[p][:, :, :], rwd[cols, :].rearrange("(n p) e -> p n e", p=128), [bx[p]],
               allow_slow_non_contiguous=True)
        for e in range(NE):
            wp = it % 2; it += 1
            for cc in range(NFC):
                i = cc % NW
                fs = slice(cc * 128, (cc + 1) * 128)
                c.load(wgst[i][:, :, :], wgd[e][:, fs].rearrange("(k p) f -> p k f", p=128), [bwgst[i]], q="sync")
                c.load(wust[i][:, :, :], wud[e][:, fs].rearrange("(k p) f -> p k f", p=128), [bwust[i]], q="sync")
                c.load(wdst[i][:, :], wdd[e][fs, :], [bwdst[i]], q="gpsimd")
                c.v("tensor_copy", [bwgst[i]], [bwgc[i]], out=wgc[i][:, :, :], in_=wgst[i][:, :, :])
                c.act(wuc[i][:, :, :], wust[i][:, :, :], AF.Copy, [bwust[i]], [bwuc[i]])
                c.v("tensor_copy", [bwdst[i]], [bwdb[wp]], eng="gpsimd", out=wdb[wp][:, cc, :], in_=wdst[i][:, :])
                pg, pgb = c.ps()
                for k in range(8):
                    c.mm(pg[:, 0:NC], wgc[i][:, k, :], x1T[p][:, k, :], k == 0, k == 7, [bwgc[i], bx[p]], [pgb])
                pu, pub = c.ps()
                for k in range(8):
                    c.mm(pu[:, 0:NC], wuc[i][:, k, :], x1T[p][:, k, :], k == 0, k == 7, [bwuc[i], bx[p]], [pub])
                s = cc % 2
                c.act(sg[s][:, :], pg[:, 0:NC], AF.Silu, [pgb], [bsg[s]])
                c.v("tensor_tensor", [bsg[s], pub], [bhT], out=hT[:, cc, :], in0=sg[s][:, :], in1=pu[:, 0:NC], op=ALU.mult)
            for b in range(TB):
                for half in range(2):
                    pt, pb = c.ps()
                    for cc in range(NFC):
                        c.mm(pt[:, 0:512], hT[:, cc, b * 128:(b + 1) * 128], wdb[wp][:, cc, half * 512:(half + 1) * 512],
                             cc == 0, cc == NFC - 1, [bhT, bwdb[wp]], [pb])
                    dst = facc[p][:, b, half * 512:(half + 1) * 512]
                    if e == 0:
                        c.v("tensor_scalar", [pb, bx[p]], [bfacc[p]], out=dst, in0=pt[:, 0:512], scalar1=rw[p][:, b, e:e + 1],
                            scalar2=None, op0=ALU.mult)
                    else:
                        c.v("scalar_tensor_tensor", [pb, bx[p], bfacc[p]], [bfacc[p]], out=dst, in0=pt[:, 0:512],
                            scalar=rw[p][:, b, e:e + 1], in1=dst, op0=ALU.mult, op1=ALU.add)
        c.store(fo[cols, :].rearrange("(n p) d -> p n d", p=128), facc[p][:, :, :], [bfacc[p]])
    return c.done()


def build_s4c(NB, c=None):
    c = c or Ctx()
    T = NB * BLK
    x1d = c.din("x1", [T, D]); x1Td = c.din("x1T", [D, T], BF16); fd = c.din("f", [T, D]); pd = c.din("p", [T, PLE])
    wpgd = c.din("wpg", [D, D]); bpgd = c.din("bpg", [1, D]); wppd = c.din("wpp", [PLE, D])
    g2d = c.din("ln2_g", [1, D]); b2d = c.din("ln2_b", [1, D]); identd = c.din("ident", [128, 128])
    xo = c.dout("x2", [T, D])
    c.init_psum()
    bw = Buf()
    ident = c.sb([128, 128]); bid = Buf()
    c.load(ident[:, :], identd[:, :], [bid])
    stage = [(c.sb([128, 2048]), Buf()) for _ in range(2)]
    wpg = c.sb([128, 8, D], BF16)
    for k in range(8):
        c.load_cast(wpg[:, k, :], bw, wpgd[k * 128:(k + 1) * 128, :], stage)
    wpp = c.sb([128, 2, D], BF16)
    for k in range(2):
        c.load_cast(wpp[:, k, :], bw, wppd[k * 128:(k + 1) * 128, :], stage)
    bpg = bcast_load(c, bpgd, D, bw); g2 = bcast_load(c, g2d, D, bw); b2 = bcast_load(c, b2d, D, bw)
    x1 = [c.sb([128, D]) for _ in range(2)]; f = [c.sb([128, D]) for _ in range(2)]; pt_ = [c.sb([128, PLE]) for _ in range(2)]
    x1T = [c.sb([128, 8, 128], BF16) for _ in range(2)]; bin_ = [Buf(), Buf()]
    pT = [c.sb([128, 2, 128], BF16) for _ in range(2)]; bpT = [Buf(), Buf()]
    gate = [c.sb([128, D]) for _ in range(2)]; bgate = [Buf(), Buf()]
    z = [c.sb([128, D]) for _ in range(2)]; bz = [Buf(), Buf()]
    out = [c.sb([128, D]) for _ in range(2)]; bout = [Buf(), Buf()]
    ln = LNorm(c, D)
    for j in range(NB):
        q = j % 2
        rows = slice(j * 128, (j + 1) * 128)
        c.load(x1[q][:, :], x1d[rows, :], [bin_[q]]); c.load(f[q][:, :], fd[rows, :], [bin_[q]])
        c.load(pt_[q][:, :], pd[rows, :], [bin_[q]])
        c.load(x1T[q][:, :, :], x1Td[:, rows].rearrange("(c p) t -> p c t", p=128), [bin_[q]])
        for k in range(2):
            tp, tb = c.ps()
            c.tr(tp[:, 0:128], pt_[q][:, k * 128:(k + 1) * 128], ident[:, :], [bin_[q], bid], [tb])
            c.v("tensor_copy", [tb], [bpT[q]], out=pT[q][:, k, :], in_=tp[:, 0:128])
        for half in range(2):
            hs = slice(half * 512, (half + 1) * 512)
            pg, pgb = c.ps()
            for k in range(8):
                c.mm(pg[:, 0:512], x1T[q][:, k, :], wpg[:, k, hs], k == 0, k == 7, [bin_[q], bw], [pgb])
            c.v("tensor_tensor", [pgb, bw], [bgate[q]], out=gate[q][:, hs], in0=pg[:, 0:512], in1=bpg[:, hs], op=ALU.add)
            c.act(gate[q][:, hs], gate[q][:, hs], AF.Sigmoid, [bgate[q]], [bgate[q]])
            pp, ppb = c.ps()
            for k in range(2):
                c.mm(pp[:, 0:512], pT[q][:, k, :], wpp[:, k, hs], k == 0, k == 1, [bpT[q], bw], [ppb])
            c.v("tensor_tensor", [bgate[q], ppb], [bgate[q]], out=gate[q][:, hs], in0=gate[q][:, hs], in1=pp[:, 0:512],
                op=ALU.mult)
        c.v("scalar_tensor_tensor", [bin_[q]], [bz[q]], out=z[q][:, :], in0=x1[q][:, :], scalar=float(ALPHA),
            in1=f[q][:, :], op0=ALU.mult, op1=ALU.add)
        c.v("tensor_tensor", [bz[q], bgate[q]], [bz[q]], out=z[q][:, :], in0=z[q][:, :], in1=gate[q][:, :], op=ALU.add)
        ln(z[q][:, :], bz[q], g2[:, :], b2[:, :], bw, out[q][:, :], bout[q])
        c.store(xo[rows, :], out[q][:, :], [bout[q]])
    return c.done()


_PROGS = {}


def _prog(key, fn):
    if key not in _PROGS:
        _PROGS[key] = fn()
    return _PROGS[key]


def _run(nc, maps):
    return run_bass_kernel_spmd(nc, maps, core_ids=list(range(8))).results


def build_B(NB, S, moe):
    c = Ctx(chained=True)
    build_s3(NB, S, c=c)
    build_s4a1(NB, c=c)
    build_s4a2(NB, moe, c=c)
    build_s4b(NB, NEXP if moe else 1, c=c)
    build_s4c(NB, c=c)
    names = list(c.in_names)
    return c.finish(), names


def forward(inp, S):
    W = {k: np.asarray(v) for k, v in inp.items()}
    x = np.ascontiguousarray(W["x"], dtype=np.float32)
    positions = np.asarray(W["positions"]).astype(np.int32)
    consts = make_consts()
    NB = S // BLK // 4
    for L in range(DEPTH):
        moe = (L % 2 == 1)
        r1 = _run(_prog(("s1a", NB), lambda: build_s1a(NB)), s1a_inputs(x, positions, L, W, S, consts))
        m3 = s3_inputs(r1, S)
        m1, m2 = s4a_inputs(x, [dict(oT=None)] * 8, L, W, S, consts)
        del r1
        j = L // 2
        if moe:
            wg, wu, wd = W["moe_w_gate"][j], W["moe_w_up"][j], W["moe_w_down"][j]
        else:
            wg, wu, wd = W["ffn_w_gate"][j][None], W["ffn_w_up"][j][None], W["ffn_w_down"][j][None]
        ncB, names = _prog(("B", NB, S, moe), lambda: build_B(NB, S, moe))
        maps = []
        for core in range(8):
            b, blocks = core_block_ids(S, core)
            tok = np.concatenate([np.arange(n * BLK, (n + 1) * BLK) for n in blocks])
            full = {}
            full.update(m3[core]); full.update(m1[core]); full.update(m2[core])
            full.update(dict(wg=wg, wu=wu, wd=wd, rw=np.ones((NB * BLK, 1), np.float32),
                             p=np.ascontiguousarray(W["p"][L][b][tok]), wpg=W["ple_gate_w"][L],
                             bpg=W["ple_gate_b"][L][None, :], wpp=W["ple_proj"][L], ln2_g=W["ln2_g"][L][None, :],
                             ln2_b=W["ln2_b"][L][None, :]))
            maps.append({k: full[k] for k in names})
        del m1, m2, m3
        rc = _run(ncB, maps)
        del maps
        xn = np.empty_like(x)
        for core in range(8):
            b, blocks = core_block_ids(S, core)
            for i, n in enumerate(blocks):
                xn[b, n * BLK:(n + 1) * BLK] = rc[core]["x2"][i * BLK:(i + 1) * BLK]
        x = xn
        del rc
    return x


def kernel(**inputs):
    S = int(np.asarray(inputs["x"]).shape[1])
    return forward(inputs, S).astype(np.float32)
```

```python
import contextlib
import numpy as np
import ml_dtypes
import concourse.bass as bass
import concourse.mybir as mybir
from concourse.bass_utils import run_bass_kernel_spmd

F32 = mybir.dt.float32
BF16 = mybir.dt.bfloat16
I32 = mybir.dt.int32
AF = mybir.ActivationFunctionType
ALU = mybir.AluOpType
AX = mybir.AxisListType
NPBF = ml_dtypes.bfloat16

D = 1024
POOL_W = 512
SG_W = 512
QW = 512
KVW = 128
HD = 64
DFF = 2816
NEXP = 8
PLE = 256
DEPTH = 2
ALPHA = (2 * DEPTH) ** 0.25
HALO = 16
BLK = 128
EXT = BLK + HALO
NEGM = -30000.0
WIDTHS = (512, 512, 512, 512, 128, 128, 128, 128, 128, 128, 24, 3072)
OFFS = np.concatenate([[0], np.cumsum(WIDTHS)]).astype(int)
(O_A, O_U, O_V, O_Q, O_KC, O_VC, O_KS, O_VS, O_KW, O_VW, O_NG, O_BG) = [int(v) for v in OFFS[:-1]]

COMPUTE = ("tensor", "vector", "scalar", "gpsimd")
SAME_ENGINE_SYNC = True


class Buf:
    __slots__ = ("name", "w", "r", "rd", "psum")

    def __init__(self, name="", psum=False):
        self.name = name
        self.psum = psum
        self.w = None
        self.r = {}
        self.rd = []


class Op:
    __slots__ = ("id", "eng", "fn", "deps", "dma", "tok", "needed")

    def __init__(self, id, eng, fn, deps, dma):
        self.id = id; self.eng = eng; self.fn = fn; self.deps = deps; self.dma = dma
        self.tok = None; self.needed = False


class Sched:
    def __init__(self, nc, es_global):
        self.nc = nc
        self.n_dma_sems = {"sync": 10, "gpsimd": 8, "scalar": 4}
        self.sems = {e: es_global.enter_context(nc.semaphore("s_" + e)) for e in COMPUTE}
        self.dsems = {q: [es_global.enter_context(nc.semaphore(f"d_{q}{i}")) for i in range(n)]
                      for q, n in self.n_dma_sems.items()}
        self.cnt = {e: 0 for e in COMPUTE}
        self.dcnt = {q: [0] * n for q, n in self.n_dma_sems.items()}
        self.drr = {q: 0 for q in self.n_dma_sems}
        self.bar_cnt = {e: 0 for e in COMPUTE}
        self.bar_dcnt = {q: [0] * n for q, n in self.n_dma_sems.items()}
        self._reset()

    def _reset(self):
        self.ops = []
        self.per_eng = {e: [] for e in ("tensor", "vector", "scalar", "gpsimd", "sync")}

    def op(self, eng, fn, reads=(), writes=(), dma=False):
        deps = set()
        oid = len(self.ops)
        px = [b for b in reads if b.psum and b not in writes]
        if px:
            writes = list(writes) + px
            reads = [b for b in reads if not b.psum]
        for b in reads:
            if b.w is not None:
                deps.add(b.w)
        for b in writes:
            if b.w is not None:
                deps.add(b.w)
            deps.update(b.r.values())
            deps.update(b.rd)
        for b in reads:
            if dma:
                b.rd.append(oid)
            else:
                b.r[eng] = oid
        for b in writes:
            b.w = oid
            b.r = {}
            b.rd = []
        deps.discard(oid)
        o = Op(oid, eng, fn, sorted(deps), dma)
        self.ops.append(o)
        self.per_eng[eng].append(o)
        return o

    def dma(self, q, out_ap, in_ap, reads=(), writes=(), **kw):
        return self.op(q, lambda e: e.dma_start(out=out_ap, in_=in_ap, **kw), reads, writes, dma=True)

    def flush(self):
        nc = self.nc
        ops = self.ops
        if not ops:
            return
        for o in ops:
            for d in o.deps:
                p = ops[d]
                if p.dma:
                    continue
                if p.eng != o.eng or o.dma or (SAME_ENGINE_SYNC and o.eng != "tensor"):
                    p.needed = True
        for e in COMPUTE:
            lst = [o for o in self.per_eng[e] if not o.dma]
            if lst:
                lst[-1].needed = True
        sems, dsems, cnt, dcnt, drr = self.sems, self.dsems, self.cnt, self.dcnt, self.drr
        prev_on_sem = {}
        for o in ops:
            if o.dma:
                j = drr[o.eng]; drr[o.eng] = (j + 1) % len(dsems[o.eng])
                dcnt[o.eng][j] += 16
                key = (o.eng, j)
                o.tok = (dsems[o.eng][j], dcnt[o.eng][j], key, prev_on_sem.get(key))
                prev_on_sem[key] = o.id
            elif o.needed:
                cnt[o.eng] += 1
                o.tok = (sems[o.eng], cnt[o.eng], o.eng, None)
        bar_cnt, bar_dcnt = self.bar_cnt, self.bar_dcnt

        def emit(engname, eng):
            seen = {}
            for F in COMPUTE:
                if F != engname and bar_cnt[F] > 0:
                    eng.wait_ge(sems[F], bar_cnt[F]); seen[F] = bar_cnt[F]
            for q, vals in bar_dcnt.items():
                for j, v in enumerate(vals):
                    if v > 0:
                        eng.wait_ge(dsems[q][j], v); seen[(q, j)] = v
            for o in self.per_eng[engname]:
                need = {}
                for d in o.deps:
                    p = ops[d]
                    if (not p.dma) and p.eng == o.eng and not o.dma and not (SAME_ENGINE_SYNC and o.eng != "tensor"):
                        continue
                    sem, val, key, _ = p.tok
                    if need.get(key, (None, 0))[1] < val:
                        need[key] = (sem, val)
                if o.dma and o.tok[3] is not None:
                    sem, val, key, _ = ops[o.tok[3]].tok
                    if need.get(key, (None, 0))[1] < val:
                        need[key] = (sem, val)
                for key, (sem, val) in need.items():
                    if seen.get(key, 0) < val:
                        eng.wait_ge(sem, val)
                        seen[key] = val
                ins = o.fn(eng)
                if o.dma:
                    ins.then_inc(o.tok[0], 16)
                elif o.needed:
                    ins.then_inc(o.tok[0], 1)
            if engname == "sync":
                for q, vals in dcnt.items():
                    for j, v in enumerate(vals):
                        if v > 0 and seen.get((q, j), 0) < v:
                            eng.wait_ge(dsems[q][j], v); seen[(q, j)] = v

        with contextlib.ExitStack() as bes:
            block = bes.enter_context(nc.Block())

            @block.tensor
            def _(e):
                emit("tensor", e)

            @block.vector
            def _(e):
                emit("vector", e)

            @block.scalar
            def _(e):
                emit("scalar", e)

            @block.gpsimd
            def _(e):
                emit("gpsimd", e)

            @block.sync
            def _(e):
                emit("sync", e)
        self.bar_cnt = dict(cnt)
        self.bar_dcnt = {q: list(v) for q, v in dcnt.items()}
        self._reset()


class Ctx:
    def __init__(self, name="k", chained=False):
        self.nc = bass.Bass("TRN2", target_bir_lowering=False)
        self.ges = contextlib.ExitStack()
        self.S = Sched(self.nc, self.ges)
        self.es = contextlib.ExitStack()
        self.outs = []
        self._n = 0
        self.psum = []
        self._pi = 0
        self._dq = 0
        self.chained = chained
        self.dram = {}
        self.in_names = []

    def din(self, name, shape, dt=F32):
        if name in self.dram:
            ap = self.dram[name]
            assert list(ap.shape) == list(shape), (name, ap.shape, shape)
            return ap
        ap = self.nc.dram_tensor(name, list(shape), dt, kind="ExternalInput").ap()
        self.dram[name] = ap
        self.in_names.append(name)
        return ap

    def dout(self, name, shape, dt=F32):
        ap = self.nc.dram_tensor(name, list(shape), dt, kind="ExternalOutput").ap()
        self.dram[name] = ap
        return ap

    def end_stage(self):
        self.S.flush()
        self.es.close()
        self.es = contextlib.ExitStack()
        self.psum = []
        self._pi = 0
        self.outs = []

    def done(self):
        if self.chained:
            self.end_stage()
            return self
        return self.finish()

    def sb(self, shape, dt=F32, name=None, es=None):
        self._n += 1
        t = (es or self.es).enter_context(self.nc.sbuf_tensor(name or f"t{self._n}", list(shape), dt))
        return t

    def init_psum(self, n=8):
        for i in range(n):
            self._n += 1
            t = self.es.enter_context(self.nc.psum_tensor(f"ps{i}_{self._n}", [128, 512], F32))
            self.psum.append((t, Buf(f"ps{i}", psum=True)))

    def ps(self):
        p = self.psum[self._pi % len(self.psum)]
        self._pi += 1
        return p

    def q(self):
        self._dq += 1
        return ("sync", "gpsimd")[self._dq % 2]

    def mm(self, out, lhsT, rhs, start, stop, reads, writes):
        return self.S.op("tensor", lambda e: e.matmul(out, lhsT=lhsT, rhs=rhs, start=start, stop=stop),
                         reads, writes)

    def tr(self, out, in_, ident, reads, writes):
        return self.S.op("tensor", lambda e: e.transpose(out, in_, ident), reads, writes)

    def act(self, out, in_, func, reads, writes, **kw):
        return self.S.op("scalar", lambda e: e.activation(out=out, in_=in_, func=func, **kw), reads, writes)

    def v(self, meth, reads, writes, eng="vector", **kw):
        return self.S.op(eng, lambda e: getattr(e, meth)(**kw), reads, writes)

    def load(self, out, in_, writes, reads=(), q=None, **kw):
        return self.S.dma(q or self.q(), out, in_, reads=reads, writes=writes, **kw)

    def store(self, out, in_, reads, q=None, writes=(), **kw):
        o = self.S.dma(q or self.q(), out, in_, reads=reads, writes=writes, **kw)
        self.outs.append(o)
        return o

    def finish(self):
        self.end_stage()
        self.ges.close()
        return self.nc

    def load_cast(self, dst, dst_buf, src, stage_pool, eng_cycle=("vector", "gpsimd")):
        st, sbuf = stage_pool[self._n % len(stage_pool)]
        self._n += 1
        shp = list(dst.shape)
        view = st
        n = 1
        for s in shp[1:]:
            n *= s
        sv = st[0:shp[0], 0:n]
        if len(shp) == 3:
            sv = sv.rearrange("p (a b) -> p a b", b=shp[2])
        self.load(sv, src, writes=[sbuf])
        eng = eng_cycle[self._n % len(eng_cycle)]
        self.v("tensor_copy", [sbuf], [dst_buf], eng=eng, out=dst, in_=sv)


def make_consts():
    ident = np.eye(128, dtype=np.float32)
    perm = np.zeros((128, 128), np.float32)
    for hh in range(2):
        for d in range(64):
            if d < 8:
                perm[hh * 64 + d + 8, hh * 64 + d] = 1
            elif d < 16:
                perm[hh * 64 + d - 8, hh * 64 + d] = 1
            else:
                perm[hh * 64 + d, hh * 64 + d] = 1
    half = 8
    inv = (500000.0 ** (-np.arange(half, dtype=np.float32) / half)).astype(np.float32)
    invf = np.zeros((128, 1), np.float32)
    sgn = np.zeros((128, 1), np.float32)
    for hh in range(2):
        for d in range(16):
            invf[hh * 64 + d, 0] = inv[d % 8]
            sgn[hh * 64 + d, 0] = -1.0 if d < 8 else 1.0
    return dict(ident=ident, perm=perm, invf=invf, sgn=sgn)


def load_x_tile(c, xin, blk0, nb, xt, bxt, xh, bxh):
    for b in range(nb):
        r0 = (blk0 + b) * EXT
        c.load(xt[:, b, :], xin[r0 + HALO:r0 + EXT, :], writes=[bxt])
        if xh is not None:
            c.load(xh[16 * b:16 * b + 16, :], xin[r0:r0 + HALO, :], writes=[bxh])


def transpose_x(c, xt, bxt, nb, xT, bxT, ident, bid, xh=None, bxh=None, xTh=None, bxTh=None):
    for k in range(8):
        pt, pb = c.ps()
        for b in range(nb):
            c.tr(pt[:, b * 128:(b + 1) * 128], xt[:, b, k * 128:(k + 1) * 128], ident[:, :], [bxt, bid], [pb])
        if k % 2 == 0:
            c.v("tensor_copy", [pb], [bxT], out=xT[:, k, 0:nb * 128], in_=pt[:, 0:nb * 128])
        else:
            c.act(xT[:, k, 0:nb * 128], pt[:, 0:nb * 128], AF.Copy, [pb], [bxT])
        if xh is not None:
            pt, pb = c.ps()
            c.tr(pt[:, 0:128], xh[:, k * 128:(k + 1) * 128], ident[:, :], [bxh, bid], [pb])
            c.v("tensor_copy", [pb], [bxTh], out=xTh[:, k, 0:16 * nb], in_=pt[:, 0:16 * nb])


WA_COLS = 1816
A_Q, A_KC, A_VC, A_KS, A_KW, A_VS, A_VW, A_NG = 0, 1024, 1152, 1280, 1408, 1536, 1664, 1792


def build_s1a(NB, stop=99, c=None):
    c = c or Ctx()
    nc = c.nc
    T = NB * BLK
    TB = min(2, NB)
    NT = NB // TB
    NS = NB * 8
    xin = c.din("xin", [NB * EXT, D])
    pos = c.din("pos", [1, NB * EXT], I32)
    wa = c.din("wa", [D, WA_COLS])
    kposT = c.din("kposT", [64, 32]); kw1 = c.din("kw1", [2048, 256]); kw2 = c.din("kw2", [256, 64])
    vposT = c.din("vposT", [64, 32]); vw1 = c.din("vw1", [2048, 256]); vw2 = c.din("vw2", [256, 64])
    identd = c.din("ident", [128, 128]); permd = c.din("perm", [128, 128])
    invfd = c.din("invf", [128, 1]); sgnd = c.din("sgn", [128, 1])
    QT = c.dout("QT", [8, 64, T], BF16)
    gates = c.dout("gates", [T, 24])
    KsT = c.dout("KsT", [128, T], BF16); KwT = c.dout("KwT", [128, T], BF16)
    Vs = c.dout("Vs", [T, 128], BF16); Vw = c.dout("Vw", [T, 128], BF16)
    kcmpT = c.dout("kcmpT", [2, 64, NS], BF16); vcmp = c.dout("vcmp", [NS, 128], BF16)
    c.init_psum()

    ident = c.sb([128, 128]); bid = Buf()
    c.load(ident[:, :], identd[:, :], [bid])
    stage = [(c.sb([128, 2048]), Buf()) for _ in range(1)]
    permb = c.sb([128, 128], BF16); bperm = Buf()
    c.load_cast(permb[:, :], bperm, permd[:, :], stage)
    invf = c.sb([128, 1]); sgn = c.sb([128, 1]); bsm = Buf()
    c.load(invf[:, :], invfd[:, :], [bsm]); c.load(sgn[:, :], sgnd[:, :], [bsm])
    wab = c.sb([128, 8, WA_COLS], BF16); bwa = Buf()
    for k in range(8):
        c.load_cast(wab[:, k, :], bwa, wa[k * 128:(k + 1) * 128, :], stage)
    w1b = {}; w2b = {}; pTb = {}; bcw = Buf()
    for nm, w1d, w2d, pd in (("k", kw1, kw2, kposT), ("v", vw1, vw2, vposT)):
        w1b[nm] = c.sb([128, 32, 256], BF16)
        w1v = w1d.rearrange("(l d) h -> d l h", d=64)
        for half in range(2):
            for l0 in range(0, 32, 8):
                c.load_cast(w1b[nm][half * 64:half * 64 + 64, l0:l0 + 8, :], bcw, w1v[:, l0:l0 + 8, :], stage)
        w2b[nm] = c.sb([128, 2, 64], BF16)
        c.load_cast(w2b[nm][:, :, :], bcw, w2d.rearrange("(c p) d -> p c d", p=128), stage)
        pTb[nm] = c.sb([128, 32], BF16)
        c.v("memset", [], [bcw], ap=pTb[nm][:, :], constant=0.0)
        c.load_cast(pTb[nm][0:64, :], bcw, pd[:, :], stage)
    if stop <= 0:
        return c.done()
    NP = NB * EXT
    Ct, bC, St, bS = rope_tables(c, pos, NP, invf, sgn, bsm)
    C3 = Ct[:, :].rearrange("p (n e) -> p n e", e=EXT)
    S3 = St[:, :].rearrange("p (n e) -> p n e", e=EXT)
    if stop <= 1:
        return c.done()
    kcx = [c.sb([128, NB, EXT], BF16) for _ in range(2)]; vcx = [c.sb([128, NB, EXT], BF16) for _ in range(2)]
    bkcx = Buf(); bvcx = Buf()
    for t_ in kcx:
        c.v("memset", [], [bkcx], eng="gpsimd", ap=t_[:, :, :], constant=0.0)
    for t_ in vcx:
        c.v("memset", [], [bvcx], eng="gpsimd", ap=t_[:, :, :], constant=0.0)

    xt = [c.sb([128, TB, D]) for _ in range(2)]; bxt = [Buf(), Buf()]
    _xh = c.sb([128, D]); _bxh = Buf()
    xh = [_xh, _xh]; bxh = [_bxh, _bxh]
    c.v("memset", [], [_bxh], eng="gpsimd", ap=_xh[:, :], constant=0.0)
    xT = [c.sb([128, 8, TB * 128], BF16) for _ in range(2)]; bxT = [Buf(), Buf()]
    xTh = [c.sb([128, 8, 128], BF16) for _ in range(2)]; bxTh = [Buf(), Buf()]
    _q = c.sb([128, 8, TB * 128], BF16); _bq = Buf()
    qst = [_q, _q]; bqst = [_bq, _bq]
    kst = [c.sb([128, 2, TB * 128], BF16) for _ in range(2)]; bkst = [Buf(), Buf()]
    vsw = [c.sb([128, TB, 256], BF16) for _ in range(2)]; bvsw = [Buf(), Buf()]
    gts = [c.sb([128, TB, 24]) for _ in range(2)]; bgts = [Buf(), Buf()]
    rope = RopeEvac(c, permb, bperm, TB * 128, bC, bS, nbuf=2)

    for ti in range(NT):
        p = ti % 2
        blk0 = ti * TB
        load_x_tile(c, xin, blk0, TB, xt[p], bxt[p], xh[p], bxh[p])
        transpose_x(c, xt[p], bxt[p], TB, xT[p], bxT[p], ident, bid, xh[p], bxh[p], xTh[p], bxTh[p])
        if stop <= 1.1:
            continue
        NC = TB * 128
        Cown = C3[:, blk0:blk0 + TB, HALO:EXT]; Sown = S3[:, blk0:blk0 + TB, HALO:EXT]
        Chal = C3[:, blk0:blk0 + TB, 0:HALO]; Shal = S3[:, blk0:blk0 + TB, 0:HALO]

        def proj(col, xTt, bx, ncols):
            pt, pb = c.ps()
            for k in range(8):
                c.mm(pt[:, 0:ncols], wab[:, k, col:col + 128], xTt[:, k, 0:ncols], k == 0, k == 7, [bwa, bx], [pb])
            return pt, pb

        v3 = lambda ap: ap.rearrange("p (n e) -> p n e", n=TB)
        for h in range(8):
            pt, pb = proj(A_Q + 128 * h, xT[p], bxT[p], NC)
            rope(pt, pb, NC, 0.125, Cown, Sown, [(slice(0, 128), v3(qst[p][:, h, :]), bqst[p])])
        if stop <= 1.2:
            continue
        c.store(QT[:, :, blk0 * 128:blk0 * 128 + NC].rearrange("h d t -> d h t"), qst[p][0:64, :, :], [bqst[p]])
        if stop <= 1.3:
            continue
        for j, col in enumerate((A_KS, A_KW)):
            pt, pb = proj(col, xT[p], bxT[p], NC)
            rope(pt, pb, NC, 1.0, Cown, Sown, [(slice(0, 128), v3(kst[p][:, j, :]), bkst[p])])
        c.store(KsT[:, blk0 * 128:blk0 * 128 + NC], kst[p][:, 0, :], [bkst[p]])
        c.store(KwT[:, blk0 * 128:blk0 * 128 + NC], kst[p][:, 1, :], [bkst[p]])
        pt, pb = proj(A_KC, xT[p], bxT[p], NC)
        rope(pt, pb, NC, 1.0, Cown, Sown, [(slice(0, 64), kcx[0][0:64, blk0:blk0 + TB, HALO:EXT], bkcx),
                                           (slice(64, 128), kcx[1][64:128, blk0:blk0 + TB, HALO:EXT], bkcx)])
        pt, pb = proj(A_KC, xTh[p], bxTh[p], 16 * TB)
        rope(pt, pb, 16 * TB, 1.0, Chal, Shal, [(slice(0, 64), kcx[0][0:64, blk0:blk0 + TB, 0:HALO], bkcx),
                                                (slice(64, 128), kcx[1][64:128, blk0:blk0 + TB, 0:HALO], bkcx)])
        pt, pb = proj(A_VC, xT[p], bxT[p], NC)
        for hh in range(2):
            ps_ = slice(hh * 64, hh * 64 + 64)
            c.act(vcx[hh][ps_, blk0:blk0 + TB, HALO:EXT], v3(pt[ps_, 0:NC]), AF.Copy, [pb], [bvcx])
        pt, pb = proj(A_VC, xTh[p], bxTh[p], 16 * TB)
        for hh in range(2):
            ps_ = slice(hh * 64, hh * 64 + 64)
            c.act(vcx[hh][ps_, blk0:blk0 + TB, 0:HALO], v3(pt[ps_, 0:16 * TB]), AF.Copy, [pb], [bvcx])
        if stop <= 1.4:
            continue
        for b in range(TB):
            pt, pb = c.ps()
            for k in range(8):
                c.mm(pt[:, 0:280], xT[p][:, k, b * 128:(b + 1) * 128], wab[:, k, A_VS:A_VS + 280], k == 0, k == 7,
                     [bwa, bxT[p]], [pb])
            c.v("tensor_copy", [pb], [bvsw[p]], out=vsw[p][:, b, :], in_=pt[:, 0:256])
            c.act(gts[p][:, b, :], pt[:, 256:280], AF.Sigmoid, [pb], [bgts[p]])
        rows = slice(blk0 * 128, blk0 * 128 + NC)
        c.store(Vs[rows, :].rearrange("(n p) f -> p n f", p=128), vsw[p][:, :, 0:128], [bvsw[p]])
        c.store(Vw[rows, :].rearrange("(n p) f -> p n f", p=128), vsw[p][:, :, 128:256], [bvsw[p]])
        c.store(gates[rows, :].rearrange("(n p) f -> p n f", p=128), gts[p][:, :, :], [bgts[p]])

    if stop <= 2:
        return c.done()
    cb = c.sb([128, 2, 2]); bcb = Buf()
    gh = c.sb([128, 2, NS], BF16); bgh = Buf()
    kco = c.sb([64, 2, NS], BF16); bkco = Buf()
    vco = c.sb([128, (NS + 127) // 128, 128], BF16); bvco = Buf()
    for kvi, (nm, ext, bext) in enumerate((("k", kcx, bkcx), ("v", vcx, bvcx))):
        for cc in range(2):
            pt, pb = c.ps()
            for l in range(32):
                c.mm(pt[:, 0:1], w1b[nm][:, l, cc * 128:(cc + 1) * 128], pTb[nm][:, l:l + 1], l == 0, l == 31,
                     [bcw], [pb])
            c.v("tensor_copy", [pb], [bcb], out=cb[:, kvi, cc:cc + 1], in_=pt[:, 0:1])
        for h in range(2):
            for cc in range(2):
                pt, pb = c.ps()
                for l in range(32):
                    rhs = ext[h][:, :, :].rearrange("p n (s r) -> p n s r", r=16)[:, :, l // 16:l // 16 + 8, l % 16]
                    c.mm(pt[:, 0:NS].rearrange("p (n s) -> p n s", s=8), w1b[nm][:, l, cc * 128:(cc + 1) * 128], rhs,
                         l == 0, l == 31, [bcw, bext], [pb])
                c.act(gh[:, cc, :], pt[:, 0:NS], AF.Gelu_apprx_tanh, [pb, bcb], [bgh], bias=cb[:, kvi, cc:cc + 1])
            if nm == "k":
                pt, pb = c.ps()
                for cc in range(2):
                    c.mm(pt[0:64, 0:NS], w2b[nm][:, cc, :], gh[:, cc, :], cc == 0, cc == 1, [bcw, bgh], [pb])
                c.v("tensor_copy", [pb], [bkco], out=kco[:, h, :], in_=pt[0:64, 0:NS])
            else:
                for s0 in range(0, NS, 128):
                    sn = min(128, NS - s0)
                    pt, pb = c.ps()
                    for cc in range(2):
                        c.mm(pt[0:sn, 0:64], gh[:, cc, s0:s0 + sn], w2b[nm][:, cc, :], cc == 0, cc == 1, [bcw, bgh], [pb])
                    c.v("tensor_copy", [pb], [bvco], out=vco[0:sn, s0 // 128, h * 64:(h + 1) * 64], in_=pt[0:sn, 0:64])
    c.store(kcmpT.rearrange("h d s -> d h s"), kco[:, :, :], [bkco])
    if NS >= 128:
        c.store(vcmp.rearrange("(n p) f -> p n f", p=128), vco[:, :, :], [bvco])
    else:
        c.store(vcmp[:, :], vco[0:NS, 0, :], [bvco])
    return c.done()


def rope_tables(c, pos, NP, invf, sgn, bsm):
    Ct = c.sb([128, NP]); St = c.sb([128, NP]); bC = Buf(); bS = Buf()
    CH = 576
    posi = c.sb([128, CH], I32); bpos = Buf()
    ang = c.sb([128, CH]); bang = Buf()
    tmpA = c.sb([128, CH]); tmpB = c.sb([128, CH]); tmpI = c.sb([128, CH], I32); btmp = Buf()
    TWO_PI = float(2 * np.pi)
    for c0 in range(0, NP, CH):
        w = min(CH, NP - c0)
        c.load(posi[:, 0:w], pos[0:1, c0:c0 + w].partition_broadcast(128), [bpos])
        c.v("tensor_copy", [bpos], [bang], out=ang[:, 0:w], in_=posi[:, 0:w])
        c.v("tensor_scalar", [bang, bsm], [bang], out=ang[:, 0:w], in0=ang[:, 0:w], scalar1=invf[:, 0:1], scalar2=None,
            op0=ALU.mult)
        for dst, bdst, shift in ((St, bS, 0.0), (Ct, bC, float(np.pi / 2))):
            A = tmpA[:, 0:w]; B = tmpB[:, 0:w]; I_ = tmpI[:, 0:w]
            c.v("tensor_scalar", [bang], [btmp], out=A, in0=ang[:, 0:w], scalar1=shift, scalar2=None, op0=ALU.add)
            c.v("tensor_scalar", [btmp], [btmp], out=I_, in0=A, scalar1=float(1 / TWO_PI), scalar2=None, op0=ALU.mult)
            c.v("tensor_copy", [btmp], [btmp], out=B, in_=I_)
            c.v("scalar_tensor_tensor", [btmp], [btmp], out=A, in0=B, scalar=-TWO_PI, in1=A, op0=ALU.mult, op1=ALU.add)
            c.v("tensor_scalar", [btmp], [btmp], out=B, in0=A, scalar1=float(np.pi), scalar2=-TWO_PI, op0=ALU.is_gt,
                op1=ALU.mult)
            c.v("tensor_tensor", [btmp], [btmp], out=A, in0=A, in1=B, op=ALU.add)
            c.v("tensor_scalar", [btmp], [btmp], out=B, in0=A, scalar1=float(-np.pi), scalar2=TWO_PI, op0=ALU.is_lt,
                op1=ALU.mult)
            c.v("tensor_tensor", [btmp], [btmp], out=A, in0=A, in1=B, op=ALU.add)
            c.v("tensor_scalar", [btmp], [btmp], out=A, in0=A, scalar1=float(np.pi), scalar2=float(-np.pi), op0=ALU.min,
                op1=ALU.max)
            c.act(dst[:, c0:c0 + w], A, AF.Sin, [btmp], [bdst])
    c.v("tensor_scalar", [bS, bsm], [bS], out=St[:, :], in0=St[:, :], scalar1=sgn[:, 0:1], scalar2=None, op0=ALU.mult)
    return Ct, bC, St, bS


ROPE_DBG = 99


class RopeEvac:
    def __init__(self, c, permb, bperm, maxcols, bC, bS, nbuf=3):
        self.c = c; self.permb = permb; self.bperm = bperm; self.bC = bC; self.bS = bS
        self.n = nbuf; self.i = 0
        self.kb = [c.sb([128, maxcols], BF16) for _ in range(nbuf)]; self.bkb = [Buf() for _ in range(nbuf)]
        self.t1 = [c.sb([128, maxcols]) for _ in range(nbuf)]; self.bt1 = [Buf() for _ in range(nbuf)]
        self.t2 = [c.sb([128, maxcols]) for _ in range(nbuf)]; self.bt2 = [Buf() for _ in range(nbuf)]

    def __call__(self, pt, pb, ncols, scale, Cap, Sap, dsts):
        c = self.c
        i = self.i % self.n; self.i += 1
        nbk = Cap.shape[1]
        kb, t1, t2 = self.kb[i], self.t1[i], self.t2[i]
        c.act(kb[:, 0:ncols], pt[:, 0:ncols], AF.Copy, [pb], [self.bkb[i]], scale=float(scale))
        if ROPE_DBG <= 0:
            return
        pp, ppb = c.ps()
        c.mm(pp[:, 0:ncols], self.permb[:, :], kb[:, 0:ncols], True, True, [self.bperm, self.bkb[i]], [ppb])
        v3 = lambda ap: ap.rearrange("p (n e) -> p n e", n=nbk)
        if ROPE_DBG <= 1:
            return
        c.v("scalar_tensor_tensor", [pb, self.bC], [self.bt1[i]], out=v3(t1[:, 0:ncols]), in0=v3(pt[:, 0:ncols]),
            scalar=float(scale), in1=Cap, op0=ALU.mult, op1=ALU.mult)
        if ROPE_DBG <= 2:
            return
        c.v("tensor_tensor", [ppb, self.bS], [self.bt2[i]], out=v3(t2[:, 0:ncols]), in0=v3(pp[:, 0:ncols]), in1=Sap,
            op=ALU.mult)
        if ROPE_DBG <= 3:
            return
        for (psl, dst3, bdst) in dsts:
            c.v("tensor_tensor", [self.bt1[i], self.bt2[i]], [bdst], eng="gpsimd", out=dst3,
                in0=v3(t1[psl, 0:ncols]), in1=v3(t2[psl, 0:ncols]), op=ALU.add)


def core_block_ids(S, core):
    nblk = S // BLK
    return core // 4, list(range(core % 4, nblk, 4))


def make_xin(xb, blocks):
    S, F = xb.shape
    out = np.zeros((len(blocks) * EXT, F), xb.dtype)
    for i, n in enumerate(blocks):
        lo = n * BLK - HALO
        if lo >= 0:
            out[i * EXT:(i + 1) * EXT] = xb[lo:lo + EXT]
        else:
            out[i * EXT + HALO:(i + 1) * EXT] = xb[0:BLK]
    return out


def wa_cols(w_in_l):
    sl = lambda o, w: w_in_l[:, o:o + w]
    z = np.zeros((w_in_l.shape[0], 64), w_in_l.dtype)
    qs = []
    for h in range(8):
        qs += [sl(O_Q + 64 * h, 64), z]
    return np.ascontiguousarray(np.concatenate(
        qs + [sl(O_KC, 128), sl(O_VC, 128), sl(O_KS, 128), sl(O_KW, 128), sl(O_VS, 128), sl(O_VW, 128),
              sl(O_NG, 24)], axis=1))


def s1a_inputs(x, positions, L, W, S, consts):
    maps = []
    wa = wa_cols(W["w_in"][L])
    for core in range(8):
        b, blocks = core_block_ids(S, core)
        xin = make_xin(x[b], blocks)
        pos = make_xin(positions[b][:, None].astype(np.int32), blocks).reshape(1, -1)
        maps.append(dict(
            xin=xin, pos=np.ascontiguousarray(pos), wa=wa,
            kposT=np.ascontiguousarray(W["cmp_k_pos"][L].T), kw1=W["cmp_k_w1"][L], kw2=W["cmp_k_w2"][L],
            vposT=np.ascontiguousarray(W["cmp_v_pos"][L].T), vw1=W["cmp_v_w1"][L], vw2=W["cmp_v_w2"][L],
            ident=consts["ident"], perm=consts["perm"], invf=consts["invf"], sgn=consts["sgn"]))
    return maps


DBG_K = -1


def build_s3(NB, S, stop=99, c=None):
    c = c or Ctx()
    T = NB * BLK
    NSLOT = S // 16
    NBLKT = S // 64
    NKT = S // 128
    NCHT = max(1, NSLOT // 128)
    QT = c.din("QT", [8, 64, T], BF16)
    gates = c.din("gates", [T, 24])
    KAd = c.din("KA", [2, 128, S], BF16)
    VsAd = c.din("VsA", [2, S, 65], BF16)
    KwBd = c.din("KwB", [NB, 2, 128, 640], BF16)
    VwBd = c.din("VwB", [NB, 2, 640, 65], BF16)
    kcTd = c.din("kcT", [2, 128, NSLOT], BF16)
    vcAd = c.din("vcA", [NSLOT, 128], BF16)
    tqd = c.din("tq", [128, NB]); curd = c.din("cur", [128, NB]); curm1d = c.din("curm1", [128, NB])
    sendd = c.din("slot_end", [128, NSLOT]); blkidxd = c.din("blkidx", [128, NBLKT])
    sdmd = c.din("sdm", [4, 128, 128], BF16); wmaskd = c.din("wmask", [2, 128, 128], BF16)
    identbd = c.din("identb", [128, 128], BF16)
    oT = c.dout("oT", [512, T], BF16)

    nc = c.nc
    def bank(nm):
        t = c.es.enter_context(nc.psum_tensor(nm, [128, 512], F32))
        return t, Buf(nm, psum=True)
    SC = [bank("sc0"), bank("sc1")]
    OC = bank("oc"); OS = bank("os"); OW = bank("ow")
    ST = [bank("st0"), bank("st1"), bank("st2")]

    bres = Buf()
    KA = [c.sb([128, S], BF16) for _ in range(2)]
    VS = [c.sb([128, NKT, 65], BF16) for _ in range(2)]
    for hk in range(2):
        for c0 in range(0, S, 2048):
            c.load(KA[hk][:, c0:c0 + 2048], KAd[hk, :, c0:c0 + 2048], [bres])
        vv = VsAd[hk].rearrange("(n p) f -> p n f", p=128)
        for n0 in range(0, NKT, 32):
            n1 = min(NKT, n0 + 32)
            c.load(VS[hk][:, n0:n1, :], vv[:, n0:n1, :], [bres])
    kcT = [c.sb([128, NSLOT], BF16) for _ in range(2)]
    for hk in range(2):
        c.load(kcT[hk][:, :], kcTd[hk], [bres])
    vcS = c.sb([128, NCHT, 128], BF16)
    c.load(vcS[:, :, :], vcAd.rearrange("(n p) f -> p n f", p=128), [bres])
    tq = c.sb([128, NB]); cur = c.sb([128, NB]); curm1 = c.sb([128, NB])
    c.load(tq[:, :], tqd[:, :], [bres]); c.load(cur[:, :], curd[:, :], [bres]); c.load(curm1[:, :], curm1d[:, :], [bres])
    send = c.sb([128, NSLOT]); blkidx = c.sb([128, NBLKT])
    c.load(send[:, :], sendd[:, :], [bres]); c.load(blkidx[:, :], blkidxd[:, :], [bres])
    identb = c.sb([128, 128], BF16)
    c.load(identb[:, :], identbd[:, :], [bres])
    sdm = c.sb([128, 4, 4, 128], BF16)
    wmask = c.sb([128, 2, 4, 128], BF16)
    for r in range(4):
        for g in range(4):
            c.load(sdm[:, r, g, :], sdmd[r], [bres])
    for r in range(2):
        for g in range(4):
            c.load(wmask[:, r, g, :], wmaskd[r], [bres])

    NG_MAX = (NBLKT + 63) // 64
    QP = [c.sb([128, 4, 128], BF16) for _ in range(2)]; bQP = [Buf(), Buf()]
    QM = [[c.sb([128, 4, 128], BF16) for _ in range(NG_MAX)] for _ in range(2)]
    bQM = [[Buf() for _ in range(NG_MAX)] for _ in range(2)]
    for p in range(2):
        c.v("memset", [], [bQP[p]], eng="gpsimd", ap=QP[p][:, :, :], constant=0.0)
    em = [[c.sb([128, NSLOT], BF16) for _ in range(4)] for _ in range(2)]; bem = [[Buf() for _ in range(4)] for _ in range(2)]
    e32 = [c.sb([128, NSLOT]) for _ in range(2)]; be32 = [Buf(), Buf()]
    rs = [c.sb([128, 4]) for _ in range(2)]; rinv = [c.sb([128, 4]) for _ in range(2)]; brs = [Buf(), Buf()]
    mx = c.sb([128, 4]); nmx = c.sb([128, 4]); bmx = Buf()
    Pacc = c.sb([128, NSLOT]); bPacc = Buf()
    valid = [c.sb([128, NSLOT]) for _ in range(2)]; bvalid = [Buf(), Buf()]
    m1 = [c.sb([128, NBLKT]) for _ in range(2)]; m2 = [c.sb([128, NBLKT]) for _ in range(2)]
    le = [c.sb([128, NBLKT]) for _ in range(2)]; bblk = [Buf(), Buf()]
    imp = c.sb([128, NBLKT]); tmpi = c.sb([128, NBLKT]); work = c.sb([128, NBLKT]); bimp = Buf()
    v8a = c.sb([128, 8]); v8b = c.sb([128, 8])
    MN = [c.sb([128, 64 + NBLKT + 64], BF16) for _ in range(2)]; bMN = [Buf(), Buf()]
    for p in range(2):
        c.v("memset", [], [bMN[p]], eng="gpsimd", ap=MN[p][:, :], constant=NEGM)
    emT = [c.sb([128, 128], BF16) for _ in range(3)]; bemT = [Buf() for _ in range(3)]
    PT = [c.sb([128, 512], BF16) for _ in range(3)]; bPT = [Buf() for _ in range(3)]
    KwB = [c.sb([128, 640], BF16) for _ in range(2)]; VwB = [c.sb([128, 5, 65], BF16) for _ in range(2)]
    bKw = [Buf(), Buf()]
    gt = [c.sb([128, 24]) for _ in range(2)]; bgt = [Buf(), Buf()]
    oacc = [c.sb([128, 8, 64]) for _ in range(2)]; boacc = [Buf(), Buf()]
    ob = c.sb([128, 512], BF16); bob = Buf()
    oTt = [c.sb([128, 4, 128], BF16) for _ in range(2)]; boTt = [Buf(), Buf()]
    coef = c.sb([128, 3, 4]); den = c.sb([128, 2, 4]); bcoef = Buf()
    cnt = {"st": 0, "emT": 0}

    def dims(j):
        NSj = min(NSLOT, 128 * ((j + 1 + 3) // 4))
        NBj = NSj // 4
        NGj = (NBj + 63) // 64
        return NSj, NBj, NGj

    def phase1a(k):
        j, hk = divmod(k, 2)
        p = k % 2
        jp = j % 2
        NSj, NBj, NGj = dims(j)
        qsrc = QT[hk * 4:(hk + 1) * 4, :, j * 128:(j + 1) * 128].rearrange("g d t -> d g t")
        c.load(QP[p][0:64, :, :], qsrc, [bQP[p]])
        for G in range(NGj):
            c.load(QM[p][G][0:64, :, :], qsrc, [bQM[p][G]])
        if hk == 0:
            c.load(gt[jp][:, :], gates[j * 128:(j + 1) * 128, :], [bgt[jp]])
            c.v("tensor_scalar", [bres], [bvalid[jp]], out=valid[jp][:, 0:NSj], in0=send[:, 0:NSj], scalar1=tq[:, j:j + 1],
                scalar2=NEGM, op0=ALU.is_gt, op1=ALU.mult)
            c.v("tensor_scalar", [bres], [bblk[jp]], out=m1[jp][:, 0:NBj], in0=blkidx[:, 0:NBj], scalar1=cur[:, j:j + 1],
                scalar2=1e4, op0=ALU.is_equal, op1=ALU.mult)
            c.v("tensor_scalar", [bres], [bblk[jp]], out=m2[jp][:, 0:NBj], in0=blkidx[:, 0:NBj], scalar1=curm1[:, j:j + 1],
                scalar2=1e4, op0=ALU.is_equal, op1=ALU.mult)
            c.v("tensor_tensor", [bblk[jp]], [bblk[jp]], out=m1[jp][:, 0:NBj], in0=m1[jp][:, 0:NBj], in1=m2[jp][:, 0:NBj],
                op=ALU.max)
            c.v("memset", [], [bblk[jp]], ap=m1[jp][:, 0:1], constant=1e4)
            c.v("tensor_scalar", [bres], [bblk[jp]], out=le[jp][:, 0:NBj], in0=blkidx[:, 0:NBj], scalar1=cur[:, j:j + 1],
                scalar2=None, op0=ALU.is_le)
        nch = (NSj + 511) // 512
        for g in range(4):
            for ci in range(nch):
                c0 = ci * 512; w = min(512, NSj - c0)
                c.mm(SC[ci][0][:, 0:w], QP[p][:, g, :], kcT[hk][:, c0:c0 + w], True, True, [bQP[p], bres], [SC[ci][1]])
                c.v("tensor_reduce", [SC[ci][1]], [bmx], out=mx[:, ci:ci + 1], in_=SC[ci][0][:, 0:w], axis=AX.X, op=ALU.max)
            if nch == 2:
                c.v("tensor_tensor", [bmx], [bmx], out=mx[:, 0:1], in0=mx[:, 0:1], in1=mx[:, 1:2], op=ALU.max)
            c.v("tensor_scalar", [bmx], [bmx], out=nmx[:, g:g + 1], in0=mx[:, 0:1], scalar1=-1.0, scalar2=None, op0=ALU.mult)
            for ci in range(nch):
                c0 = ci * 512; w = min(512, NSj - c0)
                c.v("tensor_tensor", [SC[ci][1], bvalid[jp]], [be32[g % 2]], out=e32[g % 2][:, c0:c0 + w],
                    in0=SC[ci][0][:, 0:w], in1=valid[jp][:, c0:c0 + w], op=ALU.add)
            c.act(em[p][g][:, 0:NSj], e32[g % 2][:, 0:NSj], AF.Exp, [be32[g % 2], bmx], [bem[p][g], brs[p]],
                  bias=nmx[:, g:g + 1], accum_out=rs[p][:, g:g + 1])
        c.v("tensor_scalar", [brs[p]], [brs[p]], out=rs[p][:, :], in0=rs[p][:, :], scalar1=1e-30, scalar2=None, op0=ALU.max)
        c.v("reciprocal", [brs[p]], [brs[p]], out=rinv[p][:, :], in_=rs[p][:, :])
        c.v("tensor_scalar", [bem[p][0], brs[p]], [bPacc], out=Pacc[:, 0:NSj], in0=em[p][0][:, 0:NSj],
            scalar1=rinv[p][:, 0:1], scalar2=None, op0=ALU.mult)
        for g in range(1, 4):
            c.v("scalar_tensor_tensor", [bem[p][g], brs[p], bPacc], [bPacc], out=Pacc[:, 0:NSj], in0=em[p][g][:, 0:NSj],
                scalar=rinv[p][:, g:g + 1], in1=Pacc[:, 0:NSj], op0=ALU.mult, op1=ALU.add)
        P4 = Pacc[:, 0:NSj].rearrange("p (b f) -> p b f", f=4)
        I = imp[:, 0:NBj]; Tm = tmpi[:, 0:NBj]
        c.v("tensor_tensor", [bPacc], [bimp], out=Tm, in0=P4[:, :, 1], in1=P4[:, :, 2], op=ALU.add)
        c.v("tensor_tensor", [bPacc, bimp], [bimp], out=Tm, in0=Tm, in1=P4[:, :, 3], op=ALU.add)
        c.v("scalar_tensor_tensor", [bPacc, bimp], [bimp], out=I, in0=Tm, scalar=2.0, in1=P4[:, :, 0], op0=ALU.mult,
            op1=ALU.add)
        if NBj > 1:
            c.v("tensor_tensor", [bPacc, bimp], [bimp], out=imp[:, 0:NBj - 1], in0=imp[:, 0:NBj - 1], in1=P4[:, 1:NBj, 0],
                op=ALU.add)
        c.v("tensor_tensor", [bimp, bblk[jp]], [bimp], out=I, in0=I, in1=m1[jp][:, 0:NBj], op=ALU.max)
        c.v("scalar_tensor_tensor", [bimp, bblk[jp]], [bimp], out=I, in0=I, scalar=1.0, in1=le[jp][:, 0:NBj], op0=ALU.add,
            op1=ALU.mult)
        c.v("tensor_scalar", [bimp], [bimp], out=I, in0=I, scalar1=-1.0, scalar2=None, op0=ALU.add)
        c.v("max", [bimp], [bimp], out=v8a[:, :], in_=I)
        c.v("match_replace", [bimp], [bimp], out=work[:, 0:NBj], in_to_replace=v8a[:, :], in_values=I, imm_value=-2.0)
        c.v("max", [bimp], [bimp], out=v8b[:, :], in_=work[:, 0:NBj])
        c.v("tensor_scalar", [bimp], [bimp], out=Tm, in0=I, scalar1=v8b[:, 7:8], scalar2=None, op0=ALU.is_ge)
        c.v("tensor_tensor", [bimp, bblk[jp]], [bimp], out=Tm, in0=Tm, in1=le[jp][:, 0:NBj], op=ALU.mult)
        c.v("tensor_scalar", [bimp], [bMN[p]], out=MN[p][:, 64:64 + NBj], in0=Tm, scalar1=-1.0, scalar2=-NEGM, op0=ALU.add,
            op1=ALU.mult)

    def tr_bank(i):
        t, b = SC[i % 2]
        return t[:, 0:64].bitcast(BF16), b

    def phase1b(k):
        j, hk = divmod(k, 2)
        p = k % 2
        NSj, NBj, NGj = dims(j)
        for G in range(NGj):
            tv, tb = tr_bank(G)
            c.tr(tv, MN[p][:, 64 * G:64 * G + 128], identb[:, :], [bMN[p], bres], [tb])
            for g in range(4):
                if g % 2 == 0:
                    c.v("tensor_copy", [tb], [bQM[p][G]], out=QM[p][G][64:128, g, :], in_=tv[64:128, :])
                else:
                    c.act(QM[p][G][64:128, g, :], tv[64:128, :], AF.Copy, [tb], [bQM[p][G]])
        first = True
        for g in range(4):
            for ch in range(NSj // 128):
                i = cnt["emT"]; cnt["emT"] += 1
                tv, tb = tr_bank(i)
                c.tr(tv, em[p][g][:, ch * 128:(ch + 1) * 128], identb[:, :], [bem[p][g], bres], [tb])
                et = emT[i % 3]; bet = bemT[i % 3]
                if i % 2 == 0:
                    c.v("tensor_copy", [tb], [bet], out=et[:, :], in_=tv)
                else:
                    c.act(et[:, :], tv, AF.Copy, [tb], [bet])
                last = (g == 3 and ch == NSj // 128 - 1)
                c.S.op("tensor", (lambda e, et=et, g=g, ch=ch, first=first, last=last: e.matmul(
                    OC[0][:, g * 64:(g + 1) * 64], lhsT=et[:, :], rhs=vcS[:, ch, hk * 64:(hk + 1) * 64], start=first,
                    stop=last, skip_group_check=True)), [bet, bres], [OC[1]])
                first = False

    def attend(kT_of, v_of, ntiles, qrhs_of, mask_of, Obank, reads_k):
        def qk(i):
            si = cnt["st"]; cnt["st"] += 1
            st, stb = ST[si % 3]
            msk = mask_of(i)
            rhs, brhs = qrhs_of(i)
            c.mm(st[:, 0:512], kT_of(i), rhs, True, msk is None, reads_k + [brhs], [stb])
            if msk is not None:
                c.mm(st[:, 0:512], identb[:, :], msk, False, True, [bres], [stb])
            pt = PT[si % 3]; bpt = bPT[si % 3]
            c.act(pt[:, :], st[:, 0:512], AF.Exp, [stb], [bpt])
            return pt, bpt

        first = True
        nxt = qk(0)
        for i in range(ntiles):
            pt, bpt = nxt
            if i + 1 < ntiles:
                nxt = qk(i + 1)
            for g in range(4):
                last = (i == ntiles - 1 and g == 3)
                c.S.op("tensor", (lambda e, pt=pt, g=g, i=i, first=first, last=last: e.matmul(
                    Obank[0][:, g * 65:(g + 1) * 65], lhsT=pt[:, g * 128:(g + 1) * 128], rhs=v_of(i), start=first,
                    stop=last, skip_group_check=True)), [bpt] + reads_k, [Obank[1]])
                first = False

    def phase2(k):
        j, hk = divmod(k, 2)
        p = k % 2
        jp = j % 2
        NSj, NBj, NGj = dims(j)
        NTj = 4 * j + 4
        flat = lambda t: t[:, :, :].rearrange("p g q -> p (g q)")
        attend(lambda i: KA[hk][:, i * 128:(i + 1) * 128], lambda i: VS[hk][:, i, :], NTj,
               lambda i: (flat(QM[p][i // 32]), bQM[p][i // 32]),
               lambda i: (flat4(sdm, i - 4 * j) if i >= 4 * j else None), OS, [bres])
        c.load(KwB[p][:, :], KwBd[j, hk], [bKw[p]])
        c.load(VwB[p][:, :, :], VwBd[j, hk].rearrange("(n p) f -> p n f", p=128), [bKw[p]])
        attend(lambda i: KwB[p][:, i * 128:(i + 1) * 128], lambda i: VwB[p][:, i, :], 5,
               lambda i: (flat(QP[p]), bQP[p]),
               lambda i: (flat4(wmask, 0) if i == 0 else (flat4(wmask, 1) if i == 4 else None)), OW, [bKw[p]])
        if DBG_K == k:
            dbgt = c.sb([128, 1024]); bdbg = Buf()
            dbg = c.dout("dbg", [128, 1024])
            c.v("tensor_copy", [OC[1]], [bdbg], out=dbgt[:, 0:256], in_=OC[0][:, 0:256])
            c.v("tensor_copy", [OS[1]], [bdbg], out=dbgt[:, 256:516], in_=OS[0][:, 0:260])
            c.v("tensor_copy", [OW[1]], [bdbg], out=dbgt[:, 516:776], in_=OW[0][:, 0:260])
            c.v("tensor_copy", [brs[p]], [bdbg], out=dbgt[:, 776:780], in_=rinv[p][:, :])
            c.v("tensor_copy", [bMN[p]], [bdbg], out=dbgt[:, 780:780 + NBj], in_=MN[p][:, 64:64 + NBj])
            c.v("tensor_copy", [bQM[p][0]], [bdbg], out=dbgt[:, 900:1024], in_=QM[p][0][:, 0, 0:124])
            c.store(dbg[:, :], dbgt[:, :], [bdbg])
        g3 = gt[jp][:, hk * 12:(hk + 1) * 12].rearrange("p (g b) -> p g b", b=3)
        c.v("tensor_tensor", [brs[p], bgt[jp]], [bcoef], out=coef[:, 0, :], in0=rinv[p][:, :], in1=g3[:, :, 0], op=ALU.mult)
        for bi, Ob in ((1, OS), (2, OW)):
            dv = Ob[0][:, 0:260].rearrange("p (g f) -> p g f", f=65)[:, :, 64]
            c.v("tensor_copy", [Ob[1]], [bcoef], out=den[:, bi - 1, :], in_=dv)
            c.v("reciprocal", [bcoef], [bcoef], out=den[:, bi - 1, :], in_=den[:, bi - 1, :])
            c.v("tensor_tensor", [bcoef, bgt[jp]], [bcoef], out=coef[:, bi, :], in0=den[:, bi - 1, :], in1=g3[:, :, bi],
                op=ALU.mult)
        for g in range(4):
            dst = oacc[jp][:, hk * 4 + g, :]
            c.v("tensor_scalar", [OC[1], bcoef], [boacc[jp]], out=dst, in0=OC[0][:, g * 64:(g + 1) * 64],
                scalar1=coef[:, 0, g:g + 1], scalar2=None, op0=ALU.mult)
            c.v("scalar_tensor_tensor", [OS[1], bcoef, boacc[jp]], [boacc[jp]], out=dst, in0=OS[0][:, g * 65:g * 65 + 64],
                scalar=coef[:, 1, g:g + 1], in1=dst, op0=ALU.mult, op1=ALU.add)
            c.v("scalar_tensor_tensor", [OW[1], bcoef, boacc[jp]], [boacc[jp]], out=dst, in0=OW[0][:, g * 65:g * 65 + 64],
                scalar=coef[:, 2, g:g + 1], in1=dst, op0=ALU.mult, op1=ALU.add)
        if hk == 1:
            c.act(ob[:, :], oacc[jp][:, :, :].rearrange("p h d -> p (h d)"), AF.Copy, [boacc[jp]], [bob])
            for ch in range(4):
                tv, tb = tr_bank(ch)
                c.tr(tv, ob[:, ch * 128:(ch + 1) * 128], identb[:, :], [bob, bres], [tb])
                c.v("tensor_copy", [tb], [boTt[jp]], out=oTt[jp][:, ch, :], in_=tv)
            c.store(oT[:, j * 128:(j + 1) * 128].rearrange("(c p) t -> p c t", p=128), oTt[jp][:, :, :], [boTt[jp]])

    def flat4(t, r):
        return t[:, r, :, :].rearrange("p g q -> p (g q)")

    NK = NB * 2
    if stop <= 1:
        return c.done()
    phase1a(0)
    if stop <= 2:
        return c.done()
    phase1b(0)
    if stop <= 3:
        return c.done()
    if stop <= 4:
        phase2(0)
        return c.done()
    for k in range(NK):
        if stop >= 10 and k >= stop - 10:
            break
        if k + 1 < NK:
            phase1a(k + 1)
        phase2(k)
        if k + 1 < NK:
            phase1b(k + 1)
    return c.done()


def s3_inputs(s1a_res, S):
    nblk = S // BLK
    NB = nblk // 4
    NSLOT = S // 16
    full = {}
    for b in range(2):
        Ks = np.zeros((128, S), NPBF); Kw = np.zeros((128, S), NPBF)
        Vs_ = np.zeros((S, 128), NPBF); Vw_ = np.zeros((S, 128), NPBF)
        kc = np.zeros((2, 64, NSLOT), NPBF); vc = np.zeros((NSLOT, 128), NPBF)
        for cp in range(4):
            core = b * 4 + cp
            _, blocks = core_block_ids(S, core)
            r = s1a_res[core]
            for i, n in enumerate(blocks):
                ts = slice(n * 128, (n + 1) * 128); ls = slice(i * 128, (i + 1) * 128)
                Ks[:, ts] = r["KsT"][:, ls]; Kw[:, ts] = r["KwT"][:, ls]
                Vs_[ts] = r["Vs"][ls]; Vw_[ts] = r["Vw"][ls]
                kc[:, :, n * 8:(n + 1) * 8] = r["kcmpT"][:, :, i * 8:(i + 1) * 8]
                vc[n * 8:(n + 1) * 8] = r["vcmp"][i * 8:(i + 1) * 8]
        full[b] = (Ks, Kw, Vs_, Vw_, kc, vc)
    E = np.zeros((64, S), NPBF)
    keyblk = (np.arange(S) // 64) % 64
    E[keyblk, np.arange(S)] = 1
    slot_end = (16 * np.arange(NSLOT) + 15).astype(np.float32); slot_end[0] = 1e9
    slot_end = np.ascontiguousarray(np.broadcast_to(slot_end, (128, NSLOT)))
    blkidx = np.ascontiguousarray(np.broadcast_to(np.arange(S // 64, dtype=np.float32), (128, S // 64)))
    kk = np.arange(128)[:, None]; qq = np.arange(128)[None, :]
    tri = np.where(kk > qq, NEGM, 0.0).astype(NPBF)
    wm0 = np.where(kk <= qq, NEGM, 0.0).astype(NPBF)
    wmask = np.stack([wm0, tri])
    identb = np.eye(128, dtype=np.float32).astype(NPBF)
    maps = []
    for core in range(8):
        b, blocks = core_block_ids(S, core)
        cp = core % 4
        Ks, Kw, Vs_, Vw_, kc, vc = full[b]
        KA = np.zeros((2, 128, S), NPBF); VsA = np.zeros((2, S, 65), NPBF)
        for hk in range(2):
            KA[hk, 0:64] = Ks[hk * 64:(hk + 1) * 64]; KA[hk, 64:128] = E
            VsA[hk, :, 0:64] = Vs_[:, hk * 64:(hk + 1) * 64]; VsA[hk, :, 64] = 1
        KwB = np.zeros((NB, 2, 128, 640), NPBF); VwB = np.zeros((NB, 2, 640, 65), NPBF)
        for i, n in enumerate(blocks):
            lo = n * 128 - 512
            s0 = max(lo, 0)
            for hk in range(2):
                KwB[i, hk, 0:64, s0 - lo:] = Kw[hk * 64:(hk + 1) * 64, s0:n * 128 + 128]
                VwB[i, hk, s0 - lo:, 0:64] = Vw_[s0:n * 128 + 128, hk * 64:(hk + 1) * 64]
                VwB[i, hk, s0 - lo:, 64] = 1
        kcT = np.zeros((2, 128, NSLOT), NPBF); kcT[:, 0:64] = kc
        t = (np.array(blocks)[None, :] * 128 + np.arange(128)[:, None]).astype(np.float32)
        sdm = np.zeros((4, 128, 128), NPBF); sdm[cp] = tri
        maps.append(dict(QT=s1a_res[core]["QT"], gates=s1a_res[core]["gates"], KA=KA, VsA=VsA, KwB=KwB, VwB=VwB,
                         kcT=kcT, vcA=vc, tq=t, cur=np.floor(t / 64).astype(np.float32),
                         curm1=(np.floor(t / 64) - 1).astype(np.float32), slot_end=slot_end, blkidx=blkidx,
                         sdm=sdm, wmask=wmask, identb=identb))
    return maps


def bcast_load(c, dram_vec, n, buf):
    t = c.sb([128, n])
    c.load(t[:, :], dram_vec[0:1, :].partition_broadcast(128), [buf])
    return t


class LNorm:
    def __init__(self, c, width):
        self.c = c; self.w = width; self.nch = width // 512
        self.stats = c.sb([128, self.nch, 6]); self.mv = c.sb([128, 2]); self.sd = c.sb([128, 1]); self.b = Buf()

    def __call__(self, z, bz, gbc, bbc, bgb, out, bout, eps=1e-5):
        c = self.c
        for i in range(self.nch):
            c.v("bn_stats", [bz], [self.b], out=self.stats[:, i, :], in_=z[:, i * 512:(i + 1) * 512])
        c.v("bn_aggr", [self.b], [self.b], out=self.mv[:, :], in_=self.stats[:, :, :].rearrange("p a b -> p (a b)"))
        c.v("tensor_scalar", [self.b], [self.b], out=self.sd[:, :], in0=self.mv[:, 1:2], scalar1=float(eps), scalar2=None,
            op0=ALU.add)
        c.act(self.sd[:, :], self.sd[:, :], AF.Sqrt, [self.b], [self.b])
        c.v("reciprocal", [self.b], [self.b], out=self.sd[:, :], in_=self.sd[:, :])
        c.v("tensor_scalar", [bz, self.b], [bz], out=z, in0=z, scalar1=self.mv[:, 0:1], scalar2=self.sd[:, 0:1],
            op0=ALU.subtract, op1=ALU.mult)
        c.v("tensor_tensor", [bz, bgb], [bz], eng="gpsimd", out=z, in0=z, in1=gbc, op=ALU.mult)
        c.v("tensor_tensor", [bz, bgb], [bout], out=out, in0=z, in1=bbc, op=ALU.add)


def build_s4a1(NB, c=None):
    c = c or Ctx()
    T = NB * BLK
    TB = min(4, NB)
    NT = NB // TB
    NC = TB * 128
    xin = c.din("xin", [NB * EXT, D])
    wb1d = c.din("wb1", [D, 1536])
    poolwd = c.din("pool_w", [4, 128, 128]); pscd = c.din("pool_scale", [128, 4])
    lngd = c.din("sg_ln_g", [1, 512]); lnbd = c.din("sg_ln_b", [1, 512])
    sgwTd = c.din("sg_wT", [4, 128, 128]); sgbd = c.din("sg_b", [1, 512]); trild = c.din("trilT", [128, 128])
    wpod = c.din("w_pool_out", [512, D]); wsod = c.din("w_sg_out", [512, D])
    invcd = c.din("invc", [128, 64]); identd = c.din("ident", [128, 128])
    ypo = c.dout("ypo", [D, T], BF16); yso = c.dout("yso", [D, T], BF16)
    c.init_psum()
    bw = Buf()
    ident = c.sb([128, 128]); bid = Buf()
    c.load(ident[:, :], identd[:, :], [bid])
    stage = [(c.sb([128, 2048]), Buf()) for _ in range(2)]
    wb1 = c.sb([128, 8, 1536], BF16)
    for k in range(8):
        c.load_cast(wb1[:, k, :], bw, wb1d[k * 128:(k + 1) * 128, :], stage)
    pwb = c.sb([128, 4, 128], BF16)
    c.load_cast(pwb[:, :, :], bw, poolwd.rearrange("g c d -> c g d"), stage)
    psc = c.sb([128, 4]); c.load(psc[:, :], pscd[:, :], [bw])
    wpo = c.sb([128, 4, D], BF16); wso = c.sb([128, 4, D], BF16)
    for g in range(4):
        c.load_cast(wpo[:, g, :], bw, wpod[g * 128:(g + 1) * 128, :], stage)
        c.load_cast(wso[:, g, :], bw, wsod[g * 128:(g + 1) * 128, :], stage)
    lng = bcast_load(c, lngd, 512, bw); lnb = bcast_load(c, lnbd, 512, bw); sgb = bcast_load(c, sgbd, 512, bw)
    tril = c.sb([128, 128]); c.load(tril[:, :], trild[:, :], [bw])
    wsf = c.sb([128, 4, 128]); c.load(wsf[:, :, :], sgwTd.rearrange("g s t -> s g t"), [bw])
    wsT = c.sb([128, 4, 128], BF16)
    for g in range(4):
        c.v("tensor_tensor", [bw], [bw], out=wsT[:, g, :], in0=wsf[:, g, :], in1=tril[:, :], op=ALU.mult)
    invc = c.sb([128, 4, 16]); c.load(invc[:, :, :], invcd.rearrange("p (g t) -> p g t", t=16), [bw])

    xt = [c.sb([128, TB, D]) for _ in range(2)]; bxt = [Buf(), Buf()]
    xh = [c.sb([128, D]) for _ in range(2)]; bxh = [Buf(), Buf()]
    for i in range(2):
        c.v("memset", [], [bxh[i]], eng="gpsimd", ap=xh[i][:, :], constant=0.0)
    xT = [c.sb([128, 8, NC], BF16) for _ in range(2)]; bxT = [Buf(), Buf()]
    xTh = [c.sb([128, 8, 128], BF16) for _ in range(2)]; bxTh = [Buf(), Buf()]
    aext = [c.sb([128, TB, EXT]) for _ in range(4)]; baext = [Buf() for _ in range(4)]
    B1 = c.sb([128, TB, EXT]); B2 = c.sb([128, TB, EXT]); bB1 = Buf(); bB2 = Buf()
    dT = c.sb([128, 4, NC], BF16); bdT = Buf()
    ypT = c.sb([128, 4, NC], BF16); bypT = Buf()
    uT = c.sb([128, 4, NC]); buT = Buf()
    gv = [c.sb([128, 512]) for _ in range(2)]; bgv = [Buf(), Buf()]
    vnb = [c.sb([128, 512], BF16) for _ in range(2)]; bvnb = [Buf(), Buf()]
    mtmp = c.sb([128, 512]); bmtmp = Buf()
    sguT = c.sb([128, 4, NC], BF16); bsgu = Buf()
    outb = [c.sb([128, 8, NC], BF16) for _ in range(2)]; boutb = [Buf(), Buf()]
    fix = c.sb([128, 16]); bfix = Buf()
    ln = LNorm(c, 512)
    v3 = lambda ap: ap.rearrange("p (n e) -> p n e", n=TB)

    for ti in range(NT):
        p = ti % 2
        blk0 = ti * TB
        load_x_tile(c, xin, blk0, TB, xt[p], bxt[p], xh[p], bxh[p])
        transpose_x(c, xt[p], bxt[p], TB, xT[p], bxT[p], ident, bid, xh[p], bxh[p], xTh[p], bxTh[p])
        for g in range(4):
            pt, pb = c.ps()
            for k in range(8):
                c.mm(pt[:, 0:NC], wb1[:, k, g * 128:(g + 1) * 128], xT[p][:, k, :], k == 0, k == 7, [bw, bxT[p]], [pb])
            c.act(aext[g][:, :, HALO:EXT], v3(pt[:, 0:NC]), AF.Copy, [pb], [baext[g]])
            pt, pb = c.ps()
            for k in range(8):
                c.mm(pt[:, 0:16 * TB], wb1[:, k, g * 128:(g + 1) * 128], xTh[p][:, k, 0:16 * TB], k == 0, k == 7,
                     [bw, bxTh[p]], [pb])
            c.v("tensor_copy", [pb], [baext[g]], out=aext[g][:, :, 0:HALO], in_=v3(pt[:, 0:16 * TB]))
            A = aext[g]
            src, bsrc = A, baext[g]
            sh = 1
            for step in range(g + 1):
                dst, bdst = (B1, bB1) if step % 2 == 0 else (B2, bB2)
                lo = 2 * sh - 1
                c.v("tensor_tensor", [bsrc], [bdst], eng="gpsimd", out=dst[:, :, lo:EXT], in0=src[:, :, lo:EXT],
                    in1=src[:, :, lo - sh:EXT - sh], op=ALU.add)
                src, bsrc = dst, bdst
                sh *= 2
            w = 2 ** (g + 1)
            c.v("scalar_tensor_tensor", [bsrc, baext[g]], [bdT], out=v3(dT[:, g, :]), in0=src[:, :, HALO:EXT],
                scalar=1.0 / w, in1=A[:, :, HALO:EXT], op0=ALU.mult, op1=ALU.subtract)
            if ti == 0:
                c.v("tensor_tensor", [bsrc, bw], [bfix], out=fix[:, :], in0=src[:, 0, HALO:HALO + 16], in1=invc[:, g, :],
                    op=ALU.mult)
                c.v("tensor_tensor", [bfix, baext[g]], [bdT], out=dT[:, g, 0:16], in0=fix[:, :], in1=A[:, 0, HALO:HALO + 16],
                    op=ALU.subtract)
            pt, pb = c.ps()
            c.mm(pt[:, 0:NC], pwb[:, g, :], dT[:, g, :], True, True, [bw, bdT], [pb])
            c.act(ypT[:, g, :], pt[:, 0:NC], AF.Copy, [pb, bw], [bypT], scale=psc[:, g:g + 1])
        for dc in range(8):
            pt, pb = c.ps()
            for g in range(4):
                c.mm(pt[:, 0:NC], wpo[:, g, dc * 128:(dc + 1) * 128], ypT[:, g, :], g == 0, g == 3, [bw, bypT], [pb])
            if dc % 2 == 0:
                c.v("tensor_copy", [pb], [boutb[0]], out=outb[0][:, dc, :], in_=pt[:, 0:NC])
            else:
                c.act(outb[0][:, dc, :], pt[:, 0:NC], AF.Copy, [pb], [boutb[0]])
        c.store(ypo[:, blk0 * 128:blk0 * 128 + NC].rearrange("(c p) t -> p c t", p=128), outb[0][:, :, :], [boutb[0]])
        for g in range(4):
            pt, pb = c.ps()
            for k in range(8):
                c.mm(pt[:, 0:NC], wb1[:, k, 512 + g * 128:512 + (g + 1) * 128], xT[p][:, k, :], k == 0, k == 7,
                     [bw, bxT[p]], [pb])
            c.act(uT[:, g, :], pt[:, 0:NC], AF.Gelu_apprx_tanh, [pb], [buT])
        for b in range(TB):
            q = b % 2
            pt, pb = c.ps()
            for k in range(8):
                c.mm(pt[:, 0:512], xT[p][:, k, b * 128:(b + 1) * 128], wb1[:, k, 1024:1536], k == 0, k == 7,
                     [bw, bxT[p]], [pb])
            c.act(gv[q][:, :], pt[:, 0:512], AF.Gelu_apprx_tanh, [pb], [bgv[q]])
            ln(gv[q][:, :], bgv[q], lng[:, :], lnb[:, :], bw, vnb[q][:, :], bvnb[q])
            pt, pb = c.ps()
            for g in range(4):
                c.mm(pt[:, g * 128:(g + 1) * 128], vnb[q][:, g * 128:(g + 1) * 128], wsT[:, g, :], True, True,
                     [bvnb[q], bw], [pb])
            c.v("tensor_tensor", [pb, bw], [bmtmp], out=mtmp[:, :], in0=pt[:, 0:512], in1=sgb[:, :], op=ALU.add)
            c.v("tensor_tensor", [bmtmp, buT], [bsgu], out=sguT[:, :, b * 128:(b + 1) * 128],
                in0=mtmp[:, :].rearrange("p (g t) -> p g t", g=4), in1=uT[:, :, b * 128:(b + 1) * 128], op=ALU.mult)
        for dc in range(8):
            pt, pb = c.ps()
            for g in range(4):
                c.mm(pt[:, 0:NC], wso[:, g, dc * 128:(dc + 1) * 128], sguT[:, g, :], g == 0, g == 3, [bw, bsgu], [pb])
            if dc % 2 == 0:
                c.v("tensor_copy", [pb], [boutb[1]], out=outb[1][:, dc, :], in_=pt[:, 0:NC])
            else:
                c.act(outb[1][:, dc, :], pt[:, 0:NC], AF.Copy, [pb], [boutb[1]])
        c.store(yso[:, blk0 * 128:blk0 * 128 + NC].rearrange("(c p) t -> p c t", p=128), outb[1][:, :, :], [boutb[1]])
    return c.done()


def build_s4a2(NB, moe, c=None):
    c = c or Ctx()
    T = NB * BLK
    TB = min(2, NB)
    NT = NB // TB
    NC = TB * 128
    xin = c.din("xin", [NB * EXT, D])
    oTd = c.din("oT", [512, T], BF16); ypod = c.din("ypo", [D, T], BF16); ysod = c.din("yso", [D, T], BF16)
    wbgd = c.din("wbg", [D, 3072]); wnod = c.din("w_nsa_out", [512, D]); woutd = c.din("w_out", [D, D])
    g1d = c.din("ln1_g", [1, D]); b1d = c.din("ln1_b", [1, D]); identd = c.din("ident", [128, 128])
    x1o = c.dout("x1", [T, D]); x1To = c.dout("x1T", [D, T], BF16)
    if moe:
        wrd = c.din("w_router", [D, 8]); brd = c.din("b_router", [1, 8])
        rwo = c.dout("rw", [T, 8])
    c.init_psum()
    bw = Buf()
    ident = c.sb([128, 128]); bid = Buf()
    c.load(ident[:, :], identd[:, :], [bid])
    stage = [(c.sb([128, 2048]), Buf()) for _ in range(2)]
    wbg = c.sb([128, 8, 3072], BF16)
    for k in range(8):
        for h0 in range(0, 3072, 1536):
            c.load_cast(wbg[:, k, h0:h0 + 1536], bw, wbgd[k * 128:(k + 1) * 128, h0:h0 + 1536], stage)
    wno = c.sb([128, 4, D], BF16)
    for g in range(4):
        c.load_cast(wno[:, g, :], bw, wnod[g * 128:(g + 1) * 128, :], stage)
    wout = c.sb([128, 8, D], BF16)
    for k in range(8):
        c.load_cast(wout[:, k, :], bw, woutd[k * 128:(k + 1) * 128, :], stage)
    g1 = bcast_load(c, g1d, D, bw); b1 = bcast_load(c, b1d, D, bw)
    if moe:
        wr = c.sb([128, 8, 8]); c.load(wr[:, :, :], wrd.rearrange("(k p) e -> p k e", p=128), [bw])
        brb = bcast_load(c, brd, 8, bw)

    xt = [c.sb([128, TB, D]) for _ in range(2)]; bxt = [Buf(), Buf()]
    xT = [c.sb([128, 8, NC], BF16) for _ in range(2)]; bxT = [Buf(), Buf()]
    oTt = [c.sb([128, 4, NC], BF16) for _ in range(2)]; ypt = [c.sb([128, 8, NC], BF16) for _ in range(2)]
    yst = [c.sb([128, 8, NC], BF16) for _ in range(2)]; bin_ = [Buf(), Buf()]
    gsb = [c.sb([128, NC]) for _ in range(3)]; bgsb = [Buf() for _ in range(3)]
    acc = c.sb([128, NC]); bacc = Buf()
    mT = c.sb([128, 8, NC], BF16); bmT = Buf()
    z = [c.sb([128, D]) for _ in range(2)]; bz = [Buf(), Buf()]
    x1 = [c.sb([128, D]) for _ in range(2)]; bx1 = [Buf(), Buf()]
    x1Tb = [c.sb([128, 8, 128], BF16) for _ in range(2)]; bx1T = [Buf(), Buf()]
    x1Tf = c.sb([128, 8, 128]); bx1Tf = Buf()
    lg = c.sb([128, 8]); v8 = c.sb([128, 8]); dl = c.sb([128, 2]); rwt = c.sb([128, 8]); rw2 = c.sb([128, 8]); brw = Buf()
    ln = LNorm(c, D)

    for ti in range(NT):
        p = ti % 2
        blk0 = ti * TB
        cols = slice(blk0 * 128, blk0 * 128 + NC)
        load_x_tile(c, xin, blk0, TB, xt[p], bxt[p], None, None)
        transpose_x(c, xt[p], bxt[p], TB, xT[p], bxT[p], ident, bid)
        c.load(oTt[p][:, :, :], oTd[:, cols].rearrange("(c p) t -> p c t", p=128), [bin_[p]])
        c.load(ypt[p][:, :, :], ypod[:, cols].rearrange("(c p) t -> p c t", p=128), [bin_[p]])
        c.load(yst[p][:, :, :], ysod[:, cols].rearrange("(c p) t -> p c t", p=128), [bin_[p]])
        for dc in range(8):
            for br in range(3):
                pt, pb = c.ps()
                for k in range(8):
                    c.mm(pt[:, 0:NC], wbg[:, k, br * 1024 + dc * 128:br * 1024 + (dc + 1) * 128], xT[p][:, k, :], k == 0,
                         k == 7, [bw, bxT[p]], [pb])
                c.act(gsb[br][:, :], pt[:, 0:NC], AF.Sigmoid, [pb], [bgsb[br]])
            pn, pnb = c.ps()
            for g in range(4):
                c.mm(pn[:, 0:NC], wno[:, g, dc * 128:(dc + 1) * 128], oTt[p][:, g, :], g == 0, g == 3, [bw, bin_[p]], [pnb])
            c.v("tensor_tensor", [bgsb[0], bin_[p]], [bacc], out=acc[:, :], in0=gsb[0][:, :], in1=ypt[p][:, dc, :], op=ALU.mult)
            c.v("tensor_tensor", [bgsb[1], bin_[p]], [bgsb[1]], eng="gpsimd", out=gsb[1][:, :], in0=gsb[1][:, :],
                in1=yst[p][:, dc, :], op=ALU.mult)
            c.v("tensor_tensor", [bgsb[2], pnb], [bgsb[2]], out=gsb[2][:, :], in0=gsb[2][:, :], in1=pn[:, 0:NC], op=ALU.mult)
            c.v("tensor_tensor", [bacc, bgsb[1]], [bacc], eng="gpsimd", out=acc[:, :], in0=acc[:, :], in1=gsb[1][:, :],
                op=ALU.add)
            c.v("tensor_tensor", [bacc, bgsb[2]], [bmT], out=mT[:, dc, :], in0=acc[:, :], in1=gsb[2][:, :], op=ALU.add)
        for b in range(TB):
            q = b % 2
            for half in range(2):
                pt, pb = c.ps()
                for k in range(8):
                    c.mm(pt[:, 0:512], mT[:, k, b * 128:(b + 1) * 128], wout[:, k, half * 512:(half + 1) * 512], k == 0,
                         k == 7, [bw, bmT], [pb])
                c.v("scalar_tensor_tensor", [bxt[p], pb], [bz[q]], out=z[q][:, half * 512:(half + 1) * 512],
                    in0=xt[p][:, b, half * 512:(half + 1) * 512], scalar=float(ALPHA), in1=pt[:, 0:512], op0=ALU.mult,
                    op1=ALU.add)
            ln(z[q][:, :], bz[q], g1[:, :], b1[:, :], bw, x1[q][:, :], bx1[q])
            rows = slice((blk0 + b) * 128, (blk0 + b + 1) * 128)
            c.store(x1o[rows, :], x1[q][:, :], [bx1[q]])
            for k in range(8):
                pt, pb = c.ps()
                c.tr(pt[:, 0:128], x1[q][:, k * 128:(k + 1) * 128], ident[:, :], [bx1[q], bid], [pb])
                c.act(x1Tb[q][:, k, :], pt[:, 0:128], AF.Copy, [pb], [bx1T[q]])
                if moe:
                    c.v("tensor_copy", [pb], [bx1Tf], out=x1Tf[:, k, :], in_=pt[:, 0:128])
            c.store(x1To[:, rows].rearrange("(c p) t -> p c t", p=128), x1Tb[q][:, :, :], [bx1T[q]])
            if moe:
                pt, pb = c.ps()
                for k in range(8):
                    c.mm(pt[:, 0:8], x1Tf[:, k, :], wr[:, k, :], k == 0, k == 7, [bx1Tf, bw], [pb])
                c.v("tensor_tensor", [pb, bw], [brw], out=lg[:, :], in0=pt[:, 0:8], in1=brb[:, :], op=ALU.add)
                c.v("max", [brw], [brw], out=v8[:, :], in_=lg[:, :])
                c.v("tensor_tensor", [brw], [brw], out=dl[:, 0:1], in0=v8[:, 0:1], in1=v8[:, 1:2], op=ALU.subtract)
                c.v("tensor_tensor", [brw], [brw], out=dl[:, 1:2], in0=v8[:, 1:2], in1=v8[:, 0:1], op=ALU.subtract)
                c.act(dl[:, :], dl[:, :], AF.Sigmoid, [brw], [brw])
                c.v("tensor_scalar", [brw], [brw], out=rwt[:, :], in0=lg[:, :], scalar1=v8[:, 0:1], scalar2=dl[:, 0:1],
                    op0=ALU.is_equal, op1=ALU.mult)
                c.v("tensor_scalar", [brw], [brw], out=rw2[:, :], in0=lg[:, :], scalar1=v8[:, 1:2], scalar2=dl[:, 1:2],
                    op0=ALU.is_equal, op1=ALU.mult)
                c.v("tensor_tensor", [brw], [brw], out=rwt[:, :], in0=rwt[:, :], in1=rw2[:, :], op=ALU.add)
                c.store(rwo[rows, :], rwt[:, :], [brw])
    return c.done()


def s4a_inputs(x, oT_res, L, W, S, consts):
    m1, m2 = [], []
    w_in = W["w_in"][L]
    wb1 = np.ascontiguousarray(w_in[:, 0:1536])
    wbg = np.ascontiguousarray(w_in[:, O_BG:O_BG + 3072])
    kk = np.arange(128)[:, None]; tt = np.arange(128)[None, :]
    trilT = (kk <= tt).astype(np.float32)
    sg_wT = np.ascontiguousarray(W["sg_w"][L].transpose(0, 2, 1))
    psc = np.ascontiguousarray(W["pool_scale"][L].reshape(4, 128).T)
    for core in range(8):
        b, blocks = core_block_ids(S, core)
        xin = make_xin(x[b], blocks)
        invc = np.zeros((128, 4, 16), np.float32)
        for g in range(4):
            w = 2 ** (g + 1)
            if blocks[0] == 0:
                invc[:, g, :] = 1.0 / np.minimum(np.arange(1, 17), w)
            else:
                invc[:, g, :] = 1.0 / w
        m1.append(dict(xin=xin, wb1=wb1, pool_w=W["pool_w"][L], pool_scale=psc, sg_ln_g=W["sg_ln_g"][L][None, :],
                       sg_ln_b=W["sg_ln_b"][L][None, :], sg_wT=sg_wT, sg_b=W["sg_b"][L].reshape(1, 512), trilT=trilT,
                       w_pool_out=W["w_pool_out"][L], w_sg_out=W["w_sg_out"][L], invc=invc.reshape(128, 64),
                       ident=consts["ident"]))
        d2 = dict(xin=xin, oT=oT_res[core]["oT"], wbg=wbg, w_nsa_out=W["w_nsa_out"][L], w_out=W["w_out"][L],
                  ln1_g=W["ln1_g"][L][None, :], ln1_b=W["ln1_b"][L][None, :], ident=consts["ident"])
        if L % 2 == 1:
            d2["w_router"] = W["moe_router"][L // 2]; d2["b_router"] = W["moe_router_b"][L // 2][None, :]
        m2.append(d2)
    return m1, m2


def build_s4b(NB, NE, c=None):
    c = c or Ctx()
    T = NB * BLK
    TB = min(4, NB)
    NT = NB // TB
    NC = TB * 128
    NFC = DFF // 128
    x1Td = c.din("x1T", [D, T], BF16)
    rwd = c.din("rw", [T, NE])
    wgd = c.din("wg", [NE, D, DFF]); wud = c.din("wu", [NE, D, DFF]); wdd = c.din("wd", [NE, DFF, D])
    fo = c.dout("f", [T, D])
    c.init_psum()
    x1T = [c.sb([128, 8, NC], BF16) for _ in range(2)]; bx = [Buf(), Buf()]
    rw = [c.sb([128, TB, NE]) for _ in range(2)]
    NW = 3
    wgst = [c.sb([128, 8, 128]) for _ in range(NW)]; bwgst = [Buf() for _ in range(NW)]
    wust = [c.sb([128, 8, 128]) for _ in range(NW)]; bwust = [Buf() for _ in range(NW)]
    wgc = [c.sb([128, 8, 128], BF16) for _ in range(NW)]; bwgc = [Buf() for _ in range(NW)]
    wuc = [c.sb([128, 8, 128], BF16) for _ in range(NW)]; bwuc = [Buf() for _ in range(NW)]
    wdst = [c.sb([128, D]) for _ in range(NW)]; bwdst = [Buf() for _ in range(NW)]
    wdb = [c.sb([128, NFC, D], BF16) for _ in range(2)]; bwdb = [Buf(), Buf()]
    hT = c.sb([128, NFC, NC], BF16); bhT = Buf()
    sg = [c.sb([128, NC]) for _ in range(2)]; bsg = [Buf(), Buf()]
    _f = c.sb([128, TB, D]); _bf = Buf()
    facc = [_f, _f]; bfacc = [_bf, _bf]
    it = 0
    for ti in range(NT):
        p = ti % 2
        cols = slice(ti * NC, (ti + 1) * NC)
        c.load(x1T[p][:, :, :], x1Td[:, cols].rearrange("(c p) t -> p c t", p=128), [bx[p]])
        c.load(rw[p][:, :, :], rwd[cols, :].rearrange("(n p) e -> p n e", p=128), [bx[p]],
               allow_slow_non_contiguous=True)
        for e in range(NE):
            wp = it % 2; it += 1
            for cc in range(NFC):
                i = cc % NW
                fs = slice(cc * 128, (cc + 1) * 128)
                c.load(wgst[i][:, :, :], wgd[e][:, fs].rearrange("(k p) f -> p k f", p=128), [bwgst[i]], q="sync")
                c.load(wust[i][:, :, :], wud[e][:, fs].rearrange("(k p) f -> p k f", p=128), [bwust[i]], q="sync")
                c.load(wdst[i][:, :], wdd[e][fs, :], [bwdst[i]], q="gpsimd")
                c.v("tensor_copy", [bwgst[i]], [bwgc[i]], out=wgc[i][:, :, :], in_=wgst[i][:, :, :])
                c.act(wuc[i][:, :, :], wust[i][:, :, :], AF.Copy, [bwust[i]], [bwuc[i]])
                c.v("tensor_copy", [bwdst[i]], [bwdb[wp]], eng="gpsimd", out=wdb[wp][:, cc, :], in_=wdst[i][:, :])
                pg, pgb = c.ps()
                for k in range(8):
                    c.mm(pg[:, 0:NC], wgc[i][:, k, :], x1T[p][:, k, :], k == 0, k == 7, [bwgc[i], bx[p]], [pgb])
                pu, pub = c.ps()
                for k in range(8):
                    c.mm(pu[:, 0:NC], wuc[i][:, k, :], x1T[p][:, k, :], k == 0, k == 7, [bwuc[i], bx[p]], [pub])
                s = cc % 2
                c.act(sg[s][:, :], pg[:, 0:NC], AF.Silu, [pgb], [bsg[s]])
                c.v("tensor_tensor", [bsg[s], pub], [bhT], out=hT[:, cc, :], in0=sg[s][:, :], in1=pu[:, 0:NC], op=ALU.mult)
            for b in range(TB):
                for half in range(2):
                    pt, pb = c.ps()
                    for cc in range(NFC):
                        c.mm(pt[:, 0:512], hT[:, cc, b * 128:(b + 1) * 128], wdb[wp][:, cc, half * 512:(half + 1) * 512],
                             cc == 0, cc == NFC - 1, [bhT, bwdb[wp]], [pb])
                    dst = facc[p][:, b, half * 512:(half + 1) * 512]
                    if e == 0:
                        c.v("tensor_scalar", [pb, bx[p]], [bfacc[p]], out=dst, in0=pt[:, 0:512], scalar1=rw[p][:, b, e:e + 1],
                            scalar2=None, op0=ALU.mult)
                    else:
                        c.v("scalar_tensor_tensor", [pb, bx[p], bfacc[p]], [bfacc[p]], out=dst, in0=pt[:, 0:512],
                            scalar=rw[p][:, b, e:e + 1], in1=dst, op0=ALU.mult, op1=ALU.add)
        c.store(fo[cols, :].rearrange("(n p) d -> p n d", p=128), facc[p][:, :, :], [bfacc[p]])
    return c.done()


def build_s4c(NB, c=None):
    c = c or Ctx()
    T = NB * BLK
    x1d = c.din("x1", [T, D]); x1Td = c.din("x1T", [D, T], BF16); fd = c.din("f", [T, D]); pd = c.din("p", [T, PLE])
    wpgd = c.din("wpg", [D, D]); bpgd = c.din("bpg", [1, D]); wppd = c.din("wpp", [PLE, D])
    g2d = c.din("ln2_g", [1, D]); b2d = c.din("ln2_b", [1, D]); identd = c.din("ident", [128, 128])
    xo = c.dout("x2", [T, D])
    c.init_psum()
    bw = Buf()
    ident = c.sb([128, 128]); bid = Buf()
    c.load(ident[:, :], identd[:, :], [bid])
    stage = [(c.sb([128, 2048]), Buf()) for _ in range(2)]
    wpg = c.sb([128, 8, D], BF16)
    for k in range(8):
        c.load_cast(wpg[:, k, :], bw, wpgd[k * 128:(k + 1) * 128, :], stage)
    wpp = c.sb([128, 2, D], BF16)
    for k in range(2):
        c.load_cast(wpp[:, k, :], bw, wppd[k * 128:(k + 1) * 128, :], stage)
    bpg = bcast_load(c, bpgd, D, bw); g2 = bcast_load(c, g2d, D, bw); b2 = bcast_load(c, b2d, D, bw)
    x1 = [c.sb([128, D]) for _ in range(2)]; f = [c.sb([128, D]) for _ in range(2)]; pt_ = [c.sb([128, PLE]) for _ in range(2)]
    x1T = [c.sb([128, 8, 128], BF16) for _ in range(2)]; bin_ = [Buf(), Buf()]
    pT = [c.sb([128, 2, 128], BF16) for _ in range(2)]; bpT = [Buf(), Buf()]
    gate = [c.sb([128, D]) for _ in range(2)]; bgate = [Buf(), Buf()]
    z = [c.sb([128, D]) for _ in range(2)]; bz = [Buf(), Buf()]
    out = [c.sb([128, D]) for _ in range(2)]; bout = [Buf(), Buf()]
    ln = LNorm(c, D)
    for j in range(NB):
        q = j % 2
        rows = slice(j * 128, (j + 1) * 128)
        c.load(x1[q][:, :], x1d[rows, :], [bin_[q]]); c.load(f[q][:, :], fd[rows, :], [bin_[q]])
        c.load(pt_[q][:, :], pd[rows, :], [bin_[q]])
        c.load(x1T[q][:, :, :], x1Td[:, rows].rearrange("(c p) t -> p c t", p=128), [bin_[q]])
        for k in range(2):
            tp, tb = c.ps()
            c.tr(tp[:, 0:128], pt_[q][:, k * 128:(k + 1) * 128], ident[:, :], [bin_[q], bid], [tb])
            c.v("tensor_copy", [tb], [bpT[q]], out=pT[q][:, k, :], in_=tp[:, 0:128])
        for half in range(2):
            hs = slice(half * 512, (half + 1) * 512)
            pg, pgb = c.ps()
            for k in range(8):
                c.mm(pg[:, 0:512], x1T[q][:, k, :], wpg[:, k, hs], k == 0, k == 7, [bin_[q], bw], [pgb])
            c.v("tensor_tensor", [pgb, bw], [bgate[q]], out=gate[q][:, hs], in0=pg[:, 0:512], in1=bpg[:, hs], op=ALU.add)
            c.act(gate[q][:, hs], gate[q][:, hs], AF.Sigmoid, [bgate[q]], [bgate[q]])
            pp, ppb = c.ps()
            for k in range(2):
                c.mm(pp[:, 0:512], pT[q][:, k, :], wpp[:, k, hs], k == 0, k == 1, [bpT[q], bw], [ppb])
            c.v("tensor_tensor", [bgate[q], ppb], [bgate[q]], out=gate[q][:, hs], in0=gate[q][:, hs], in1=pp[:, 0:512],
                op=ALU.mult)
        c.v("scalar_tensor_tensor", [bin_[q]], [bz[q]], out=z[q][:, :], in0=x1[q][:, :], scalar=float(ALPHA),
            in1=f[q][:, :], op0=ALU.mult, op1=ALU.add)
        c.v("tensor_tensor", [bz[q], bgate[q]], [bz[q]], out=z[q][:, :], in0=z[q][:, :], in1=gate[q][:, :], op=ALU.add)
        ln(z[q][:, :], bz[q], g2[:, :], b2[:, :], bw, out[q][:, :], bout[q])
        c.store(xo[rows, :], out[q][:, :], [bout[q]])
    return c.done()


_PROGS = {}


def _prog(key, fn):
    if key not in _PROGS:
        _PROGS[key] = fn()
    return _PROGS[key]


def _run(nc, maps):
    return run_bass_kernel_spmd(nc, maps, core_ids=list(range(8))).results


def build_B(NB, S, moe):
    c = Ctx(chained=True)
    build_s3(NB, S, c=c)
    build_s4a1(NB, c=c)
    build_s4a2(NB, moe, c=c)
    build_s4b(NB, NEXP if moe else 1, c=c)
    build_s4c(NB, c=c)
    names = list(c.in_names)
    return c.finish(), names


def forward(inp, S):
    W = {k: np.asarray(v) for k, v in inp.items()}
    x = np.ascontiguousarray(W["x"], dtype=np.float32)
    positions = np.asarray(W["positions"]).astype(np.int32)
    consts = make_consts()
    NB = S // BLK // 4
    for L in range(DEPTH):
        moe = (L % 2 == 1)
        r1 = _run(_prog(("s1a", NB), lambda: build_s1a(NB)), s1a_inputs(x, positions, L, W, S, consts))
        m3 = s3_inputs(r1, S)
        m1, m2 = s4a_inputs(x, [dict(oT=None)] * 8, L, W, S, consts)
        del r1
        j = L // 2
        if moe:
            wg, wu, wd = W["moe_w_gate"][j], W["moe_w_up"][j], W["moe_w_down"][j]
        else:
            wg, wu, wd = W["ffn_w_gate"][j][None], W["ffn_w_up"][j][None], W["ffn_w_down"][j][None]
        ncB, names = _prog(("B", NB, S, moe), lambda: build_B(NB, S, moe))
        maps = []
        for core in range(8):
            b, blocks = core_block_ids(S, core)
            tok = np.concatenate([np.arange(n * BLK, (n + 1) * BLK) for n in blocks])
            full = {}
            full.update(m3[core]); full.update(m1[core]); full.update(m2[core])
            full.update(dict(wg=wg, wu=wu, wd=wd, rw=np.ones((NB * BLK, 1), np.float32),
                             p=np.ascontiguousarray(W["p"][L][b][tok]), wpg=W["ple_gate_w"][L],
                             bpg=W["ple_gate_b"][L][None, :], wpp=W["ple_proj"][L], ln2_g=W["ln2_g"][L][None, :],
                             ln2_b=W["ln2_b"][L][None, :]))
            maps.append({k: full[k] for k in names})
        del m1, m2, m3
        rc = _run(ncB, maps)
        del maps
        xn = np.empty_like(x)
        for core in range(8):
            b, blocks = core_block_ids(S, core)
            for i, n in enumerate(blocks):
                xn[b, n * BLK:(n + 1) * BLK] = rc[core]["x2"][i * BLK:(i + 1) * BLK]
        x = xn
        del rc
    return x


def kernel(**inputs):
    S = int(np.asarray(inputs["x"]).shape[1])
    return forward(inputs, S).astype(np.float32)
```
